# Optimizing a Trainium2 kernel written in Bass

```python
import jax, jax.numpy as jnp
from jax import lax
import numpy as np

D_MODEL = 4096
BATCH = 4
SEQ = 4096
DEPTH = 1

GRID_W = 64
CTX_LEN = 256
HEAD_DIM = 128
POOL_W = D_MODEL // 4
ATTN_W = D_MODEL - POOL_W
N_HEADS = ATTN_W // HEAD_DIM
N_KV_HEADS = N_HEADS // 3
Q_PER_KV = N_HEADS // N_KV_HEADS
KV_W = N_KV_HEADS * HEAD_DIM
IN_W = ATTN_W + 2 * KV_W + POOL_W
ROPE_PAIRS_PER_AXIS = HEAD_DIM // 4
ROPE_THETA = 10000.0
Q_BLOCK = 128
POOL_WINDOWS = (2, 4, 8, 16)
POOL_GROUP_W = POOL_W // len(POOL_WINDOWS)
N_EXPERTS = 32
TOP_K = 4
EXPERT_FF = 1536
SWIGLU_ALPHA = 1.702
SWIGLU_LIMIT = 7.0
MOE_BLOCK = 256
N_MOD = 6
EPS = 1e-6
ATTN_SCALE = HEAD_DIM ** -0.5

kernel_name = 'hybrid_attn_pool_moe_flow_layer'


def rmsnorm(x, g):
    xf = x.astype(jnp.float32)
    y = xf * lax.rsqrt(jnp.mean(xf * xf, axis=-1, keepdims=True) + EPS)
    return (y * g.astype(jnp.float32)).astype(x.dtype)


def modulate(h, shift, scale):
    return h * (1 + scale) + shift


def axial_rope_tables(seq_len):
    rows = seq_len // GRID_W
    row = jnp.repeat(jnp.arange(rows, dtype=jnp.float32), GRID_W)
    col = jnp.tile(jnp.arange(GRID_W, dtype=jnp.float32), rows)
    freqs = ROPE_THETA ** (-jnp.arange(ROPE_PAIRS_PER_AXIS, dtype=jnp.float32) / ROPE_PAIRS_PER_AXIS)
    ang = jnp.concatenate([row[:, None] * freqs, col[:, None] * freqs], axis=-1)
    return jnp.cos(ang), jnp.sin(ang)


def apply_rope(x, cos, sin):
    half = HEAD_DIM // 2
    cos = cos[None, :, None, :].astype(x.dtype)
    sin = sin[None, :, None, :].astype(x.dtype)
    x1, x2 = x[..., :half], x[..., half:]
    return jnp.concatenate([x1 * cos - x2 * sin, x1 * sin + x2 * cos], axis=-1)


def split_in(z):
    return (z[..., :ATTN_W], z[..., ATTN_W:ATTN_W + KV_W],
            z[..., ATTN_W + KV_W:ATTN_W + 2 * KV_W], z[..., ATTN_W + 2 * KV_W:])


def attention(q, k, v):
    s = jnp.einsum('bqkgd,bskd->bkgqs', q, k, preferred_element_type=jnp.float32) * ATTN_SCALE
    p = jax.nn.softmax(s, axis=-1).astype(v.dtype)
    o = jnp.einsum('bkgqs,bskd->bqkgd', p, v)
    return o.reshape(o.shape[0], o.shape[1], -1)


def blocked_attention(q, k, v):
    B, L = q.shape[:2]
    qb = jnp.moveaxis(q.reshape(B, L // Q_BLOCK, Q_BLOCK, *q.shape[2:]), 1, 0)
    o = lax.map(lambda qi: attention(qi, k, v), qb)
    return jnp.moveaxis(o, 0, 1).reshape(B, L, -1)


def pool_mixer(u, w_pool, pool_scale):
    B, L, _ = u.shape
    uf = u.astype(jnp.float32).reshape(B, L, len(POOL_WINDOWS), POOL_GROUP_W)
    cs = jnp.pad(jnp.cumsum(uf, axis=1), ((0, 0), (1, 0), (0, 0), (0, 0)))
    t = jnp.arange(L)
    outs = []
    for gi, w in enumerate(POOL_WINDOWS):
        lo = jnp.clip(t - w // 2, 0, L)
        hi = jnp.clip(t + w // 2, 0, L)
        cg = cs[:, :, gi]
        cnt = (hi - lo).astype(jnp.float32)[None, :, None]
        outs.append((cg[:, hi] - cg[:, lo]) / cnt - uf[:, :, gi])
    d = jnp.stack(outs, axis=2).astype(u.dtype)
    y = jnp.einsum('blgc,gce->blge', d, w_pool)
    return y.reshape(B, L, POOL_W) * pool_scale


def moe_ffn(h, w_router, b_router, w_gate, b_gate, w_up, b_up, w_down, b_down):
    n, d = h.shape
    logits = jnp.einsum('nd,de->ne', h, w_router, preferred_element_type=jnp.float32) + b_router.astype(jnp.float32)
    top_logit, top_idx = lax.top_k(logits, TOP_K)
    gate = jax.nn.softmax(top_logit, axis=-1)
    n_assign = n * TOP_K
    e_flat = top_idx.reshape(n_assign)
    order = jnp.argsort(e_flat)
    e_sorted = e_flat[order]
    tok_sorted = (order // TOP_K).astype(jnp.int32)
    gate_sorted = gate.reshape(n_assign)[order]
    counts = jnp.bincount(e_flat, length=N_EXPERTS)
    starts = jnp.cumsum(counts) - counts
    padded = (counts + MOE_BLOCK - 1) // MOE_BLOCK * MOE_BLOCK
    padded_end = jnp.cumsum(padded)
    dest = padded_end[e_sorted] - padded[e_sorted] + jnp.arange(n_assign) - starts[e_sorted]
    n_blocks = -(-(n_assign + N_EXPERTS * (MOE_BLOCK - 1)) // MOE_BLOCK)
    slots = n_blocks * MOE_BLOCK
    tok_buf = jnp.zeros((slots,), jnp.int32).at[dest].set(tok_sorted).reshape(n_blocks, MOE_BLOCK)
    gate_buf = jnp.zeros((slots,), jnp.float32).at[dest].set(gate_sorted).reshape(n_blocks, MOE_BLOCK)
    block_expert = jnp.minimum(
        jnp.searchsorted(padded_end, jnp.arange(n_blocks) * MOE_BLOCK, side='right'), N_EXPERTS - 1)

    def expert_block(acc, blk):
        tok, g, e = blk
        xb = h[tok]
        a = jnp.minimum(xb @ w_gate[e] + b_gate[e], SWIGLU_LIMIT)
        b = jnp.clip(xb @ w_up[e] + b_up[e], -SWIGLU_LIMIT, SWIGLU_LIMIT)
        y = ((b + 1) * (a * jax.nn.sigmoid(SWIGLU_ALPHA * a))) @ w_down[e] + b_down[e]
        return acc.at[tok].add(y.astype(jnp.float32) * g[:, None]), None

    acc, _ = lax.scan(expert_block, jnp.zeros((n, d), jnp.float32), (tok_buf, gate_buf, block_expert))
    return acc.astype(h.dtype)


def setup_inputs(seed: int = 0) -> dict:
    key = jax.random.key(seed)
    ks = jax.random.split(key, 23)
    f32 = jnp.float32

    def nrm(k, shape, scale):
        return jax.random.normal(k, shape, f32) * scale

    L = DEPTH
    return {
        'x': nrm(ks[0], (BATCH, SEQ, D_MODEL), 1.0),
        'c': nrm(ks[1], (BATCH, D_MODEL), 1.0),
        'ctx': nrm(ks[2], (BATCH, CTX_LEN, D_MODEL), 1.0),
        'c_ctx': nrm(ks[3], (D_MODEL,), 1.0),
        'w_ada': nrm(ks[4], (L, D_MODEL, N_MOD * D_MODEL), 0.5 * D_MODEL ** -0.5),
        'b_ada': nrm(ks[5], (L, N_MOD * D_MODEL), 0.02),
        'norm1_g': 1.0 + nrm(ks[6], (L, D_MODEL), 0.05),
        'w_in': nrm(ks[7], (L, D_MODEL, IN_W), D_MODEL ** -0.5),
        'q_norm_g': 1.0 + nrm(ks[8], (L, HEAD_DIM), 0.05),
        'k_norm_g': 1.0 + nrm(ks[9], (L, HEAD_DIM), 0.05),
        'w_pool': nrm(ks[10], (L, len(POOL_WINDOWS), POOL_GROUP_W, POOL_GROUP_W), POOL_GROUP_W ** -0.5),
        'pool_scale': 1.0 + nrm(ks[11], (L, POOL_W), 0.1),
        'w_out': nrm(ks[12], (L, D_MODEL, D_MODEL), D_MODEL ** -0.5),
        'norm2_g': 1.0 + nrm(ks[13], (L, D_MODEL), 0.05),
        'w_router': nrm(ks[14], (L, D_MODEL, N_EXPERTS), D_MODEL ** -0.5),
        'b_router': nrm(ks[15], (L, N_EXPERTS), 0.01),
        'w_gate': nrm(ks[16], (L, N_EXPERTS, D_MODEL, EXPERT_FF), D_MODEL ** -0.5),
        'b_gate': nrm(ks[17], (L, N_EXPERTS, EXPERT_FF), 0.02),
        'w_up': nrm(ks[18], (L, N_EXPERTS, D_MODEL, EXPERT_FF), D_MODEL ** -0.5),
        'b_up': nrm(ks[19], (L, N_EXPERTS, EXPERT_FF), 0.02),
        'w_down': nrm(ks[20], (L, N_EXPERTS, EXPERT_FF, D_MODEL), EXPERT_FF ** -0.5),
        'b_down': nrm(ks[21], (L, N_EXPERTS, D_MODEL), 0.02),
        'final_g': 1.0 + nrm(ks[22], (D_MODEL,), 0.05),
    }


def reference(x, c, ctx, c_ctx, w_ada, b_ada, norm1_g, w_in, q_norm_g, k_norm_g, w_pool, pool_scale,
              w_out, norm2_g, w_router, b_router, w_gate, b_gate, w_up, b_up, w_down, b_down, final_g):
    B, S, D = x.shape
    C = ctx.shape[1]
    cos, sin = axial_rope_tables(S)
    for l in range(DEPTH):
        last = l == DEPTH - 1
        mod = (jnp.einsum('bd,de->be', jax.nn.silu(c), w_ada[l]) + b_ada[l]).reshape(B, N_MOD, 1, D)
        mod_c = (jax.nn.silu(c_ctx) @ w_ada[l] + b_ada[l]).reshape(N_MOD, 1, D)

        h = modulate(rmsnorm(x, norm1_g[l]), mod[:, 0], mod[:, 1])
        hc = modulate(rmsnorm(ctx, norm1_g[l]), mod_c[0], mod_c[1])
        q, k, v, u = split_in(jnp.einsum('bld,de->ble', h, w_in[l]))
        q = apply_rope(rmsnorm(q.reshape(B, S, N_HEADS, HEAD_DIM), q_norm_g[l]), cos, sin)
        k = apply_rope(rmsnorm(k.reshape(B, S, N_KV_HEADS, HEAD_DIM), k_norm_g[l]), cos, sin)
        v = v.reshape(B, S, N_KV_HEADS, HEAD_DIM)
        if last:
            kc, vc = jnp.split(jnp.einsum('bld,de->ble', hc, w_in[l][:, ATTN_W:ATTN_W + 2 * KV_W]), 2, axis=-1)
        else:
            qc, kc, vc, uc = split_in(jnp.einsum('bld,de->ble', hc, w_in[l]))
        kc = rmsnorm(kc.reshape(B, C, N_KV_HEADS, HEAD_DIM), k_norm_g[l])
        vc = vc.reshape(B, C, N_KV_HEADS, HEAD_DIM)
        k_all = jnp.concatenate([kc, k], axis=1)
        v_all = jnp.concatenate([vc, v], axis=1)
        attn = blocked_attention(q.reshape(B, S, N_KV_HEADS, Q_PER_KV, HEAD_DIM), k_all, v_all)
        mix = jnp.concatenate([attn, pool_mixer(u, w_pool[l], pool_scale[l])], axis=-1)
        x = x + mod[:, 2] * jnp.einsum('ble,ed->bld', mix, w_out[l])
        if not last:
            qc = rmsnorm(qc.reshape(B, C, N_HEADS, HEAD_DIM), q_norm_g[l]).reshape(B, C, N_KV_HEADS, Q_PER_KV, HEAD_DIM)
            mix_c = jnp.concatenate([attention(qc, kc, vc), pool_mixer(uc, w_pool[l], pool_scale[l])], axis=-1)
            ctx = ctx + mod_c[2] * jnp.einsum('ble,ed->bld', mix_c, w_out[l])

        h2 = modulate(rmsnorm(x, norm2_g[l]), mod[:, 3], mod[:, 4]).reshape(B * S, D)
        moe_w = (w_router[l], b_router[l], w_gate[l], b_gate[l], w_up[l], b_up[l], w_down[l], b_down[l])
        if last:
            x = x + mod[:, 5] * moe_ffn(h2, *moe_w).reshape(B, S, D)
        else:
            h2c = modulate(rmsnorm(ctx, norm2_g[l]), mod_c[3], mod_c[4]).reshape(B * C, D)
            f = moe_ffn(jnp.concatenate([h2, h2c], axis=0), *moe_w)
            x = x + mod[:, 5] * f[:B * S].reshape(B, S, D)
            ctx = ctx + mod_c[5] * f[B * S:].reshape(B, C, D)
    return rmsnorm(x, final_g)
```

```python
import numpy as np
import concourse.bass as bass
import concourse.mybir as mybir
from concourse.bass_utils import run_bass_kernel_spmd
from contextlib import ExitStack

F32 = mybir.dt.float32
BF16 = mybir.dt.bfloat16
U32 = mybir.dt.uint32
AF = mybir.ActivationFunctionType
ALU = mybir.AluOpType
AX = mybir.AxisListType


class Buf:
    def __init__(self, name, ap):
        self.name = name
        self._ap = ap
        self.lw = None
        self.rd = []

    def ap(self):
        return self._ap


class Sched:
    def __init__(self, nc, n_dma_sems=24):
        self.nc = nc
        self.eng = {"pe": nc.tensor, "act": nc.scalar, "dve": nc.vector,
                    "pool": nc.gpsimd, "sync": nc.sync}
        self.sems = {}
        self.tick = {}
        self.waited = {k: {} for k in self.eng}
        for k in ("pe", "act", "dve", "pool"):
            self.sems[k] = nc.alloc_semaphore("s_" + k)
            self.tick[k] = 0
        self.dpool = {}
        for q in ("sync", "pool", "act"):
            lst = []
            for i in range(n_dma_sems if q != "act" else 8):
                key = "d_%s_%d" % (q, i)
                self.sems[key] = nc.alloc_semaphore(key)
                lst.append([key, 0])
            self.dpool[q] = [lst, 0]
        self.out_deps = []
        self.n_ops = 0

    def sbuf(self, name, shape, dt):
        return Buf(name, self.nc.alloc_sbuf_tensor(name, list(shape), dt).ap())

    def psum(self, name, shape, dt):
        return Buf(name, self.nc.alloc_psum_tensor(name, list(shape), dt).ap())

    def dram(self, ap, name):
        return Buf(name, ap)

    def scratch(self, name, shape, dt):
        return Buf(name, self.nc.dram_tensor(name, list(shape), dt, kind="Internal").ap())

    def _wait(self, engine, key, val):
        if key == engine and engine == "pe":
            return
        w = self.waited[engine]
        if w.get(key, 0) >= val:
            return
        self.eng[engine].wait_ge(self.sems[key], val)
        w[key] = val

    def _deps(self, r, w):
        deps = []
        for b in r:
            if b.lw is not None:
                deps.append(b.lw)
        for b in w:
            if b.lw is not None:
                deps.append(b.lw)
            deps.extend(b.rd)
        return deps

    def _mark(self, me, r, w):
        for b in r:
            b.rd.append(me)
            if len(b.rd) > 64:
                d = {}
                for k, v in b.rd:
                    d[k] = max(d.get(k, 0), v)
                b.rd = list(d.items())
        for b in w:
            b.lw = me
            b.rd = []

    def op(self, engine, fn, r=(), w=()):
        for k, v in self._deps(r, w):
            self._wait(engine, k, v)
        ins = fn(self.eng[engine])
        self.tick[engine] += 1
        ins.then_inc(self.sems[engine], 1)
        self._mark((engine, self.tick[engine]), r, w)
        self.n_ops += 1
        return ins

    def dma(self, queue, out_ap, in_ap, r=(), w=(), is_output=False, **kw):
        for k, v in self._deps(r, w):
            self._wait(queue, k, v)
        lst, idx = self.dpool[queue]
        ent = lst[idx % len(lst)]
        self.dpool[queue][1] = idx + 1
        key, cnt = ent
        if cnt > 0:
            self._wait(queue, key, 16 * cnt)
        ins = self.eng[queue].dma_start(out=out_ap, in_=in_ap, **kw)
        ent[1] = cnt + 1
        ins.then_inc(self.sems[key], 16)
        me = (key, 16 * (cnt + 1))
        self._mark(me, r, w)
        if is_output:
            self.out_deps.append(me)
        self.n_ops += 1
        return ins

    def idma(self, out_ap, in_ap, idx_ap, scatter, r=(), w=()):
        queue = "pool"
        for k, v in self._deps(r, w):
            self._wait(queue, k, v)
        lst, idx = self.dpool[queue]
        ent = lst[idx % len(lst)]
        self.dpool[queue][1] = idx + 1
        key, cnt = ent
        if cnt > 0:
            self._wait(queue, key, 16 * cnt)
        off = bass.IndirectOffsetOnAxis(ap=idx_ap, axis=0)
        if scatter:
            ins = self.nc.gpsimd.indirect_dma_start(out=out_ap, out_offset=off, in_=in_ap, in_offset=None)
        else:
            ins = self.nc.gpsimd.indirect_dma_start(out=out_ap, out_offset=None, in_=in_ap, in_offset=off)
        ent[1] = cnt + 1
        ins.then_inc(self.sems[key], 16)
        self._mark((key, 16 * (cnt + 1)), r, w)
        self.n_ops += 1
        return ins

    def finish(self):
        for q in ("sync", "pool", "act"):
            for key, cnt in self.dpool[q][0]:
                if cnt > 0:
                    self._wait("sync", key, 16 * cnt)
        for k in ("pe", "act", "dve", "pool"):
            if self.tick[k] > 0:
                self._wait("sync", k, self.tick[k])


def make_cfg(D=4096, S=4096, CTX=256, E=32, FF=1536, B=4):
    c = dict(D=D, S=S, CTX=CTX, E=E, FF=FF, B=B)
    c["KC"] = D // 128
    c["HALF"] = S // 2
    c["NT"] = S + CTX
    c["G"] = min(512, c["HALF"])
    c["ATTN_W"] = 3 * D // 4
    c["NH"] = c["ATTN_W"] // 128
    c["NKV"] = c["NH"] // 3
    c["KVW"] = c["NKV"] * 128
    c["POOL_W"] = D // 4
    c["PG"] = c["POOL_W"] // 4
    c["IN_W"] = c["ATTN_W"] + 2 * c["KVW"] + c["POOL_W"]
    c["FC"] = FF // 128
    return c


FULL = make_cfg()
_STOP = ''
EPS = 1e-6
ROPE_THETA = 10000.0
GRID_W = 64
TOPK = 4


def bcast_rows(buf_ap_tensor, offset, n, parts=128):
    return bass.AP(tensor=buf_ap_tensor, offset=offset, ap=[[0, parts], [1, n]])


def build_program(cfg, debug=False):
    D, KC, HALF, NT, G, CTX = cfg["D"], cfg["KC"], cfg["HALF"], cfg["NT"], cfg["G"], cfg["CTX"]
    NH, NKV, KVW, ATTN_W, POOL_W, PG, IN_W = (cfg["NH"], cfg["NKV"], cfg["KVW"], cfg["ATTN_W"],
                                             cfg["POOL_W"], cfg["PG"], cfg["IN_W"])
    E, FF, FC, S = cfg["E"], cfg["FF"], cfg["FC"], cfg["S"]
    GT_ = G // 128
    TT = NT // 128
    NG_OWN = HALF // G
    PCH = POOL_W // 128
    PGC = PG // 128
    KS = 8
    assert KC % KS == 0 and FC % 2 == 0 and CTX <= G and CTX % 128 == 0

    nc = bass.Bass("TRN2", target_bir_lowering=False)
    S_ = Sched(nc)

    def din(name, shape, dt=F32):
        return S_.dram(nc.dram_tensor(name, list(shape), dt, kind="ExternalInput").ap(), name)

    xin = din("xin", [NT, D])
    cT = din("cT", [128, KC, 2])
    w_ada = din("w_ada", [D, 6 * D])
    bT_ada = din("bT_ada", [128, 6 * KC])
    g1T = din("g1T", [128, KC])
    g2T = din("g2T", [128, KC])
    fg_row = din("fg_row", [1, D])
    w_in = din("w_in", [D, IN_W])
    gq = din("gq", [128, 128])
    gk = din("gk", [128, 128])
    w_pool = din("w_pool", [4 * PG, PG])
    pscT = din("pscT", [128, PCH])
    w_out = din("w_out", [D, D])
    w_router = din("w_router", [128, KC * E])
    b_router = din("b_router", [128, E])
    DC = D // 512
    H2 = FC // 2
    BS = G
    NTL = HALF // 128
    NB = 4 * HALF // BS + E
    NSLOT = NB * BS
    w_gate = din("w_gate", [E * FC * 128, KC * 128])
    bgT = din("bgT", [E * 128, FC])
    w_up = din("w_up", [E * FC * 128, KC * 128])
    buT = din("buT", [E * 128, FC])
    w_down = din("w_down", [E * DC * 2 * 128, H2 * 512])
    b_down = din("b_down", [E, D])
    ustrict = din("ustrict", [128, 128])
    iota_e = din("iota_e", [128, E])
    iota_pf = din("iota_pf", [128, FC])
    iota_pd = din("iota_pd", [128, DC * 2])
    iota_p = din("iota_p", [128, 1])
    blkthr = din("blkthr", [128, NB])
    cs = din("cs", [NT, 128])
    ident_in = din("ident", [128, 128])
    invcnt = din("invcnt", [4, HALF])
    hmask = din("hmask", [1, 16])

    out = S_.dram(nc.dram_tensor("out", [HALF, D], F32, kind="ExternalOutput").ap(), "out")

    dbg_kind = "ExternalOutput" if debug else "Internal"

    def scr(name, shape, dt):
        return S_.dram(nc.dram_tensor(name, list(shape), dt, kind=dbg_kind).ap(), name)

    qT_s = scr("qT_s", [NH, 128, HALF], BF16)
    kT_s = scr("kT_s", [NKV, 128, NT], BF16)
    v_s = scr("v_s", [NT, KVW], BF16)
    uT_s = scr("uT_s", [PCH, 128, HALF + 16], F32)
    mixT_s = scr("mixT_s", [KC, 128, HALF], BF16)
    x1_s = scr("x1_s", [HALF, D], F32)
    h2tok_s = scr("h2tok_s", [HALF, D], BF16)
    xs_s = scr("xs_s", [NSLOT, D], BF16)
    NYS = 2
    YW = D // NYS
    ys_parts = [scr("ys_s%d" % i, [NSLOT, YW], F32) for i in range(NYS)]
    modrow_s = scr("modrow_s", [4, D], F32)

    banks = [S_.psum("bank%d" % i, [128, 512], F32) for i in range(8)]

    ident = S_.sbuf("ident_sb", [128, 128], F32)
    identb = S_.sbuf("identb", [128, 128], BF16)
    modT = S_.sbuf("modT", [128, 6 * KC, 2], F32)
    s1 = S_.sbuf("s1", [128, KC, 2], F32)
    s2 = S_.sbuf("s2", [128, KC], F32)
    slot_u = S_.sbuf("slot_u", [128, NTL, 4], U32)
    gk_all = S_.sbuf("gk_all", [128, NTL, 4], F32)
    widx_g = S_.sbuf("widx_g", [128, NB, FC], U32)
    widx_d = S_.sbuf("widx_d", [128, NB, DC * 2], U32)
    bidx = S_.sbuf("bidx", [128, NB], U32)
    bdidx = S_.sbuf("bdidx", [128, NB], U32)
    S_.dma("sync", ident.ap(), ident_in.ap(), r=[ident_in], w=[ident])
    S_.op("dve", lambda e: e.tensor_copy(identb.ap(), ident.ap()), r=[ident], w=[identb])

    def barrier():
        keys = []
        for k in ("pe", "act", "dve", "pool"):
            if S_.tick[k] > 0:
                keys.append((k, S_.tick[k]))
        for q in ("sync", "pool", "act"):
            for key, cnt in S_.dpool[q][0]:
                if cnt > 0:
                    keys.append((key, 16 * cnt))
        for eng in ("pe", "act", "dve", "pool", "sync"):
            for k, v in keys:
                if k != eng:
                    S_._wait(eng, k, v)

    def rstd_from_ss(ss, rstd, n, inv_n):
        S_.op("dve", lambda e: e.tensor_scalar(rstd.ap()[:, 0:n], ss.ap()[:, 0:n], inv_n, EPS, ALU.mult, ALU.add),
              r=[ss], w=[rstd])
        S_.op("act", lambda e: e.activation(rstd.ap()[:, 0:n], rstd.ap()[:, 0:n], AF.Sqrt), r=[rstd], w=[rstd])
        S_.op("dve", lambda e: e.reciprocal(rstd.ap()[:, 0:n], rstd.ap()[:, 0:n]), r=[rstd], w=[rstd])

    with ExitStack() as es:
        def sb(name, shape, dt):
            return Buf(name, es.enter_context(nc.sbuf_tensor(name, list(shape), dt)).ap())
        sc = sb("sc", [128, KC, 2], F32)
        aw = [sb("adaw0", [128, KC, 128], F32), sb("adaw1", [128, KC, 128], F32)]
        sm = sb("smallT", [128, 6 * KC], F32); rowt = sb("rowt", [KC, 128], F32)
        S_.dma("sync", sc.ap(), cT.ap(), r=[cT], w=[sc])
        S_.op("act", lambda e: e.activation(sc.ap(), sc.ap(), AF.Silu), r=[sc], w=[sc])
        S_.dma("sync", sm.ap(), bT_ada.ap(), r=[bT_ada], w=[sm])
        pm = banks[0]
        wv = w_ada.ap().rearrange("(kc p) n -> p kc n", p=128)
        for j in range(6 * KC):
            a = aw[j % 2]
            S_.dma("sync", a.ap(), wv[:, :, j * 128:(j + 1) * 128], r=[w_ada], w=[a])
            for kc in range(KC):
                S_.op("pe", lambda e, a=a, kc=kc, j=j: e.matmul(
                    pm.ap()[:, 2 * j:2 * j + 2], a.ap()[:, kc, :], sc.ap()[:, kc, :],
                    start=(kc == 0), stop=(kc == KC - 1)), r=[a, sc], w=[pm])
        S_.op("dve", lambda e: e.tensor_tensor(
            modT.ap(), pm.ap()[:, 0:12 * KC].rearrange("p (j m) -> p j m", m=2),
            sm.ap().unsqueeze(2).broadcast_to([128, 6 * KC, 2]), ALU.add), r=[pm, sm], w=[modT])
        S_.dma("sync", sm.ap()[:, 0:KC], g1T.ap(), r=[g1T], w=[sm])
        S_.dma("sync", sm.ap()[:, KC:2 * KC], g2T.ap(), r=[g2T], w=[sm])
        S_.op("dve", lambda e: e.tensor_scalar(s1.ap(), modT.ap()[:, KC:2 * KC, :], 1.0, None, ALU.add),
              r=[modT], w=[s1])
        S_.op("dve", lambda e: e.tensor_tensor(
            s1.ap(), s1.ap(), sm.ap()[:, 0:KC].unsqueeze(2).broadcast_to([128, KC, 2]), ALU.mult),
            r=[s1, sm], w=[s1])
        S_.op("dve", lambda e: e.tensor_scalar(s2.ap(), modT.ap()[:, 4 * KC:5 * KC, 0], 1.0, None, ALU.add),
              r=[modT], w=[s2])
        S_.op("dve", lambda e: e.tensor_tensor(s2.ap(), s2.ap(), sm.ap()[:, KC:2 * KC], ALU.mult),
              r=[s2, sm], w=[s2])
        row_srcs = [(modT, modT.ap()[:, 2 * KC:3 * KC, 0]), (modT, modT.ap()[:, 5 * KC:6 * KC, 0]),
                    (s2, s2.ap()), (modT, modT.ap()[:, 3 * KC:4 * KC, 0])]
        for i, (sb_src, src_ap) in enumerate(row_srcs):
            S_.op("dve", lambda e, src_ap=src_ap: e.tensor_copy(sm.ap()[:, 2 * KC:3 * KC], src_ap),
                  r=[sb_src], w=[sm])
            S_.op("pe", lambda e: e.transpose(banks[1].ap()[0:KC, 0:128], sm.ap()[:, 2 * KC:3 * KC], ident.ap()),
                  r=[sm, ident], w=[banks[1]])
            S_.op("dve", lambda e: e.tensor_copy(rowt.ap(), banks[1].ap()[0:KC, 0:128]), r=[banks[1]], w=[rowt])
            S_.dma("pool", modrow_s.ap()[i:i + 1, :].rearrange("o (kc p) -> (o kc) p", p=128), rowt.ap(),
                   r=[rowt], w=[modrow_s])
    barrier()

    groups = []
    for g in range(NG_OWN):
        groups.append((g * G, G, "own"))
    for g in range(NG_OWN):
        groups.append((HALF + g * G, G, "other0" if g == 0 else "other"))
    groups.append((S, CTX, "ctx"))
    QC = ATTN_W // 512
    KCH = KVW // 512
    UCH = POOL_W // 512
    assert ATTN_W % 512 == 0 and KVW % 512 == 0 and POOL_W % 512 == 0

    with ExitStack() as es:
        def sb(name, shape, dt):
            return Buf(name, es.enter_context(nc.sbuf_tensor(name, list(shape), dt)).ap())
        hT = sb("hT", [128, KC, G], BF16)
        xt = [sb("xt0", [128, D], F32), sb("xt1", [128, D], F32)]
        ws = [sb("ws0", [128, KS, 512], F32), sb("ws1", [128, KS, 512], F32)]
        wb = [sb("wb0", [128, KS, 512], BF16), sb("wb1", [128, KS, 512], BF16)]
        ss = sb("ssA", [128, 8], F32); rs = sb("rsA", [128, 8], F32)
        tmpA = sb("tmpA", [128, 512], F32); tmpB = sb("tmpB", [128, 512], F32)
        tmpC = sb("tmpC", [128, 256], F32); tmpD = sb("tmpD", [128, 256], F32)
        qr = sb("qr", [128, 512], BF16); qTg = sb("qTg", [128, 4, G], BF16)
        vg = sb("vg", [128, GT_, 512], BF16); ug = sb("ug", [128, 4, G], F32)
        gqs = sb("gqs", [128, 128], F32); gks = sb("gks", [128, 128], F32)
        cst = sb("cst", [128, GT_, 128], F32)
        S_.dma("sync", gqs.ap(), gq.ap(), r=[gq], w=[gqs])
        S_.dma("sync", gks.ap(), gk.ap(), r=[gk], w=[gks])
        win_v = w_in.ap().rearrange("(kc p) n -> p kc n", p=128)
        slab_i = [0]
        xi = [0]

        for (t0, ng, kind) in groups:
            ntt = ng // 128
            mcol = 1 if kind == "ctx" else 0
            S_.dma("sync", cst.ap()[:, 0:ntt, :], cs.ap()[t0:t0 + ng, :].rearrange("(t p) c -> p t c", p=128),
                   r=[cs], w=[cst])
            for tt in range(ntt):
                x_ = xt[xi[0] % 2]; xi[0] += 1
                S_.dma("sync", x_.ap(), xin.ap()[t0 + tt * 128:t0 + (tt + 1) * 128, :], r=[xin], w=[x_])
                S_.op("act", lambda e, x_=x_: e.activation(tmpA.ap()[:, 0:512], x_.ap()[:, 0:512], AF.Square),
                      r=[x_], w=[tmpA]) if False else None
                for ch in range(D // 512):
                    S_.op("act", lambda e, x_=x_, ch=ch: e.activation(
                        tmpA.ap(), x_.ap()[:, ch * 512:(ch + 1) * 512], AF.Square,
                        accum_out=ss.ap()[:, ch:ch + 1]), r=[x_], w=[tmpA, ss])
                S_.op("dve", lambda e: e.tensor_reduce(ss.ap()[:, 7:8] if D // 512 < 8 else rs.ap()[:, 1:2],
                                                       ss.ap()[:, 0:D // 512], AX.X, ALU.add), r=[ss], w=[rs, ss])
                src = ss if D // 512 < 8 else rs
                sc_col = 7 if D // 512 < 8 else 1
                S_.op("dve", lambda e, src=src, sc_col=sc_col: e.tensor_scalar(
                    rs.ap()[:, 0:1], src.ap()[:, sc_col:sc_col + 1], 1.0 / D, EPS, ALU.mult, ALU.add), r=[src], w=[rs])
                S_.op("act", lambda e: e.activation(rs.ap()[:, 0:1], rs.ap()[:, 0:1], AF.Sqrt), r=[rs], w=[rs])
                S_.op("dve", lambda e: e.reciprocal(rs.ap()[:, 0:1], rs.ap()[:, 0:1]), r=[rs], w=[rs])
                S_.op("act", lambda e, x_=x_: e.activation(x_.ap(), x_.ap(), AF.Copy, scale=rs.ap()[:, 0:1]),
                      r=[x_, rs], w=[x_])
                for kc in range(KC):
                    pb = banks[kc % 2]
                    S_.op("pe", lambda e, x_=x_, kc=kc, pb=pb: e.transpose(
                        pb.ap()[:, 0:128], x_.ap()[:, kc * 128:(kc + 1) * 128], ident.ap()), r=[x_, ident], w=[pb])
                    S_.op("act", lambda e, kc=kc, pb=pb, tt=tt, mcol=mcol: e.activation(
                        hT.ap()[:, kc, tt * 128:(tt + 1) * 128], pb.ap()[:, 0:128], AF.Identity,
                        bias=modT.ap()[:, kc, mcol:mcol + 1], scale=s1.ap()[:, kc, mcol:mcol + 1]),
                        r=[pb, modT, s1], w=[hT])

            def proj_chunk(col0, mode, hidx):
                for sl in range(KC // KS):
                    w_s = ws[slab_i[0] % 2]; w_b = wb[slab_i[0] % 2]; slab_i[0] += 1
                    S_.dma("sync", w_s.ap(), win_v[:, sl * KS:(sl + 1) * KS, col0:col0 + 512], r=[w_in], w=[w_s])
                    S_.op("dve", lambda e, w_s=w_s, w_b=w_b: e.tensor_copy(w_b.ap(), w_s.ap()), r=[w_s], w=[w_b])
                    for k8 in range(KS):
                        kc = sl * KS + k8
                        for j in range(4 if mode == "u" else ntt):
                            pb = banks[2 + j]
                            if mode == "u":
                                S_.op("pe", lambda e, pb=pb, j=j, k8=k8, kc=kc, w_b=w_b: e.matmul(
                                    pb.ap()[:, 0:ng], w_b.ap()[:, k8, j * 128:(j + 1) * 128], hT.ap()[:, kc, 0:ng],
                                    start=(kc == 0), stop=(kc == KC - 1)), r=[w_b, hT], w=[pb])
                            else:
                                S_.op("pe", lambda e, pb=pb, j=j, k8=k8, kc=kc, w_b=w_b: e.matmul(
                                    pb.ap(), hT.ap()[:, kc, j * 128:(j + 1) * 128], w_b.ap()[:, k8, :],
                                    start=(kc == 0), stop=(kc == KC - 1)), r=[w_b, hT], w=[pb])
                if mode == "u":
                    for j in range(4):
                        S_.op("act", lambda e, j=j: e.activation(ug.ap()[:, j, 0:ng], banks[2 + j].ap()[:, 0:ng], AF.Copy),
                              r=[banks[2 + j]], w=[ug])
                    n_st = ng if kind == "own" else 16
                    S_.dma("pool", uT_s.ap()[hidx * 4:(hidx + 1) * 4, :, t0 if kind == "own" else HALF:(t0 if kind == "own" else HALF) + n_st]
                           .rearrange("c p t -> p c t"), ug.ap()[:, :, 0:n_st], r=[ug], w=[uT_s])
                    return
                if mode == "v":
                    for j in range(ntt):
                        S_.op("act", lambda e, j=j: e.activation(vg.ap()[:, j, :], banks[2 + j].ap(), AF.Copy),
                              r=[banks[2 + j]], w=[vg])
                    S_.dma("pool", v_s.ap()[t0:t0 + ng, hidx * 512:(hidx + 1) * 512].rearrange("(t p) c -> p t c", p=128),
                           vg.ap()[:, 0:ntt, :], r=[vg], w=[v_s])
                    return
                gg = gqs if mode == "q" else gks
                for j in range(ntt):
                    pb = banks[2 + j]
                    S_.op("act", lambda e, pb=pb: e.activation(tmpA.ap(), pb.ap(), AF.Square), r=[pb], w=[tmpA])
                    S_.op("dve", lambda e: e.tensor_reduce(ss.ap()[:, 0:4], tmpA.ap().rearrange("p (h d) -> p h d", d=128),
                                                           AX.X, ALU.add), r=[tmpA], w=[ss])
                    rstd_from_ss(ss, rs, 4, 1.0 / 128)
                    S_.op("dve", lambda e, pb=pb: e.tensor_tensor(
                        tmpB.ap().rearrange("p (h d) -> p h d", d=128), pb.ap().rearrange("p (h d) -> p h d", d=128),
                        rs.ap()[:, 0:4].unsqueeze(2).broadcast_to([128, 4, 128]), ALU.mult), r=[pb, rs], w=[tmpB])
                    S_.op("dve", lambda e, gg=gg: e.tensor_tensor(
                        tmpB.ap().rearrange("p (h d) -> p h d", d=128), tmpB.ap().rearrange("p (h d) -> p h d", d=128),
                        gg.ap().unsqueeze(1).broadcast_to([128, 4, 128]), ALU.mult), r=[tmpB, gg], w=[tmpB])
                    tb = tmpB.ap().rearrange("p (h d) -> p h d", d=128)
                    x1v, x2v = tb[:, :, 0:64], tb[:, :, 64:128]
                    cosv = cst.ap()[:, j, 0:64].unsqueeze(1).broadcast_to([128, 4, 64])
                    sinv = cst.ap()[:, j, 64:128].unsqueeze(1).broadcast_to([128, 4, 64])
                    cv = tmpC.ap().rearrange("p (h d) -> p h d", d=64)
                    dv = tmpD.ap().rearrange("p (h d) -> p h d", d=64)
                    qv = qr.ap().rearrange("p (h d) -> p h d", d=128)
                    S_.op("dve", lambda e: e.tensor_tensor(cv, x1v, cosv, ALU.mult), r=[tmpB, cst], w=[tmpC])
                    S_.op("dve", lambda e: e.tensor_tensor(dv, x2v, sinv, ALU.mult), r=[tmpB, cst], w=[tmpD])
                    S_.op("dve", lambda e: e.tensor_tensor(qv[:, :, 0:64], cv, dv, ALU.subtract), r=[tmpC, tmpD], w=[qr])
                    S_.op("dve", lambda e: e.tensor_tensor(cv, x1v, sinv, ALU.mult), r=[tmpB, cst], w=[tmpC])
                    S_.op("dve", lambda e: e.tensor_tensor(dv, x2v, cosv, ALU.mult), r=[tmpB, cst], w=[tmpD])
                    S_.op("dve", lambda e: e.tensor_tensor(qv[:, :, 64:128], cv, dv, ALU.add), r=[tmpC, tmpD], w=[qr])
                    tp = banks[6 + (j % 2)]
                    tpv = tp.ap().bitcast(BF16)
                    for hd in range(4):
                        S_.op("pe", lambda e, hd=hd, tpv=tpv, tp=tp: e.transpose(
                            tpv[:, hd * 128:(hd + 1) * 128], qr.ap()[:, hd * 128:(hd + 1) * 128], identb.ap()),
                            r=[qr, identb], w=[tp])
                    S_.op("act", lambda e, tpv=tpv, tp=tp, j=j: e.activation(
                        qTg.ap()[:, :, j * 128:(j + 1) * 128], tpv[:, 0:512].rearrange("p (h t) -> p h t", t=128), AF.Copy),
                        r=[tp], w=[qTg])
                if mode == "q":
                    S_.dma("pool", qT_s.ap()[hidx * 4:(hidx + 1) * 4, :, t0:t0 + ng].rearrange("h p t -> p h t"),
                           qTg.ap()[:, :, 0:ng], r=[qTg], w=[qT_s])
                else:
                    S_.dma("pool", kT_s.ap()[hidx * 4:(hidx + 1) * 4, :, t0:t0 + ng].rearrange("h p t -> p h t"),
                           qTg.ap()[:, :, 0:ng], r=[qTg], w=[kT_s])

            if kind == "own":
                for qc in range(QC):
                    proj_chunk(qc * 512, "q", qc)
            for kc_ in range(KCH):
                proj_chunk(ATTN_W + kc_ * 512, "k", kc_)
            for vc in range(KCH):
                proj_chunk(ATTN_W + KVW + vc * 512, "v", vc)
            if kind in ("own", "other0"):
                for uc in range(UCH):
                    proj_chunk(ATTN_W + 2 * KVW + uc * 512, "u", uc)
    barrier()

    ATT_SCALE = 128.0 ** -0.5
    with ExitStack() as es:
        def sb(name, shape, dt):
            return Buf(name, es.enter_context(nc.sbuf_tensor(name, list(shape), dt)).ap())
        KT = sb("KT", [128, NT], BF16); Vh = sb("Vh", [128, TT, 130], BF16)
        QT = [sb("QT0", [128, G], BF16), sb("QT1", [128, G], BF16)]
        PT = [sb("PT0", [128, G], BF16), sb("PT1", [128, G], BF16)]
        ob = [sb("ob0", [128, 128], BF16), sb("ob1", [128, 128], BF16)]
        rinv = [sb("rinv0", [128, 1], F32), sb("rinv1", [128, 1], F32)]
        oT = [sb("oT0", [128, G], BF16), sb("oT1", [128, G], BF16)]
        S_.op("dve", lambda e: e.memset(Vh.ap()[:, :, 128:130], 1.0), w=[Vh])
        qi = 0
        for h in range(NKV):
            S_.dma("sync", KT.ap(), kT_s.ap()[h], r=[kT_s], w=[KT])
            S_.dma("sync", Vh.ap()[:, :, 0:128], v_s.ap()[:, h * 128:(h + 1) * 128].rearrange("(t p) c -> p t c", p=128),
                   r=[v_s], w=[Vh])
            for gi in range(3):
                n = h * 3 + gi
                for qg in range(NG_OWN):
                    Q = QT[qi % 2]; o_T = oT[qi % 2]; qi += 1
                    S_.dma("sync", Q.ap(), qT_s.ap()[n, :, qg * G:(qg + 1) * G], r=[qT_s], w=[Q])
                    for kt in range(TT):
                        sbk = banks[kt % 2]; P = PT[kt % 2]
                        S_.op("pe", lambda e, sbk=sbk, kt=kt, Q=Q: e.matmul(
                            sbk.ap()[:, 0:G], KT.ap()[:, kt * 128:(kt + 1) * 128], Q.ap(), start=True, stop=True),
                            r=[KT, Q], w=[sbk])
                        S_.op("act", lambda e, sbk=sbk, P=P: e.activation(P.ap(), sbk.ap()[:, 0:G], AF.Exp, scale=ATT_SCALE),
                              r=[sbk], w=[P])
                        for qs in range(GT_):
                            S_.op("pe", lambda e, qs=qs, kt=kt, P=P: e.matmul(
                                banks[2 + qs].ap()[:, 0:129], P.ap()[:, qs * 128:(qs + 1) * 128], Vh.ap()[:, kt, 0:129],
                                start=(kt == 0), stop=(kt == TT - 1)), r=[P, Vh], w=[banks[2 + qs]])
                    tpv = banks[6].ap().bitcast(BF16)
                    for qs in range(GT_):
                        ri = rinv[qs % 2]; o_ = ob[qs % 2]
                        S_.op("dve", lambda e, ri=ri, qs=qs: e.reciprocal(ri.ap(), banks[2 + qs].ap()[:, 128:129]),
                              r=[banks[2 + qs]], w=[ri])
                        S_.op("act", lambda e, ri=ri, o_=o_, qs=qs: e.activation(
                            o_.ap(), banks[2 + qs].ap()[:, 0:128], AF.Copy, scale=ri.ap()), r=[banks[2 + qs], ri], w=[o_])
                        S_.op("pe", lambda e, o_=o_, qs=qs: e.transpose(tpv[:, qs * 128:(qs + 1) * 128], o_.ap(), identb.ap()),
                              r=[o_, identb], w=[banks[6]])
                    S_.op("dve", lambda e, o_T=o_T: e.tensor_copy(o_T.ap(), tpv[:, 0:G]), r=[banks[6]], w=[o_T])
                    S_.dma("pool", mixT_s.ap()[n, :, qg * G:(qg + 1) * G], o_T.ap(), r=[o_T], w=[mixT_s])
    barrier()

    with ExitStack() as es:
        def sb(name, shape, dt):
            return Buf(name, es.enter_context(nc.sbuf_tensor(name, list(shape), dt)).ap())
        L = HALF + 16
        Up = sb("Up", [128, L], F32); PA = sb("PA", [128, L], F32); PB = sb("PB", [128, L], F32)
        icn = sb("icn", [128, HALF], F32); dT = sb("dT", [128, PGC, HALF], BF16)
        wpf = sb("wpf", [128, PGC, PG], F32); wpb = sb("wpb", [128, PGC, PG], BF16)
        hmt = sb("hmt", [128, 16], F32); psc = sb("psc", [128, PCH], F32)
        yb = [sb("yb0", [128, G], BF16), sb("yb1", [128, G], BF16)]
        S_.dma("sync", hmt.ap(), bcast_rows(hmask.ap().tensor, 0, 16), r=[hmask], w=[hmt])
        S_.dma("sync", psc.ap(), pscT.ap(), r=[pscT], w=[psc])
        yi = 0
        for gi, w in enumerate((2, 4, 8, 16)):
            S_.dma("sync", icn.ap(), bcast_rows(invcnt.ap().tensor, gi * HALF, HALF), r=[invcnt], w=[icn])
            S_.dma("sync", wpf.ap(), w_pool.ap()[gi * PG:(gi + 1) * PG, :].rearrange("(cc p) e -> p cc e", p=128),
                   r=[w_pool], w=[wpf])
            S_.op("dve", lambda e: e.tensor_copy(wpb.ap(), wpf.ap()), r=[wpf], w=[wpb])
            for cc in range(PGC):
                ch = gi * PGC + cc
                S_.dma("sync", Up.ap()[:, 0:8], uT_s.ap()[ch, :, HALF:HALF + 8], r=[uT_s], w=[Up])
                S_.dma("sync", Up.ap()[:, 8:8 + HALF], uT_s.ap()[ch, :, 0:HALF], r=[uT_s], w=[Up])
                S_.dma("sync", Up.ap()[:, 8 + HALF:L], uT_s.ap()[ch, :, HALF + 8:HALF + 16], r=[uT_s], w=[Up])
                S_.op("dve", lambda e: e.tensor_tensor(Up.ap()[:, 0:8], Up.ap()[:, 0:8], hmt.ap()[:, 0:8], ALU.mult),
                      r=[Up, hmt], w=[Up])
                S_.op("dve", lambda e: e.tensor_tensor(Up.ap()[:, 8 + HALF:L], Up.ap()[:, 8 + HALF:L], hmt.ap()[:, 8:16], ALU.mult),
                      r=[Up, hmt], w=[Up])
                S_.op("dve", lambda e: e.tensor_tensor(PA.ap()[:, 1:L], Up.ap()[:, 0:L - 1], Up.ap()[:, 1:L], ALU.add),
                      r=[Up], w=[PA])
                fin = PA
                if w >= 4:
                    S_.op("dve", lambda e: e.tensor_tensor(PB.ap()[:, 2:L - 1], PA.ap()[:, 1:L - 2], PA.ap()[:, 3:L], ALU.add),
                          r=[PA], w=[PB])
                    fin = PB
                if w >= 8:
                    S_.op("dve", lambda e: e.tensor_tensor(PA.ap()[:, 4:L - 3], PB.ap()[:, 2:L - 5], PB.ap()[:, 6:L - 1], ALU.add),
                          r=[PB], w=[PA])
                    fin = PA
                if w >= 16:
                    S_.op("dve", lambda e: e.tensor_tensor(PB.ap()[:, 8:L - 7], PA.ap()[:, 4:L - 11], PA.ap()[:, 12:L - 3], ALU.add),
                          r=[PA], w=[PB])
                    fin = PB
                oth = PA if fin is PB else PB
                S_.op("dve", lambda e, fin=fin, oth=oth: e.tensor_tensor(
                    oth.ap()[:, 8:8 + HALF], fin.ap()[:, 8:8 + HALF], icn.ap(), ALU.mult), r=[fin, icn], w=[oth])
                S_.op("dve", lambda e, oth=oth, cc=cc: e.tensor_tensor(
                    dT.ap()[:, cc, :], oth.ap()[:, 8:8 + HALF], Up.ap()[:, 8:8 + HALF], ALU.subtract), r=[oth, Up], w=[dT])
            for ec in range(PGC):
                for tg in range(NG_OWN):
                    pb = banks[yi % 2]; y_ = yb[yi % 2]; yi += 1
                    for cc in range(PGC):
                        S_.op("pe", lambda e, pb=pb, cc=cc, ec=ec, tg=tg: e.matmul(
                            pb.ap()[:, 0:G], wpb.ap()[:, cc, ec * 128:(ec + 1) * 128], dT.ap()[:, cc, tg * G:(tg + 1) * G],
                            start=(cc == 0), stop=(cc == PGC - 1)), r=[wpb, dT], w=[pb])
                    co = gi * PGC + ec
                    S_.op("act", lambda e, pb=pb, y_=y_, co=co: e.activation(
                        y_.ap(), pb.ap()[:, 0:G], AF.Copy, scale=psc.ap()[:, co:co + 1]), r=[pb, psc], w=[y_])
                    S_.dma("pool", mixT_s.ap()[NH + co, :, tg * G:(tg + 1) * G], y_.ap(), r=[y_], w=[mixT_s])
    barrier()

    with ExitStack() as es:
        def sb(name, shape, dt):
            return Buf(name, es.enter_context(nc.sbuf_tensor(name, list(shape), dt)).ap())
        mT = sb("mT", [128, KC, G], BF16)
        h2f = sb("h2f", [128, KC, 128], F32)
        xt = [sb("xe0", [128, D], F32)]
        s2row = sb("s2row", [128, D], F32); t2row = sb("t2row", [128, D], F32)
        hk = sb("hk", [128, D], BF16)
        i8 = sb("i8", [128, 8], U32); e4 = sb("e4", [128, 4], F32)
        msk_all = sb("msk_all", [128, NTL, E], F32); rank_all = sb("rank_all", [128, NTL, E], F32)
        i8f_all = sb("i8f_all", [128, NTL, 4], F32)
        ones128 = sb("ones128", [128, 128], F32); ust = sb("ust", [128, 128], F32)
        S_.op("dve", lambda e: e.memset(ones128.ap(), 1.0), w=[ones128])
        S_.dma("sync", ust.ap(), ustrict.ap(), r=[ustrict], w=[ust])
        S_.dma("sync", s2row.ap(), bcast_rows(modrow_s.ap().tensor, 2 * D, D), r=[modrow_s], w=[s2row])
        S_.dma("sync", t2row.ap(), bcast_rows(modrow_s.ap().tensor, 3 * D, D), r=[modrow_s], w=[t2row])
        ws = [sb("wse0", [128, KS, 512], F32), sb("wse1", [128, KS, 512], F32)]
        wb = [sb("wbe0", [128, KS, 512], BF16), sb("wbe1", [128, KS, 512], BF16)]
        g2b = sb("g2b", [128, D], F32)
        xc = [sb("xc0", [128, 512], F32), sb("xc1", [128, 512], F32)]
        wr = sb("wr", [128, KC, E], F32); brb = sb("brb", [128, E], F32)
        ss = sb("ssE", [128, 8], F32); rs = sb("rsE", [128, 8], F32); tmpA = sb("tmpE", [128, 512], F32)
        lg = sb("lg", [128, E], F32); m8 = sb("m8", [128, 8], F32); msk = sb("msk", [128, E], F32)
        nm = sb("nm", [128, 1], F32)
        S_.dma("sync", g2b.ap(), bcast_rows(modrow_s.ap().tensor, 0, D), r=[modrow_s], w=[g2b])
        S_.dma("sync", wr.ap(), w_router.ap().rearrange("p (kc e) -> p kc e", e=E), r=[w_router], w=[wr])
        S_.dma("sync", brb.ap(), b_router.ap(), r=[b_router], w=[brb])
        wout_v = w_out.ap().rearrange("(kc p) n -> p kc n", p=128)
        si = 0; xi_ = 0; ci = 0
        for g in range(NG_OWN):
            t0 = g * G
            S_.dma("sync", mT.ap(), mixT_s.ap()[:, :, t0:t0 + G].rearrange("c p t -> p c t"), r=[mixT_s], w=[mT])
            for dc in range(D // 512):
                for sl in range(KC // KS):
                    w_s = ws[si % 2]; w_b = wb[si % 2]; si += 1
                    S_.dma("sync", w_s.ap(), wout_v[:, sl * KS:(sl + 1) * KS, dc * 512:(dc + 1) * 512], r=[w_out], w=[w_s])
                    S_.op("dve", lambda e, w_s=w_s, w_b=w_b: e.tensor_copy(w_b.ap(), w_s.ap()), r=[w_s], w=[w_b])
                    for k8 in range(KS):
                        kc = sl * KS + k8
                        for j in range(GT_):
                            S_.op("pe", lambda e, j=j, k8=k8, kc=kc, w_b=w_b: e.matmul(
                                banks[2 + j].ap(), mT.ap()[:, kc, j * 128:(j + 1) * 128], w_b.ap()[:, k8, :],
                                start=(kc == 0), stop=(kc == KC - 1)), r=[w_b, mT], w=[banks[2 + j]])
                for j in range(GT_):
                    x_c = xc[ci % 2]; ci += 1
                    rows = slice(t0 + j * 128, t0 + (j + 1) * 128)
                    S_.dma("sync", x_c.ap(), xin.ap()[rows, dc * 512:(dc + 1) * 512], r=[xin], w=[x_c])
                    S_.op("dve", lambda e, j=j, dc=dc: e.tensor_tensor(
                        tmpA.ap(), banks[2 + j].ap(), g2b.ap()[:, dc * 512:(dc + 1) * 512], ALU.mult),
                        r=[banks[2 + j], g2b], w=[tmpA])
                    S_.op("dve", lambda e, x_c=x_c: e.tensor_tensor(x_c.ap(), x_c.ap(), tmpA.ap(), ALU.add),
                          r=[x_c, tmpA], w=[x_c])
                    S_.dma("pool", x1_s.ap()[rows, dc * 512:(dc + 1) * 512], x_c.ap(), r=[x_c], w=[x1_s])
            for tt in range(GT_):
                ti = g * GT_ + tt
                x_ = xt[0]
                rows = slice(t0 + tt * 128, t0 + (tt + 1) * 128)
                S_.dma("sync", x_.ap(), x1_s.ap()[rows, :], r=[x1_s], w=[x_])
                for ch in range(D // 512):
                    S_.op("act", lambda e, x_=x_, ch=ch: e.activation(
                        tmpA.ap(), x_.ap()[:, ch * 512:(ch + 1) * 512], AF.Square,
                        accum_out=ss.ap()[:, ch:ch + 1]), r=[x_], w=[tmpA, ss])
                S_.op("dve", lambda e: e.tensor_reduce(rs.ap()[:, 1:2], ss.ap()[:, 0:D // 512], AX.X, ALU.add), r=[ss], w=[rs])
                S_.op("dve", lambda e: e.tensor_scalar(rs.ap()[:, 0:1], rs.ap()[:, 1:2], 1.0 / D, EPS, ALU.mult, ALU.add),
                      r=[rs], w=[rs])
                S_.op("act", lambda e: e.activation(rs.ap()[:, 0:1], rs.ap()[:, 0:1], AF.Sqrt), r=[rs], w=[rs])
                S_.op("dve", lambda e: e.reciprocal(rs.ap()[:, 0:1], rs.ap()[:, 0:1]), r=[rs], w=[rs])
                S_.op("act", lambda e, x_=x_: e.activation(x_.ap(), x_.ap(), AF.Copy, scale=rs.ap()[:, 0:1]),
                      r=[x_, rs], w=[x_])
                for ch in range(D // 512):
                    cs_ = slice(ch * 512, (ch + 1) * 512)
                    S_.op("dve", lambda e, cs_=cs_: e.tensor_tensor(tmpA.ap(), x_.ap()[:, cs_], s2row.ap()[:, cs_], ALU.mult),
                          r=[x_, s2row], w=[tmpA])
                    S_.op("dve", lambda e, cs_=cs_: e.tensor_tensor(hk.ap()[:, cs_], tmpA.ap(), t2row.ap()[:, cs_], ALU.add),
                          r=[tmpA, t2row], w=[hk])
                S_.dma("pool", h2tok_s.ap()[rows, :], hk.ap(), r=[hk], w=[h2tok_s])
                for kc in range(KC):
                    pb = banks[kc % 2]
                    S_.op("pe", lambda e, x_=x_, kc=kc, pb=pb: e.transpose(
                        pb.ap()[:, 0:128], x_.ap()[:, kc * 128:(kc + 1) * 128], ident.ap()), r=[x_, ident], w=[pb])
                    S_.op("act", lambda e, kc=kc, pb=pb: e.activation(
                        h2f.ap()[:, kc, :], pb.ap()[:, 0:128], AF.Identity,
                        bias=modT.ap()[:, 3 * KC + kc, 0:1], scale=s2.ap()[:, kc:kc + 1]), r=[pb, modT, s2], w=[h2f])
                for kc in range(KC):
                    S_.op("pe", lambda e, kc=kc: e.matmul(banks[6].ap()[:, 0:E], h2f.ap()[:, kc, :], wr.ap()[:, kc, :],
                                                          start=(kc == 0), stop=(kc == KC - 1)), r=[h2f, wr], w=[banks[6]])
                S_.op("dve", lambda e: e.tensor_tensor(lg.ap(), banks[6].ap()[:, 0:E], brb.ap(), ALU.add), r=[banks[6], brb], w=[lg])
                S_.op("dve", lambda e: e.max(m8.ap(), lg.ap()), r=[lg], w=[m8])
                S_.op("dve", lambda e: e.max_index(i8.ap(), m8.ap(), lg.ap()), r=[lg, m8], w=[i8])
                S_.op("dve", lambda e, ti=ti: e.tensor_scalar(msk_all.ap()[:, ti, :], lg.ap(), m8.ap()[:, TOPK - 1:TOPK], None, ALU.is_ge),
                      r=[lg, m8], w=[msk_all])
                S_.op("dve", lambda e, ti=ti: e.tensor_copy(i8f_all.ap()[:, ti, :], i8.ap()[:, 0:4]), r=[i8], w=[i8f_all])
                S_.op("dve", lambda e: e.tensor_scalar(nm.ap(), m8.ap()[:, 0:1], -1.0, None, ALU.mult), r=[m8], w=[nm])
                S_.op("act", lambda e: e.activation(e4.ap(), m8.ap()[:, 0:4], AF.Exp, bias=nm.ap()), r=[m8, nm], w=[e4])
                S_.op("dve", lambda e: e.tensor_reduce(nm.ap(), e4.ap(), AX.X, ALU.add), r=[e4], w=[nm])
                S_.op("dve", lambda e: e.reciprocal(nm.ap(), nm.ap()), r=[nm], w=[nm])
                S_.op("dve", lambda e, ti=ti: e.tensor_scalar(gk_all.ap()[:, ti, :], e4.ap(), nm.ap(), None, ALU.mult),
                      r=[e4, nm], w=[gk_all])
                S_.op("pe", lambda e, ti=ti: e.matmul(banks[7].ap()[:, 0:E], ust.ap(), msk_all.ap()[:, ti, :],
                                                      start=True, stop=(ti == 0)), r=[ust, msk_all], w=[banks[7]])
                for tp in range(ti):
                    S_.op("pe", lambda e, tp=tp, ti=ti: e.matmul(banks[7].ap()[:, 0:E], ones128.ap(), msk_all.ap()[:, tp, :],
                                                                 start=False, stop=(tp == ti - 1)), r=[ones128, msk_all], w=[banks[7]])
                S_.op("dve", lambda e, ti=ti: e.tensor_copy(rank_all.ap()[:, ti, :], banks[7].ap()[:, 0:E]), r=[banks[7]], w=[rank_all])

        cnt = sb("cnt", [128, E], F32); nbt = sb("nbt", [128, E], F32); tq = sb("tq", [128, E], F32)
        cA = sb("cA", [128, E], F32); cB = sb("cB", [128, E], F32); startv = sb("startv", [128, E], F32)
        sfull = sb("sfull", [128, NTL, E], F32); oh = sb("oh", [128, NTL, E], F32); slot_f = sb("slot_f", [128, NTL, 4], F32)
        ie = sb("ie", [128, E], F32); thr = sb("thr", [128, NB], F32); cmpb = sb("cmpb", [128, NB, E], F32)
        ebf = sb("ebf", [128, NB], F32); ebm = sb("ebm", [128, NB], F32)
        ipf = sb("ipf", [128, FC], F32); ipd = sb("ipd", [128, DC * 2], F32); ipp = sb("ipp", [128, 1], F32)
        wtmp = sb("wtmp", [128, NB, max(FC, DC * 2)], F32)
        S_.dma("sync", ie.ap(), iota_e.ap(), r=[iota_e], w=[ie])
        S_.dma("sync", thr.ap(), blkthr.ap(), r=[blkthr], w=[thr])
        S_.dma("sync", ipf.ap(), iota_pf.ap(), r=[iota_pf], w=[ipf])
        S_.dma("sync", ipd.ap(), iota_pd.ap(), r=[iota_pd], w=[ipd])
        S_.dma("sync", ipp.ap(), iota_p.ap(), r=[iota_p], w=[ipp])
        for i in range(NTL):
            S_.op("pe", lambda e, i=i: e.matmul(banks[7].ap()[:, 0:E], ones128.ap(), msk_all.ap()[:, i, :],
                                                start=(i == 0), stop=(i == NTL - 1)), r=[ones128, msk_all], w=[banks[7]])
        S_.op("dve", lambda e: e.tensor_copy(cnt.ap(), banks[7].ap()[:, 0:E]), r=[banks[7]], w=[cnt])
        S_.op("dve", lambda e: e.tensor_scalar(nbt.ap(), cnt.ap(), 0.0, None, ALU.is_gt), r=[cnt], w=[nbt])
        for j in range(1, HALF // BS):
            S_.op("dve", lambda e, j=j: e.tensor_scalar(tq.ap(), cnt.ap(), float(j * BS), None, ALU.is_gt), r=[cnt], w=[tq])
            S_.op("dve", lambda e: e.tensor_tensor(nbt.ap(), nbt.ap(), tq.ap(), ALU.add), r=[nbt, tq], w=[nbt])
        S_.op("dve", lambda e: e.tensor_scalar(nbt.ap(), nbt.ap(), float(BS), None, ALU.mult), r=[nbt], w=[nbt])
        S_.op("dve", lambda e: e.tensor_copy(cA.ap(), nbt.ap()), r=[nbt], w=[cA])
        cur, nxt = cA, cB
        sh_ = 1
        while sh_ < E:
            S_.op("dve", lambda e, cur=cur, nxt=nxt, sh_=sh_: e.tensor_copy(nxt.ap()[:, 0:sh_], cur.ap()[:, 0:sh_]), r=[cur], w=[nxt])
            S_.op("dve", lambda e, cur=cur, nxt=nxt, sh_=sh_: e.tensor_tensor(
                nxt.ap()[:, sh_:E], cur.ap()[:, sh_:E], cur.ap()[:, 0:E - sh_], ALU.add), r=[cur], w=[nxt])
            cur, nxt = nxt, cur
            sh_ *= 2
        endp = cur
        S_.op("dve", lambda e: e.tensor_tensor(startv.ap(), endp.ap(), nbt.ap(), ALU.subtract), r=[endp, nbt], w=[startv])
        S_.op("dve", lambda e: e.tensor_tensor(sfull.ap(), rank_all.ap(), startv.ap().unsqueeze(1).broadcast_to([128, NTL, E]), ALU.add),
              r=[rank_all, startv], w=[sfull])
        for k in range(4):
            S_.op("dve", lambda e, k=k: e.tensor_tensor(
                oh.ap(), ie.ap().unsqueeze(1).broadcast_to([128, NTL, E]),
                i8f_all.ap()[:, :, k:k + 1].broadcast_to([128, NTL, E]), ALU.is_equal), r=[ie, i8f_all], w=[oh])
            S_.op("dve", lambda e: e.tensor_tensor(oh.ap(), oh.ap(), sfull.ap(), ALU.mult), r=[oh, sfull], w=[oh])
            S_.op("dve", lambda e, k=k: e.tensor_reduce(slot_f.ap()[:, :, k], oh.ap(), AX.X, ALU.add), r=[oh], w=[slot_f])
        S_.op("dve", lambda e: e.tensor_copy(slot_u.ap(), slot_f.ap()), r=[slot_f], w=[slot_u])
        S_.op("dve", lambda e: e.tensor_tensor(
            cmpb.ap(), endp.ap().unsqueeze(1).broadcast_to([128, NB, E]),
            thr.ap().unsqueeze(2).broadcast_to([128, NB, E]), ALU.is_le), r=[endp, thr], w=[cmpb])
        S_.op("dve", lambda e: e.tensor_reduce(ebf.ap(), cmpb.ap(), AX.X, ALU.add), r=[cmpb], w=[ebf])
        S_.op("dve", lambda e: e.tensor_scalar(ebf.ap(), ebf.ap(), float(E - 1), None, ALU.min), r=[ebf], w=[ebf])
        S_.op("dve", lambda e: e.tensor_scalar(ebm.ap(), ebf.ap(), float(FC * 128), None, ALU.mult), r=[ebf], w=[ebm])
        S_.op("dve", lambda e: e.tensor_tensor(
            wtmp.ap()[:, :, 0:FC], ebm.ap().unsqueeze(2).broadcast_to([128, NB, FC]),
            ipf.ap().unsqueeze(1).broadcast_to([128, NB, FC]), ALU.add), r=[ebm, ipf], w=[wtmp])
        S_.op("dve", lambda e: e.tensor_copy(widx_g.ap(), wtmp.ap()[:, :, 0:FC]), r=[wtmp], w=[widx_g])
        S_.op("dve", lambda e: e.tensor_scalar(ebm.ap(), ebf.ap(), float(DC * 2 * 128), None, ALU.mult), r=[ebf], w=[ebm])
        S_.op("dve", lambda e: e.tensor_tensor(
            wtmp.ap()[:, :, 0:DC * 2], ebm.ap().unsqueeze(2).broadcast_to([128, NB, DC * 2]),
            ipd.ap().unsqueeze(1).broadcast_to([128, NB, DC * 2]), ALU.add), r=[ebm, ipd], w=[wtmp])
        S_.op("dve", lambda e: e.tensor_copy(widx_d.ap(), wtmp.ap()[:, :, 0:DC * 2]), r=[wtmp], w=[widx_d])
        S_.op("dve", lambda e: e.tensor_scalar(ebm.ap(), ebf.ap(), 128.0, ipp.ap(), ALU.mult, ALU.add), r=[ebf, ipp], w=[ebm])
        S_.op("dve", lambda e: e.tensor_copy(bidx.ap(), ebm.ap()), r=[ebm], w=[bidx])
        S_.op("dve", lambda e: e.tensor_copy(bdidx.ap(), ebf.ap()), r=[ebf], w=[bdidx])
        for i in range(NTL):
            S_.dma("sync", hk.ap(), h2tok_s.ap()[i * 128:(i + 1) * 128, :], r=[h2tok_s], w=[hk])
            for k in range(4):
                S_.idma(xs_s.ap(), hk.ap(), slot_u.ap()[:, i, k:k + 1], scatter=True, r=[hk, slot_u], w=[xs_s])
    barrier()
    if _STOP == 'E':
        S_.finish()
        return nc

    LIM = 7.0
    with ExitStack() as es:
        def sb(name, shape, dt):
            return Buf(name, es.enter_context(nc.sbuf_tensor(name, list(shape), dt)).ap())
        h2T = sb("h2Tf", [128, KC, BS], BF16)
        xr = [sb("xr0", [128, D], BF16), sb("xr1", [128, D], BF16)]
        STW = max(KC * 128, H2 * 512)
        st = [sb("st0", [128, STW], F32), sb("st1", [128, STW], F32), sb("st2", [128, STW], F32)]
        wgb = [sb("wgb0", [128, KC, 128], BF16), sb("wgb1", [128, KC, 128], BF16)]
        wub = [sb("wub0", [128, KC, 128], BF16), sb("wub1", [128, KC, 128], BF16)]
        wdb = [sb("wdb0", [128, H2, 512], BF16), sb("wdb1", [128, H2, 512], BF16)]
        actT = sb("actT", [128, FC, BS], BF16)
        ta = sb("ta", [128, BS], F32); tb_ = sb("tb", [128, BS], F32); tsg = sb("tsg", [128, BS], F32)
        bgb = [sb("bgb0", [128, FC], F32), sb("bgb1", [128, FC], F32)]
        bub = [sb("bub0", [128, FC], F32), sb("bub1", [128, FC], F32)]
        bdb = [sb("bdb0", [128, D], F32), sb("bdb1", [128, D], F32)]
        yc = [sb("yc0", [128, 512], F32), sb("yc1", [128, 512], F32), sb("yc2", [128, 512], F32), sb("yc3", [128, 512], F32)]
        sti = 0; fci = 0; xri = 0; yci = 0
        for b in range(NB):
            bg_ = bgb[b % 2]; bu_ = bub[b % 2]; bd_ = bdb[b % 2]
            S_.idma(bg_.ap(), bgT.ap(), bidx.ap()[:, b:b + 1], scatter=False, r=[bgT, bidx], w=[bg_])
            S_.idma(bu_.ap(), buT.ap(), bidx.ap()[:, b:b + 1], scatter=False, r=[buT, bidx], w=[bu_])
            S_.idma(bd_.ap(), b_down.ap(), bdidx.ap()[:, b:b + 1], scatter=False, r=[b_down, bdidx], w=[bd_])
            for tt in range(BS // 128):
                x_ = xr[xri % 2]; xri += 1
                S_.dma("sync", x_.ap(), xs_s.ap()[b * BS + tt * 128:b * BS + (tt + 1) * 128, :], r=[xs_s], w=[x_])
                for k4 in range(KC // 4):
                    pb = banks[6 + (k4 % 2)]
                    pbv = pb.ap().bitcast(BF16)
                    for q_ in range(4):
                        kc = k4 * 4 + q_
                        S_.op("pe", lambda e, x_=x_, kc=kc, q_=q_, pbv=pbv: e.transpose(
                            pbv[:, q_ * 128:(q_ + 1) * 128], x_.ap()[:, kc * 128:(kc + 1) * 128], identb.ap()),
                            r=[x_, identb], w=[pb])
                    eng = "act" if k4 % 2 == 0 else "dve"
                    if eng == "act":
                        S_.op("act", lambda e, k4=k4, tt=tt, pbv=pbv: e.activation(
                            h2T.ap()[:, k4 * 4:(k4 + 1) * 4, tt * 128:(tt + 1) * 128],
                            pbv[:, 0:512].rearrange("p (c t) -> p c t", t=128), AF.Copy), r=[pb], w=[h2T])
                    else:
                        S_.op("dve", lambda e, k4=k4, tt=tt, pbv=pbv: e.tensor_copy(
                            h2T.ap()[:, k4 * 4:(k4 + 1) * 4, tt * 128:(tt + 1) * 128],
                            pbv[:, 0:512].rearrange("p (c t) -> p c t", t=128)), r=[pb], w=[h2T])
            for fc in range(FC):
                psg = banks[fci % 2]; psu = banks[2 + fci % 2]
                wg_ = wgb[fci % 2]; wu_ = wub[fci % 2]; fci += 1
                sg_ = st[sti % 3]; sti += 1
                S_.idma(sg_.ap()[:, 0:KC * 128], w_gate.ap(), widx_g.ap()[:, b, fc:fc + 1], scatter=False, r=[w_gate, widx_g], w=[sg_])
                S_.op("dve", lambda e, sg_=sg_, wg_=wg_: e.tensor_copy(
                    wg_.ap(), sg_.ap()[:, 0:KC * 128].rearrange("p (kc f) -> p kc f", f=128)), r=[sg_], w=[wg_])
                su_ = st[sti % 3]; sti += 1
                S_.idma(su_.ap()[:, 0:KC * 128], w_up.ap(), widx_g.ap()[:, b, fc:fc + 1], scatter=False, r=[w_up, widx_g], w=[su_])
                S_.op("act", lambda e, su_=su_, wu_=wu_: e.activation(
                    wu_.ap(), su_.ap()[:, 0:KC * 128].rearrange("p (kc f) -> p kc f", f=128), AF.Copy), r=[su_], w=[wu_])
                for kc in range(KC):
                    S_.op("pe", lambda e, kc=kc, psg=psg, wg_=wg_: e.matmul(
                        psg.ap()[:, 0:BS], wg_.ap()[:, kc, :], h2T.ap()[:, kc, :], start=(kc == 0), stop=(kc == KC - 1)),
                        r=[wg_, h2T], w=[psg])
                for kc in range(KC):
                    S_.op("pe", lambda e, kc=kc, psu=psu, wu_=wu_: e.matmul(
                        psu.ap()[:, 0:BS], wu_.ap()[:, kc, :], h2T.ap()[:, kc, :], start=(kc == 0), stop=(kc == KC - 1)),
                        r=[wu_, h2T], w=[psu])
                S_.op("dve", lambda e, psg=psg, fc=fc, bg_=bg_: e.tensor_scalar(
                    ta.ap(), psg.ap()[:, 0:BS], bg_.ap()[:, fc:fc + 1], LIM, ALU.add, ALU.min), r=[psg, bg_], w=[ta])
                S_.op("act", lambda e: e.activation(tsg.ap(), ta.ap(), AF.Sigmoid, scale=1.702), r=[ta], w=[tsg])
                S_.op("dve", lambda e, psu=psu, fc=fc, bu_=bu_: e.tensor_scalar(
                    tb_.ap(), psu.ap()[:, 0:BS], bu_.ap()[:, fc:fc + 1], LIM, ALU.add, ALU.min), r=[psu, bu_], w=[tb_])
                S_.op("dve", lambda e: e.tensor_scalar(tb_.ap(), tb_.ap(), -LIM, 1.0, ALU.max, ALU.add), r=[tb_], w=[tb_])
                S_.op("dve", lambda e: e.tensor_tensor(ta.ap(), ta.ap(), tsg.ap(), ALU.mult), r=[ta, tsg], w=[ta])
                S_.op("dve", lambda e, fc=fc: e.tensor_tensor(actT.ap()[:, fc, :], ta.ap(), tb_.ap(), ALU.mult),
                      r=[ta, tb_], w=[actT])
            for dc in range(DC):
                for hf in range(2):
                    sd_ = st[sti % 3]; sti += 1
                    S_.idma(sd_.ap()[:, 0:H2 * 512], w_down.ap(), widx_d.ap()[:, b, dc * 2 + hf:dc * 2 + hf + 1], scatter=False,
                            r=[w_down, widx_d], w=[sd_])
                    if hf == 0:
                        S_.op("act", lambda e, sd_=sd_: e.activation(
                            wdb[0].ap(), sd_.ap()[:, 0:H2 * 512].rearrange("p (fc d) -> p fc d", d=512), AF.Copy),
                            r=[sd_], w=[wdb[0]])
                    else:
                        S_.op("dve", lambda e, sd_=sd_: e.tensor_copy(
                            wdb[1].ap(), sd_.ap()[:, 0:H2 * 512].rearrange("p (fc d) -> p fc d", d=512)),
                            r=[sd_], w=[wdb[1]])
                for tt in range(BS // 128):
                    for fc in range(FC):
                        S_.op("pe", lambda e, tt=tt, fc=fc: e.matmul(
                            banks[4 + tt].ap(), actT.ap()[:, fc, tt * 128:(tt + 1) * 128], wdb[fc // H2].ap()[:, fc % H2, :],
                            start=(fc == 0), stop=(fc == FC - 1)), r=[actT, wdb[fc // H2]], w=[banks[4 + tt]])
                for tt in range(BS // 128):
                    y_ = yc[yci % 4]; yci += 1
                    S_.op("dve", lambda e, tt=tt, dc=dc, y_=y_, bd_=bd_: e.tensor_tensor(
                        y_.ap(), banks[4 + tt].ap(), bd_.ap()[:, dc * 512:(dc + 1) * 512], ALU.add),
                        r=[banks[4 + tt], bd_], w=[y_])
                    yp = ys_parts[(dc * 512) // YW]; c0 = (dc * 512) % YW
                    S_.dma("sync", yp.ap()[b * BS + tt * 128:b * BS + (tt + 1) * 128, c0:c0 + 512], y_.ap(),
                           r=[y_], w=[yp])
    barrier()

    with ExitStack() as es:
        def sb(name, shape, dt):
            return Buf(name, es.enter_context(nc.sbuf_tensor(name, list(shape), dt)).ap())
        x1t = sb("x1t", [128, D], F32); g5b = sb("g5b", [128, D], F32); fgb = sb("fgb", [128, D], F32)
        yk = [sb("yk0", [128, D], F32), sb("yk1", [128, D], F32)]
        accs = [sb("acc0", [128, D], F32), sb("acc1", [128, D], F32)]
        ss = sb("ssG", [128, 8], F32); rs = sb("rsG", [128, 8], F32); junk = sb("junkG", [128, 512], F32)
        S_.dma("sync", g5b.ap(), bcast_rows(modrow_s.ap().tensor, D, D), r=[modrow_s], w=[g5b])
        S_.dma("sync", fgb.ap(), bcast_rows(fg_row.ap().tensor, 0, D), r=[fg_row], w=[fgb])
        yi = 0
        for i in range(NTL):
            rows = slice(i * 128, (i + 1) * 128)
            acc = accs[i % 2]
            S_.dma("sync", x1t.ap(), x1_s.ap()[rows, :], r=[x1_s], w=[x1t])
            for k in range(4):
                y_ = yk[yi % 2]; yi += 1
                for yp_i, yp in enumerate(ys_parts):
                    S_.idma(y_.ap()[:, yp_i * YW:(yp_i + 1) * YW], yp.ap(), slot_u.ap()[:, i, k:k + 1], scatter=False,
                            r=[yp, slot_u], w=[y_])
                if k == 0:
                    S_.op("dve", lambda e, y_=y_, i=i, acc=acc: e.tensor_scalar(
                        acc.ap(), y_.ap(), gk_all.ap()[:, i, 0:1], None, ALU.mult), r=[y_, gk_all], w=[acc])
                else:
                    S_.op("dve", lambda e, y_=y_, i=i, k=k, acc=acc: e.scalar_tensor_tensor(
                        acc.ap(), y_.ap(), gk_all.ap()[:, i, k:k + 1], acc.ap(), ALU.mult, ALU.add), r=[y_, gk_all, acc], w=[acc])
            S_.op("dve", lambda e, acc=acc: e.tensor_tensor(acc.ap(), acc.ap(), g5b.ap(), ALU.mult), r=[acc, g5b], w=[acc])
            S_.op("dve", lambda e, acc=acc: e.tensor_tensor(acc.ap(), acc.ap(), x1t.ap(), ALU.add), r=[acc, x1t], w=[acc])
            for ch in range(D // 512):
                S_.op("act", lambda e, ch=ch, acc=acc: e.activation(
                    junk.ap(), acc.ap()[:, ch * 512:(ch + 1) * 512], AF.Square,
                    accum_out=ss.ap()[:, ch:ch + 1]), r=[acc], w=[junk, ss])
            S_.op("dve", lambda e: e.tensor_reduce(rs.ap()[:, 1:2], ss.ap()[:, 0:D // 512], AX.X, ALU.add), r=[ss], w=[rs])
            S_.op("dve", lambda e: e.tensor_scalar(rs.ap()[:, 0:1], rs.ap()[:, 1:2], 1.0 / D, EPS, ALU.mult, ALU.add),
                  r=[rs], w=[rs])
            S_.op("act", lambda e: e.activation(rs.ap()[:, 0:1], rs.ap()[:, 0:1], AF.Sqrt), r=[rs], w=[rs])
            S_.op("dve", lambda e: e.reciprocal(rs.ap()[:, 0:1], rs.ap()[:, 0:1]), r=[rs], w=[rs])
            S_.op("act", lambda e, acc=acc: e.activation(acc.ap(), acc.ap(), AF.Copy, scale=rs.ap()[:, 0:1]), r=[acc, rs], w=[acc])
            S_.op("dve", lambda e, acc=acc: e.tensor_tensor(acc.ap(), acc.ap(), fgb.ap(), ALU.mult), r=[acc, fgb], w=[acc])
            S_.dma("sync", out.ap()[rows, :], acc.ap(), r=[acc], w=[out], is_output=True)
    barrier()
    S_.finish()
    return nc


def token_order(cfg, s):
    S, HALF = cfg["S"], cfg["HALF"]
    own = np.arange(s * HALF, (s + 1) * HALF)
    if s == 0:
        before = np.arange(S - 8, S); after = np.arange(HALF, HALF + 8); rest = np.arange(HALF + 8, S - 8)
        hm = np.array([0.0] * 8 + [1.0] * 8, np.float32)
    else:
        before = np.arange(HALF - 8, HALF); after = np.arange(0, 8); rest = np.arange(8, HALF - 8)
        hm = np.array([1.0] * 8 + [0.0] * 8, np.float32)
    return own, np.concatenate([before, after, rest]), hm


def fmaj(v, n=128):
    v = np.asarray(v, np.float32)
    return np.ascontiguousarray(v.reshape(-1, n).T)


def prep_shared(cfg, inp):
    D, E, FF, FC, PG = cfg["D"], cfg["E"], cfg["FF"], cfg["FC"], cfg["PG"]
    f = lambda a: np.ascontiguousarray(np.asarray(a, np.float32))
    sh = {}
    sh["w_ada"] = f(inp["w_ada"][0]); sh["bT_ada"] = fmaj(inp["b_ada"][0])
    sh["g1T"] = fmaj(inp["norm1_g"][0]); sh["g2T"] = fmaj(inp["norm2_g"][0])
    sh["fg_row"] = f(inp["final_g"]).reshape(1, D)
    sh["w_in"] = f(inp["w_in"][0])
    sh["gq"] = np.ascontiguousarray(np.tile(f(inp["q_norm_g"][0])[None, :], (128, 1)))
    sh["gk"] = np.ascontiguousarray(np.tile(f(inp["k_norm_g"][0])[None, :], (128, 1)))
    sh["w_pool"] = f(inp["w_pool"][0]).reshape(4 * PG, PG)
    sh["pscT"] = fmaj(inp["pool_scale"][0])
    sh["w_out"] = f(inp["w_out"][0])
    sh["w_router"] = np.ascontiguousarray(f(inp["w_router"][0]).reshape(cfg["KC"], 128, E).transpose(1, 0, 2).reshape(128, cfg["KC"] * E))
    sh["b_router"] = np.ascontiguousarray(np.tile(f(inp["b_router"][0]).reshape(1, E), (128, 1)))
    KC, DC, H2 = cfg["KC"], D // 512, FC // 2
    G = cfg["G"]; NB = 4 * cfg["HALF"] // G + E
    def relayout_gu(w):
        w = f(w).reshape(E, KC, 128, FC, 128)
        return np.ascontiguousarray(w.transpose(0, 3, 2, 1, 4)).reshape(E * FC * 128, KC * 128)
    sh["w_gate"] = relayout_gu(inp["w_gate"][0])
    sh["w_up"] = relayout_gu(inp["w_up"][0])
    wd = f(inp["w_down"][0]).reshape(E, 2, H2, 128, DC, 512)
    sh["w_down"] = np.ascontiguousarray(wd.transpose(0, 4, 1, 3, 2, 5)).reshape(E * DC * 2 * 128, H2 * 512)
    sh["bgT"] = np.ascontiguousarray(f(inp["b_gate"][0]).reshape(E, FC, 128).transpose(0, 2, 1)).reshape(E * 128, FC)
    sh["buT"] = np.ascontiguousarray(f(inp["b_up"][0]).reshape(E, FC, 128).transpose(0, 2, 1)).reshape(E * 128, FC)
    sh["b_down"] = f(inp["b_down"][0])
    p = np.arange(128, dtype=np.float32)
    sh["ustrict"] = np.ascontiguousarray((p[:, None] < p[None, :]).astype(np.float32))
    sh["iota_e"] = np.ascontiguousarray(np.tile(np.arange(E, dtype=np.float32)[None, :], (128, 1)))
    sh["iota_pf"] = np.ascontiguousarray(p[:, None] + 128.0 * np.arange(FC, dtype=np.float32)[None, :])
    sh["iota_pd"] = np.ascontiguousarray(p[:, None] + 128.0 * np.arange(DC * 2, dtype=np.float32)[None, :])
    sh["iota_p"] = np.ascontiguousarray(p[:, None])
    sh["blkthr"] = np.ascontiguousarray(np.tile((np.arange(NB, dtype=np.float32) * G)[None, :], (128, 1)))
    sh["ident"] = np.eye(128, dtype=np.float32)
    return sh


def prep_core(cfg, inp, sh, b, s):
    D, S, HALF, CTX, NT, KC = cfg["D"], cfg["S"], cfg["HALF"], cfg["CTX"], cfg["NT"], cfg["KC"]
    own, other, hm = token_order(cfg, s)
    x = np.asarray(inp["x"], np.float32); ctx = np.asarray(inp["ctx"], np.float32)
    m = dict(sh)
    m["xin"] = np.ascontiguousarray(np.concatenate([x[b][own], x[b][other], ctx[b]], axis=0))
    cT = np.stack([fmaj(np.asarray(inp["c"], np.float32)[b]), fmaj(inp["c_ctx"])], axis=-1)
    m["cT"] = np.ascontiguousarray(cT)
    tok = np.concatenate([own, other]).astype(np.float64)
    freqs = ROPE_THETA ** (-np.arange(32, dtype=np.float64) / 32.0)
    freqs32 = freqs.astype(np.float32)
    row = np.floor(tok / GRID_W).astype(np.float32); col = (tok % GRID_W).astype(np.float32)
    ang = np.concatenate([row[:, None] * freqs32[None, :], col[:, None] * freqs32[None, :]], axis=-1).astype(np.float32)
    cs = np.concatenate([np.cos(ang), np.sin(ang)], axis=-1).astype(np.float32)
    cs_ctx = np.concatenate([np.ones((CTX, 64), np.float32), np.zeros((CTX, 64), np.float32)], axis=-1)
    m["cs"] = np.ascontiguousarray(np.concatenate([cs, cs_ctx], axis=0))
    t = own
    ic = []
    for w in (2, 4, 8, 16):
        lo = np.clip(t - w // 2, 0, S); hi = np.clip(t + w // 2, 0, S)
        ic.append(1.0 / (hi - lo).astype(np.float32))
    m["invcnt"] = np.ascontiguousarray(np.stack(ic).astype(np.float32))
    m["hmask"] = hm.reshape(1, 16)
    return m


def kernel(**inputs):
    cfg = FULL
    B, S, HALF, D = cfg["B"], cfg["S"], cfg["HALF"], cfg["D"]
    inp = {k: np.asarray(v) for k, v in inputs.items()}
    sh = prep_shared(cfg, inp)
    in_maps = []
    for b in range(B):
        for s in range(2):
            in_maps.append(prep_core(cfg, inp, sh, b, s))
    nc = build_program(cfg, debug=False)
    res = run_bass_kernel_spmd(nc, in_maps, core_ids=list(range(2 * B)))
    out = np.empty((B, S, D), np.float32)
    for b in range(B):
        for s in range(2):
            out[b, s * HALF:(s + 1) * HALF] = res.results[b * 2 + s]["out"]
    return out
```

```python
import numpy as np
import concourse.bass as bass
import concourse.mybir as mybir
from concourse.bass_utils import run_bass_kernel_spmd
from contextlib import ExitStack

F32 = mybir.dt.float32
BF16 = mybir.dt.bfloat16
U32 = mybir.dt.uint32
AF = mybir.ActivationFunctionType
ALU = mybir.AluOpType
AX = mybir.AxisListType


class Buf:
    def __init__(self, name, ap):
        self.name = name
        self._ap = ap
        self.lw = None
        self.rd = []

    def ap(self):
        return self._ap


class Sched:
    def __init__(self, nc, n_dma_sems=24):
        self.nc = nc
        self.eng = {"pe": nc.tensor, "act": nc.scalar, "dve": nc.vector,
                    "pool": nc.gpsimd, "sync": nc.sync}
        self.sems = {}
        self.tick = {}
        self.waited = {k: {} for k in self.eng}
        for k in ("pe", "act", "dve", "pool"):
            self.sems[k] = nc.alloc_semaphore("s_" + k)
            self.tick[k] = 0
        self.dpool = {}
        for q in ("sync", "pool", "act"):
            lst = []
            for i in range(n_dma_sems if q != "act" else 8):
                key = "d_%s_%d" % (q, i)
                self.sems[key] = nc.alloc_semaphore(key)
                lst.append([key, 0])
            self.dpool[q] = [lst, 0]
        self.out_deps = []
        self.n_ops = 0

    def sbuf(self, name, shape, dt):
        return Buf(name, self.nc.alloc_sbuf_tensor(name, list(shape), dt).ap())

    def psum(self, name, shape, dt):
        return Buf(name, self.nc.alloc_psum_tensor(name, list(shape), dt).ap())

    def dram(self, ap, name):
        return Buf(name, ap)

    def scratch(self, name, shape, dt):
        return Buf(name, self.nc.dram_tensor(name, list(shape), dt, kind="Internal").ap())

    def _wait(self, engine, key, val):
        if key == engine and engine == "pe":
            return
        w = self.waited[engine]
        if w.get(key, 0) >= val:
            return
        self.eng[engine].wait_ge(self.sems[key], val)
        w[key] = val

    def _deps(self, r, w):
        deps = []
        for b in r:
            if b.lw is not None:
                deps.append(b.lw)
        for b in w:
            if b.lw is not None:
                deps.append(b.lw)
            deps.extend(b.rd)
        return deps

    def _mark(self, me, r, w):
        for b in r:
            b.rd.append(me)
            if len(b.rd) > 64:
                d = {}
                for k, v in b.rd:
                    d[k] = max(d.get(k, 0), v)
                b.rd = list(d.items())
        for b in w:
            b.lw = me
            b.rd = []

    def op(self, engine, fn, r=(), w=()):
        for k, v in self._deps(r, w):
            self._wait(engine, k, v)
        ins = fn(self.eng[engine])
        self.tick[engine] += 1
        ins.then_inc(self.sems[engine], 1)
        self._mark((engine, self.tick[engine]), r, w)
        self.n_ops += 1
        return ins

    def dma(self, queue, out_ap, in_ap, r=(), w=(), is_output=False, **kw):
        for k, v in self._deps(r, w):
            self._wait(queue, k, v)
        lst, idx = self.dpool[queue]
        ent = lst[idx % len(lst)]
        self.dpool[queue][1] = idx + 1
        key, cnt = ent
        if cnt > 0:
            self._wait(queue, key, 16 * cnt)
        ins = self.eng[queue].dma_start(out=out_ap, in_=in_ap, **kw)
        ent[1] = cnt + 1
        ins.then_inc(self.sems[key], 16)
        me = (key, 16 * (cnt + 1))
        self._mark(me, r, w)
        if is_output:
            self.out_deps.append(me)
        self.n_ops += 1
        return ins

    def idma(self, out_ap, in_ap, idx_ap, scatter, r=(), w=()):
        queue = "pool"
        for k, v in self._deps(r, w):
            self._wait(queue, k, v)
        lst, idx = self.dpool[queue]
        ent = lst[idx % len(lst)]
        self.dpool[queue][1] = idx + 1
        key, cnt = ent
        if cnt > 0:
            self._wait(queue, key, 16 * cnt)
        off = bass.IndirectOffsetOnAxis(ap=idx_ap, axis=0)
        if scatter:
            ins = self.nc.gpsimd.indirect_dma_start(out=out_ap, out_offset=off, in_=in_ap, in_offset=None)
        else:
            ins = self.nc.gpsimd.indirect_dma_start(out=out_ap, out_offset=None, in_=in_ap, in_offset=off)
        ent[1] = cnt + 1
        ins.then_inc(self.sems[key], 16)
        self._mark((key, 16 * (cnt + 1)), r, w)
        self.n_ops += 1
        return ins

    def finish(self):
        for q in ("sync", "pool", "act"):
            for key, cnt in self.dpool[q][0]:
                if cnt > 0:
                    self._wait("sync", key, 16 * cnt)
        for k in ("pe", "act", "dve", "pool"):
            if self.tick[k] > 0:
                self._wait("sync", k, self.tick[k])


def make_cfg(D=4096, S=4096, CTX=256, E=32, FF=1536, B=4, BS=None):
    c = dict(D=D, S=S, CTX=CTX, E=E, FF=FF, B=B)
    c["KC"] = D // 128
    c["HALF"] = S // 2
    c["NT"] = S + CTX
    c["G"] = min(512, c["HALF"])
    c["ATTN_W"] = 3 * D // 4
    c["NH"] = c["ATTN_W"] // 128
    c["NKV"] = c["NH"] // 3
    c["KVW"] = c["NKV"] * 128
    c["POOL_W"] = D // 4
    c["PG"] = c["POOL_W"] // 4
    c["IN_W"] = c["ATTN_W"] + 2 * c["KVW"] + c["POOL_W"]
    c["FC"] = FF // 128
    c["BS"] = BS if BS else c["G"]
    return c


FULL = make_cfg(BS=384)
_STOP = ''
EPS = 1e-6
ROPE_THETA = 10000.0
GRID_W = 64
TOPK = 4


def bcast_rows(buf_ap_tensor, offset, n, parts=128):
    return bass.AP(tensor=buf_ap_tensor, offset=offset, ap=[[0, parts], [1, n]])


def build_program(cfg, debug=False):
    D, KC, HALF, NT, G, CTX = cfg["D"], cfg["KC"], cfg["HALF"], cfg["NT"], cfg["G"], cfg["CTX"]
    NH, NKV, KVW, ATTN_W, POOL_W, PG, IN_W = (cfg["NH"], cfg["NKV"], cfg["KVW"], cfg["ATTN_W"],
                                             cfg["POOL_W"], cfg["PG"], cfg["IN_W"])
    E, FF, FC, S = cfg["E"], cfg["FF"], cfg["FC"], cfg["S"]
    GT_ = G // 128
    TT = NT // 128
    NG_OWN = HALF // G
    PCH = POOL_W // 128
    PGC = PG // 128
    KS = 8
    assert KC % KS == 0 and FC % 2 == 0 and CTX <= G and CTX % 128 == 0

    nc = bass.Bass("TRN2", target_bir_lowering=False)
    S_ = Sched(nc)

    def din(name, shape, dt=F32):
        return S_.dram(nc.dram_tensor(name, list(shape), dt, kind="ExternalInput").ap(), name)

    xin = din("xin", [NT, D])
    cT = din("cT", [128, KC, 2])
    w_ada = din("w_ada", [D, 6 * D])
    bT_ada = din("bT_ada", [128, 6 * KC])
    g1T = din("g1T", [128, KC])
    g2T = din("g2T", [128, KC])
    fg_row = din("fg_row", [1, D])
    w_in = din("w_in", [D, IN_W])
    gq = din("gq", [128, 128])
    gk = din("gk", [128, 128])
    w_pool = din("w_pool", [4 * PG, PG])
    pscT = din("pscT", [128, PCH])
    w_out = din("w_out", [D, D])
    w_router = din("w_router", [128, KC * E])
    b_router = din("b_router", [128, E])
    DC = D // 512
    H2 = FC // 2
    BS = cfg["BS"]
    NTL = HALF // 128
    NB = -(-4 * HALF // BS) + E
    NSLOT = NB * BS
    w_gate = din("w_gate", [E * FC * 128, KC * 128])
    bgT = din("bgT", [E * 128, FC])
    w_up = din("w_up", [E * FC * 128, KC * 128])
    buT = din("buT", [E * 128, FC])
    w_down = din("w_down", [E * DC * 2 * 128, H2 * 512])
    b_down = din("b_down", [E, D])
    ustrict = din("ustrict", [128, 128])
    iota_e = din("iota_e", [128, E])
    iota_pf = din("iota_pf", [128, FC])
    iota_pd = din("iota_pd", [128, DC * 2])
    iota_p = din("iota_p", [128, 1])
    blkthr = din("blkthr", [128, NB])
    cs = din("cs", [NT, 128])
    ident_in = din("ident", [128, 128])
    invcnt = din("invcnt", [4, HALF])
    hmask = din("hmask", [1, 16])

    out = S_.dram(nc.dram_tensor("out", [HALF, D], F32, kind="ExternalOutput").ap(), "out")

    dbg_kind = "ExternalOutput" if debug else "Internal"

    def scr(name, shape, dt):
        return S_.dram(nc.dram_tensor(name, list(shape), dt, kind=dbg_kind).ap(), name)

    qT_s = scr("qT_s", [NH, 128, HALF], BF16)
    kT_s = scr("kT_s", [NKV, 128, NT], BF16)
    v_s = scr("v_s", [NT, KVW], BF16)
    uT_s = scr("uT_s", [PCH, 128, HALF + 16], F32)
    mixT_s = scr("mixT_s", [KC, 128, HALF], BF16)
    x1_s = scr("x1_s", [HALF, D], F32)
    h2tok_s = scr("h2tok_s", [HALF, D], BF16)
    xs_s = scr("xs_s", [NSLOT, D], BF16)
    NYS = 2
    YW = D // NYS
    ys_parts = [scr("ys_s%d" % i, [NSLOT, YW], F32) for i in range(NYS)]
    modrow_s = scr("modrow_s", [4, D], F32)

    banks = [S_.psum("bank%d" % i, [128, 512], F32) for i in range(8)]

    ident = S_.sbuf("ident_sb", [128, 128], F32)
    identb = S_.sbuf("identb", [128, 128], BF16)
    modT = S_.sbuf("modT", [128, 6 * KC, 2], F32)
    s1 = S_.sbuf("s1", [128, KC, 2], F32)
    s2 = S_.sbuf("s2", [128, KC], F32)
    slot_u = S_.sbuf("slot_u", [128, NTL, 4], U32)
    gk_all = S_.sbuf("gk_all", [128, NTL, 4], F32)
    widx_g = S_.sbuf("widx_g", [128, NB, FC], U32)
    widx_d = S_.sbuf("widx_d", [128, NB, DC * 2], U32)
    bidx = S_.sbuf("bidx", [128, NB], U32)
    bdidx = S_.sbuf("bdidx", [128, NB], U32)
    S_.dma("sync", ident.ap(), ident_in.ap(), r=[ident_in], w=[ident])
    S_.op("dve", lambda e: e.tensor_copy(identb.ap(), ident.ap()), r=[ident], w=[identb])

    def barrier():
        keys = []
        for k in ("pe", "act", "dve", "pool"):
            if S_.tick[k] > 0:
                keys.append((k, S_.tick[k]))
        for q in ("sync", "pool", "act"):
            for key, cnt in S_.dpool[q][0]:
                if cnt > 0:
                    keys.append((key, 16 * cnt))
        for eng in ("pe", "act", "dve", "pool", "sync"):
            for k, v in keys:
                if k != eng:
                    S_._wait(eng, k, v)

    def rstd_from_ss(ss, rstd, n, inv_n):
        S_.op("dve", lambda e: e.tensor_scalar(rstd.ap()[:, 0:n], ss.ap()[:, 0:n], inv_n, EPS, ALU.mult, ALU.add),
              r=[ss], w=[rstd])
        S_.op("act", lambda e: e.activation(rstd.ap()[:, 0:n], rstd.ap()[:, 0:n], AF.Sqrt), r=[rstd], w=[rstd])
        S_.op("dve", lambda e: e.reciprocal(rstd.ap()[:, 0:n], rstd.ap()[:, 0:n]), r=[rstd], w=[rstd])

    with ExitStack() as es:
        def sb(name, shape, dt):
            return Buf(name, es.enter_context(nc.sbuf_tensor(name, list(shape), dt)).ap())
        sc = sb("sc", [128, KC, 2], F32)
        aw = [sb("adaw0", [128, KC, 128], F32), sb("adaw1", [128, KC, 128], F32)]
        sm = sb("smallT", [128, 6 * KC], F32); rowt = sb("rowt", [KC, 128], F32)
        S_.dma("sync", sc.ap(), cT.ap(), r=[cT], w=[sc])
        S_.op("act", lambda e: e.activation(sc.ap(), sc.ap(), AF.Silu), r=[sc], w=[sc])
        S_.dma("sync", sm.ap(), bT_ada.ap(), r=[bT_ada], w=[sm])
        pm = banks[0]
        wv = w_ada.ap().rearrange("(kc p) n -> p kc n", p=128)
        for j in range(6 * KC):
            a = aw[j % 2]
            S_.dma("sync", a.ap(), wv[:, :, j * 128:(j + 1) * 128], r=[w_ada], w=[a])
            for kc in range(KC):
                S_.op("pe", lambda e, a=a, kc=kc, j=j: e.matmul(
                    pm.ap()[:, 2 * j:2 * j + 2], a.ap()[:, kc, :], sc.ap()[:, kc, :],
                    start=(kc == 0), stop=(kc == KC - 1)), r=[a, sc], w=[pm])
        S_.op("dve", lambda e: e.tensor_tensor(
            modT.ap(), pm.ap()[:, 0:12 * KC].rearrange("p (j m) -> p j m", m=2),
            sm.ap().unsqueeze(2).broadcast_to([128, 6 * KC, 2]), ALU.add), r=[pm, sm], w=[modT])
        S_.dma("sync", sm.ap()[:, 0:KC], g1T.ap(), r=[g1T], w=[sm])
        S_.dma("sync", sm.ap()[:, KC:2 * KC], g2T.ap(), r=[g2T], w=[sm])
        S_.op("dve", lambda e: e.tensor_scalar(s1.ap(), modT.ap()[:, KC:2 * KC, :], 1.0, None, ALU.add),
              r=[modT], w=[s1])
        S_.op("dve", lambda e: e.tensor_tensor(
            s1.ap(), s1.ap(), sm.ap()[:, 0:KC].unsqueeze(2).broadcast_to([128, KC, 2]), ALU.mult),
            r=[s1, sm], w=[s1])
        S_.op("dve", lambda e: e.tensor_scalar(s2.ap(), modT.ap()[:, 4 * KC:5 * KC, 0], 1.0, None, ALU.add),
              r=[modT], w=[s2])
        S_.op("dve", lambda e: e.tensor_tensor(s2.ap(), s2.ap(), sm.ap()[:, KC:2 * KC], ALU.mult),
              r=[s2, sm], w=[s2])
        row_srcs = [(modT, modT.ap()[:, 2 * KC:3 * KC, 0]), (modT, modT.ap()[:, 5 * KC:6 * KC, 0]),
                    (s2, s2.ap()), (modT, modT.ap()[:, 3 * KC:4 * KC, 0])]
        for i, (sb_src, src_ap) in enumerate(row_srcs):
            S_.op("dve", lambda e, src_ap=src_ap: e.tensor_copy(sm.ap()[:, 2 * KC:3 * KC], src_ap),
                  r=[sb_src], w=[sm])
            S_.op("pe", lambda e: e.transpose(banks[1].ap()[0:KC, 0:128], sm.ap()[:, 2 * KC:3 * KC], ident.ap()),
                  r=[sm, ident], w=[banks[1]])
            S_.op("dve", lambda e: e.tensor_copy(rowt.ap(), banks[1].ap()[0:KC, 0:128]), r=[banks[1]], w=[rowt])
            S_.dma("pool", modrow_s.ap()[i:i + 1, :].rearrange("o (kc p) -> (o kc) p", p=128), rowt.ap(),
                   r=[rowt], w=[modrow_s])
    barrier()

    groups = []
    for g in range(NG_OWN):
        groups.append((g * G, G, "own"))
    for g in range(NG_OWN):
        groups.append((HALF + g * G, G, "other0" if g == 0 else "other"))
    groups.append((S, CTX, "ctx"))
    QC = ATTN_W // 512
    KCH = KVW // 512
    UCH = POOL_W // 512
    assert ATTN_W % 512 == 0 and KVW % 512 == 0 and POOL_W % 512 == 0

    with ExitStack() as es:
        def sb(name, shape, dt):
            return Buf(name, es.enter_context(nc.sbuf_tensor(name, list(shape), dt)).ap())
        hT = sb("hT", [128, KC, G], BF16)
        xt = [sb("xt0", [128, D], F32), sb("xt1", [128, D], F32)]
        ws = [sb("ws0", [128, KS, 512], F32), sb("ws1", [128, KS, 512], F32)]
        wb = [sb("wb0", [128, KS, 512], BF16), sb("wb1", [128, KS, 512], BF16)]
        ss = sb("ssA", [128, 8], F32); rs = sb("rsA", [128, 8], F32)
        tmpA = sb("tmpA", [128, 512], F32); tmpB = sb("tmpB", [128, 512], F32)
        tmpC = sb("tmpC", [128, 256], F32); tmpD = sb("tmpD", [128, 256], F32)
        qr = sb("qr", [128, 512], BF16); qTg = sb("qTg", [128, 4, G], BF16)
        vg = sb("vg", [128, GT_, 512], BF16); ug = sb("ug", [128, 4, G], F32)
        gqs = sb("gqs", [128, 128], F32); gks = sb("gks", [128, 128], F32)
        cst = sb("cst", [128, GT_, 128], F32)
        S_.dma("sync", gqs.ap(), gq.ap(), r=[gq], w=[gqs])
        S_.dma("sync", gks.ap(), gk.ap(), r=[gk], w=[gks])
        win_v = w_in.ap().rearrange("(kc p) n -> p kc n", p=128)
        slab_i = [0]
        xi = [0]

        for (t0, ng, kind) in groups:
            ntt = ng // 128
            mcol = 1 if kind == "ctx" else 0
            S_.dma("sync", cst.ap()[:, 0:ntt, :], cs.ap()[t0:t0 + ng, :].rearrange("(t p) c -> p t c", p=128),
                   r=[cs], w=[cst])
            for tt in range(ntt):
                x_ = xt[xi[0] % 2]; xi[0] += 1
                S_.dma("sync", x_.ap(), xin.ap()[t0 + tt * 128:t0 + (tt + 1) * 128, :], r=[xin], w=[x_])
                S_.op("act", lambda e, x_=x_: e.activation(tmpA.ap()[:, 0:512], x_.ap()[:, 0:512], AF.Square),
                      r=[x_], w=[tmpA]) if False else None
                for ch in range(D // 512):
                    S_.op("act", lambda e, x_=x_, ch=ch: e.activation(
                        tmpA.ap(), x_.ap()[:, ch * 512:(ch + 1) * 512], AF.Square,
                        accum_out=ss.ap()[:, ch:ch + 1]), r=[x_], w=[tmpA, ss])
                S_.op("dve", lambda e: e.tensor_reduce(ss.ap()[:, 7:8] if D // 512 < 8 else rs.ap()[:, 1:2],
                                                       ss.ap()[:, 0:D // 512], AX.X, ALU.add), r=[ss], w=[rs, ss])
                src = ss if D // 512 < 8 else rs
                sc_col = 7 if D // 512 < 8 else 1
                S_.op("dve", lambda e, src=src, sc_col=sc_col: e.tensor_scalar(
                    rs.ap()[:, 0:1], src.ap()[:, sc_col:sc_col + 1], 1.0 / D, EPS, ALU.mult, ALU.add), r=[src], w=[rs])
                S_.op("act", lambda e: e.activation(rs.ap()[:, 0:1], rs.ap()[:, 0:1], AF.Sqrt), r=[rs], w=[rs])
                S_.op("dve", lambda e: e.reciprocal(rs.ap()[:, 0:1], rs.ap()[:, 0:1]), r=[rs], w=[rs])
                S_.op("act", lambda e, x_=x_: e.activation(x_.ap(), x_.ap(), AF.Copy, scale=rs.ap()[:, 0:1]),
                      r=[x_, rs], w=[x_])
                for kc in range(KC):
                    pb = banks[kc % 2]
                    S_.op("pe", lambda e, x_=x_, kc=kc, pb=pb: e.transpose(
                        pb.ap()[:, 0:128], x_.ap()[:, kc * 128:(kc + 1) * 128], ident.ap()), r=[x_, ident], w=[pb])
                    S_.op("act", lambda e, kc=kc, pb=pb, tt=tt, mcol=mcol: e.activation(
                        hT.ap()[:, kc, tt * 128:(tt + 1) * 128], pb.ap()[:, 0:128], AF.Identity,
                        bias=modT.ap()[:, kc, mcol:mcol + 1], scale=s1.ap()[:, kc, mcol:mcol + 1]),
                        r=[pb, modT, s1], w=[hT])

            def proj_chunk(col0, mode, hidx):
                for sl in range(KC // KS):
                    w_s = ws[slab_i[0] % 2]; w_b = wb[slab_i[0] % 2]; slab_i[0] += 1
                    S_.dma("sync", w_s.ap(), win_v[:, sl * KS:(sl + 1) * KS, col0:col0 + 512], r=[w_in], w=[w_s])
                    S_.op("dve", lambda e, w_s=w_s, w_b=w_b: e.tensor_copy(w_b.ap(), w_s.ap()), r=[w_s], w=[w_b])
                    for k8 in range(KS):
                        kc = sl * KS + k8
                        for j in range(4 if mode == "u" else ntt):
                            pb = banks[2 + j]
                            if mode == "u":
                                S_.op("pe", lambda e, pb=pb, j=j, k8=k8, kc=kc, w_b=w_b: e.matmul(
                                    pb.ap()[:, 0:ng], w_b.ap()[:, k8, j * 128:(j + 1) * 128], hT.ap()[:, kc, 0:ng],
                                    start=(kc == 0), stop=(kc == KC - 1)), r=[w_b, hT], w=[pb])
                            else:
                                S_.op("pe", lambda e, pb=pb, j=j, k8=k8, kc=kc, w_b=w_b: e.matmul(
                                    pb.ap(), hT.ap()[:, kc, j * 128:(j + 1) * 128], w_b.ap()[:, k8, :],
                                    start=(kc == 0), stop=(kc == KC - 1)), r=[w_b, hT], w=[pb])
                if mode == "u":
                    for j in range(4):
                        S_.op("act", lambda e, j=j: e.activation(ug.ap()[:, j, 0:ng], banks[2 + j].ap()[:, 0:ng], AF.Copy),
                              r=[banks[2 + j]], w=[ug])
                    n_st = ng if kind == "own" else 16
                    S_.dma("pool", uT_s.ap()[hidx * 4:(hidx + 1) * 4, :, t0 if kind == "own" else HALF:(t0 if kind == "own" else HALF) + n_st]
                           .rearrange("c p t -> p c t"), ug.ap()[:, :, 0:n_st], r=[ug], w=[uT_s])
                    return
                if mode == "v":
                    for j in range(ntt):
                        S_.op("act", lambda e, j=j: e.activation(vg.ap()[:, j, :], banks[2 + j].ap(), AF.Copy),
                              r=[banks[2 + j]], w=[vg])
                    S_.dma("pool", v_s.ap()[t0:t0 + ng, hidx * 512:(hidx + 1) * 512].rearrange("(t p) c -> p t c", p=128),
                           vg.ap()[:, 0:ntt, :], r=[vg], w=[v_s])
                    return
                gg = gqs if mode == "q" else gks
                for j in range(ntt):
                    pb = banks[2 + j]
                    S_.op("act", lambda e, pb=pb: e.activation(tmpA.ap(), pb.ap(), AF.Square), r=[pb], w=[tmpA])
                    S_.op("dve", lambda e: e.tensor_reduce(ss.ap()[:, 0:4], tmpA.ap().rearrange("p (h d) -> p h d", d=128),
                                                           AX.X, ALU.add), r=[tmpA], w=[ss])
                    rstd_from_ss(ss, rs, 4, 1.0 / 128)
                    S_.op("dve", lambda e, pb=pb: e.tensor_tensor(
                        tmpB.ap().rearrange("p (h d) -> p h d", d=128), pb.ap().rearrange("p (h d) -> p h d", d=128),
                        rs.ap()[:, 0:4].unsqueeze(2).broadcast_to([128, 4, 128]), ALU.mult), r=[pb, rs], w=[tmpB])
                    S_.op("dve", lambda e, gg=gg: e.tensor_tensor(
                        tmpB.ap().rearrange("p (h d) -> p h d", d=128), tmpB.ap().rearrange("p (h d) -> p h d", d=128),
                        gg.ap().unsqueeze(1).broadcast_to([128, 4, 128]), ALU.mult), r=[tmpB, gg], w=[tmpB])
                    tb = tmpB.ap().rearrange("p (h d) -> p h d", d=128)
                    x1v, x2v = tb[:, :, 0:64], tb[:, :, 64:128]
                    cosv = cst.ap()[:, j, 0:64].unsqueeze(1).broadcast_to([128, 4, 64])
                    sinv = cst.ap()[:, j, 64:128].unsqueeze(1).broadcast_to([128, 4, 64])
                    cv = tmpC.ap().rearrange("p (h d) -> p h d", d=64)
                    dv = tmpD.ap().rearrange("p (h d) -> p h d", d=64)
                    qv = qr.ap().rearrange("p (h d) -> p h d", d=128)
                    S_.op("dve", lambda e: e.tensor_tensor(cv, x1v, cosv, ALU.mult), r=[tmpB, cst], w=[tmpC])
                    S_.op("dve", lambda e: e.tensor_tensor(dv, x2v, sinv, ALU.mult), r=[tmpB, cst], w=[tmpD])
                    S_.op("dve", lambda e: e.tensor_tensor(qv[:, :, 0:64], cv, dv, ALU.subtract), r=[tmpC, tmpD], w=[qr])
                    S_.op("dve", lambda e: e.tensor_tensor(cv, x1v, sinv, ALU.mult), r=[tmpB, cst], w=[tmpC])
                    S_.op("dve", lambda e: e.tensor_tensor(dv, x2v, cosv, ALU.mult), r=[tmpB, cst], w=[tmpD])
                    S_.op("dve", lambda e: e.tensor_tensor(qv[:, :, 64:128], cv, dv, ALU.add), r=[tmpC, tmpD], w=[qr])
                    tp = banks[6 + (j % 2)]
                    tpv = tp.ap().bitcast(BF16)
                    for hd in range(4):
                        S_.op("pe", lambda e, hd=hd, tpv=tpv, tp=tp: e.transpose(
                            tpv[:, hd * 128:(hd + 1) * 128], qr.ap()[:, hd * 128:(hd + 1) * 128], identb.ap()),
                            r=[qr, identb], w=[tp])
                    S_.op("act", lambda e, tpv=tpv, tp=tp, j=j: e.activation(
                        qTg.ap()[:, :, j * 128:(j + 1) * 128], tpv[:, 0:512].rearrange("p (h t) -> p h t", t=128), AF.Copy),
                        r=[tp], w=[qTg])
                if mode == "q":
                    S_.dma("pool", qT_s.ap()[hidx * 4:(hidx + 1) * 4, :, t0:t0 + ng].rearrange("h p t -> p h t"),
                           qTg.ap()[:, :, 0:ng], r=[qTg], w=[qT_s])
                else:
                    S_.dma("pool", kT_s.ap()[hidx * 4:(hidx + 1) * 4, :, t0:t0 + ng].rearrange("h p t -> p h t"),
                           qTg.ap()[:, :, 0:ng], r=[qTg], w=[kT_s])

            if kind == "own":
                for qc in range(QC):
                    proj_chunk(qc * 512, "q", qc)
            for kc_ in range(KCH):
                proj_chunk(ATTN_W + kc_ * 512, "k", kc_)
            for vc in range(KCH):
                proj_chunk(ATTN_W + KVW + vc * 512, "v", vc)
            if kind in ("own", "other0"):
                for uc in range(UCH):
                    proj_chunk(ATTN_W + 2 * KVW + uc * 512, "u", uc)
    barrier()

    ATT_SCALE = 128.0 ** -0.5
    with ExitStack() as es:
        def sb(name, shape, dt):
            return Buf(name, es.enter_context(nc.sbuf_tensor(name, list(shape), dt)).ap())
        KT = sb("KT", [128, NT], BF16); Vh = sb("Vh", [128, TT, 130], BF16)
        QT = [sb("QT0", [128, G], BF16), sb("QT1", [128, G], BF16)]
        PT = [sb("PT0", [128, G], BF16), sb("PT1", [128, G], BF16)]
        ob = [sb("ob0", [128, 128], BF16), sb("ob1", [128, 128], BF16)]
        rinv = [sb("rinv0", [128, 1], F32), sb("rinv1", [128, 1], F32)]
        oT = [sb("oT0", [128, G], BF16), sb("oT1", [128, G], BF16)]
        S_.op("dve", lambda e: e.memset(Vh.ap()[:, :, 128:130], 1.0), w=[Vh])
        qi = 0
        for h in range(NKV):
            S_.dma("sync", KT.ap(), kT_s.ap()[h], r=[kT_s], w=[KT])
            S_.dma("sync", Vh.ap()[:, :, 0:128], v_s.ap()[:, h * 128:(h + 1) * 128].rearrange("(t p) c -> p t c", p=128),
                   r=[v_s], w=[Vh])
            for gi in range(3):
                n = h * 3 + gi
                for qg in range(NG_OWN):
                    Q = QT[qi % 2]; o_T = oT[qi % 2]; qi += 1
                    S_.dma("sync", Q.ap(), qT_s.ap()[n, :, qg * G:(qg + 1) * G], r=[qT_s], w=[Q])
                    def qk(kt, Q=Q):
                        sbk = banks[kt % 2]
                        S_.op("pe", lambda e, sbk=sbk, kt=kt, Q=Q: e.matmul(
                            sbk.ap()[:, 0:G], KT.ap()[:, kt * 128:(kt + 1) * 128], Q.ap(), start=True, stop=True),
                            r=[KT, Q], w=[sbk])
                    qk(0)
                    for kt in range(TT):
                        sbk = banks[kt % 2]; P = PT[kt % 2]
                        S_.op("act", lambda e, sbk=sbk, P=P: e.activation(P.ap(), sbk.ap()[:, 0:G], AF.Exp, scale=ATT_SCALE),
                              r=[sbk], w=[P])
                        if kt + 1 < TT:
                            qk(kt + 1)
                        for qs in range(GT_):
                            S_.op("pe", lambda e, qs=qs, kt=kt, P=P: e.matmul(
                                banks[2 + qs].ap()[:, 0:129], P.ap()[:, qs * 128:(qs + 1) * 128], Vh.ap()[:, kt, 0:129],
                                start=(kt == 0), stop=(kt == TT - 1)), r=[P, Vh], w=[banks[2 + qs]])
                    tpv = banks[6].ap().bitcast(BF16)
                    for qs in range(GT_):
                        ri = rinv[qs % 2]; o_ = ob[qs % 2]
                        S_.op("dve", lambda e, ri=ri, qs=qs: e.reciprocal(ri.ap(), banks[2 + qs].ap()[:, 128:129]),
                              r=[banks[2 + qs]], w=[ri])
                        S_.op("act", lambda e, ri=ri, o_=o_, qs=qs: e.activation(
                            o_.ap(), banks[2 + qs].ap()[:, 0:128], AF.Copy, scale=ri.ap()), r=[banks[2 + qs], ri], w=[o_])
                        S_.op("pe", lambda e, o_=o_, qs=qs: e.transpose(tpv[:, qs * 128:(qs + 1) * 128], o_.ap(), identb.ap()),
                              r=[o_, identb], w=[banks[6]])
                    S_.op("dve", lambda e, o_T=o_T: e.tensor_copy(o_T.ap(), tpv[:, 0:G]), r=[banks[6]], w=[o_T])
                    S_.dma("pool", mixT_s.ap()[n, :, qg * G:(qg + 1) * G], o_T.ap(), r=[o_T], w=[mixT_s])
    barrier()

    with ExitStack() as es:
        def sb(name, shape, dt):
            return Buf(name, es.enter_context(nc.sbuf_tensor(name, list(shape), dt)).ap())
        L = HALF + 16
        Up = sb("Up", [128, L], F32); PA = sb("PA", [128, L], F32); PB = sb("PB", [128, L], F32)
        icn = sb("icn", [128, HALF], F32); dT = sb("dT", [128, PGC, HALF], BF16)
        wpf = sb("wpf", [128, PGC, PG], F32); wpb = sb("wpb", [128, PGC, PG], BF16)
        hmt = sb("hmt", [128, 16], F32); psc = sb("psc", [128, PCH], F32)
        yb = [sb("yb0", [128, G], BF16), sb("yb1", [128, G], BF16)]
        S_.dma("sync", hmt.ap(), bcast_rows(hmask.ap().tensor, 0, 16), r=[hmask], w=[hmt])
        S_.dma("sync", psc.ap(), pscT.ap(), r=[pscT], w=[psc])
        yi = 0
        for gi, w in enumerate((2, 4, 8, 16)):
            S_.dma("sync", icn.ap(), bcast_rows(invcnt.ap().tensor, gi * HALF, HALF), r=[invcnt], w=[icn])
            S_.dma("sync", wpf.ap(), w_pool.ap()[gi * PG:(gi + 1) * PG, :].rearrange("(cc p) e -> p cc e", p=128),
                   r=[w_pool], w=[wpf])
            S_.op("dve", lambda e: e.tensor_copy(wpb.ap(), wpf.ap()), r=[wpf], w=[wpb])
            for cc in range(PGC):
                ch = gi * PGC + cc
                S_.dma("sync", Up.ap()[:, 0:8], uT_s.ap()[ch, :, HALF:HALF + 8], r=[uT_s], w=[Up])
                S_.dma("sync", Up.ap()[:, 8:8 + HALF], uT_s.ap()[ch, :, 0:HALF], r=[uT_s], w=[Up])
                S_.dma("sync", Up.ap()[:, 8 + HALF:L], uT_s.ap()[ch, :, HALF + 8:HALF + 16], r=[uT_s], w=[Up])
                S_.op("dve", lambda e: e.tensor_tensor(Up.ap()[:, 0:8], Up.ap()[:, 0:8], hmt.ap()[:, 0:8], ALU.mult),
                      r=[Up, hmt], w=[Up])
                S_.op("dve", lambda e: e.tensor_tensor(Up.ap()[:, 8 + HALF:L], Up.ap()[:, 8 + HALF:L], hmt.ap()[:, 8:16], ALU.mult),
                      r=[Up, hmt], w=[Up])
                S_.op("dve", lambda e: e.tensor_tensor(PA.ap()[:, 1:L], Up.ap()[:, 0:L - 1], Up.ap()[:, 1:L], ALU.add),
                      r=[Up], w=[PA])
                fin = PA
                if w >= 4:
                    S_.op("dve", lambda e: e.tensor_tensor(PB.ap()[:, 2:L - 1], PA.ap()[:, 1:L - 2], PA.ap()[:, 3:L], ALU.add),
                          r=[PA], w=[PB])
                    fin = PB
                if w >= 8:
                    S_.op("dve", lambda e: e.tensor_tensor(PA.ap()[:, 4:L - 3], PB.ap()[:, 2:L - 5], PB.ap()[:, 6:L - 1], ALU.add),
                          r=[PB], w=[PA])
                    fin = PA
                if w >= 16:
                    S_.op("dve", lambda e: e.tensor_tensor(PB.ap()[:, 8:L - 7], PA.ap()[:, 4:L - 11], PA.ap()[:, 12:L - 3], ALU.add),
                          r=[PA], w=[PB])
                    fin = PB
                oth = PA if fin is PB else PB
                S_.op("dve", lambda e, fin=fin, oth=oth: e.tensor_tensor(
                    oth.ap()[:, 8:8 + HALF], fin.ap()[:, 8:8 + HALF], icn.ap(), ALU.mult), r=[fin, icn], w=[oth])
                S_.op("dve", lambda e, oth=oth, cc=cc: e.tensor_tensor(
                    dT.ap()[:, cc, :], oth.ap()[:, 8:8 + HALF], Up.ap()[:, 8:8 + HALF], ALU.subtract), r=[oth, Up], w=[dT])
            for ec in range(PGC):
                for tg in range(NG_OWN):
                    pb = banks[yi % 2]; y_ = yb[yi % 2]; yi += 1
                    for cc in range(PGC):
                        S_.op("pe", lambda e, pb=pb, cc=cc, ec=ec, tg=tg: e.matmul(
                            pb.ap()[:, 0:G], wpb.ap()[:, cc, ec * 128:(ec + 1) * 128], dT.ap()[:, cc, tg * G:(tg + 1) * G],
                            start=(cc == 0), stop=(cc == PGC - 1)), r=[wpb, dT], w=[pb])
                    co = gi * PGC + ec
                    S_.op("act", lambda e, pb=pb, y_=y_, co=co: e.activation(
                        y_.ap(), pb.ap()[:, 0:G], AF.Copy, scale=psc.ap()[:, co:co + 1]), r=[pb, psc], w=[y_])
                    S_.dma("pool", mixT_s.ap()[NH + co, :, tg * G:(tg + 1) * G], y_.ap(), r=[y_], w=[mixT_s])
    barrier()

    with ExitStack() as es:
        def sb(name, shape, dt):
            return Buf(name, es.enter_context(nc.sbuf_tensor(name, list(shape), dt)).ap())
        mT = sb("mT", [128, KC, G], BF16)
        h2f = sb("h2f", [128, KC, 128], F32)
        xt = [sb("xe0", [128, D], F32)]
        s2row = sb("s2row", [128, D], F32); t2row = sb("t2row", [128, D], F32)
        hk = sb("hk", [128, D], BF16)
        i8 = sb("i8", [128, 8], U32); e4 = sb("e4", [128, 4], F32)
        msk_all = sb("msk_all", [128, NTL, E], F32); rank_all = sb("rank_all", [128, NTL, E], F32)
        i8f_all = sb("i8f_all", [128, NTL, 4], F32)
        ones128 = sb("ones128", [128, 128], F32); ust = sb("ust", [128, 128], F32)
        S_.op("dve", lambda e: e.memset(ones128.ap(), 1.0), w=[ones128])
        S_.dma("sync", ust.ap(), ustrict.ap(), r=[ustrict], w=[ust])
        S_.dma("sync", s2row.ap(), bcast_rows(modrow_s.ap().tensor, 2 * D, D), r=[modrow_s], w=[s2row])
        S_.dma("sync", t2row.ap(), bcast_rows(modrow_s.ap().tensor, 3 * D, D), r=[modrow_s], w=[t2row])
        ws = [sb("wse0", [128, KS, 512], F32), sb("wse1", [128, KS, 512], F32)]
        wb = [sb("wbe0", [128, KS, 512], BF16), sb("wbe1", [128, KS, 512], BF16)]
        g2b = sb("g2b", [128, D], F32)
        xc = [sb("xc0", [128, 512], F32), sb("xc1", [128, 512], F32)]
        wr = sb("wr", [128, KC, E], F32); brb = sb("brb", [128, E], F32)
        ss = sb("ssE", [128, 8], F32); rs = sb("rsE", [128, 8], F32); tmpA = sb("tmpE", [128, 512], F32)
        lg = sb("lg", [128, E], F32); m8 = sb("m8", [128, 8], F32); msk = sb("msk", [128, E], F32)
        nm = sb("nm", [128, 1], F32)
        S_.dma("sync", g2b.ap(), bcast_rows(modrow_s.ap().tensor, 0, D), r=[modrow_s], w=[g2b])
        S_.dma("sync", wr.ap(), w_router.ap().rearrange("p (kc e) -> p kc e", e=E), r=[w_router], w=[wr])
        S_.dma("sync", brb.ap(), b_router.ap(), r=[b_router], w=[brb])
        wout_v = w_out.ap().rearrange("(kc p) n -> p kc n", p=128)
        si = 0; xi_ = 0; ci = 0
        for g in range(NG_OWN):
            t0 = g * G
            S_.dma("sync", mT.ap(), mixT_s.ap()[:, :, t0:t0 + G].rearrange("c p t -> p c t"), r=[mixT_s], w=[mT])
            for dc in range(D // 512):
                for sl in range(KC // KS):
                    w_s = ws[si % 2]; w_b = wb[si % 2]; si += 1
                    S_.dma("sync", w_s.ap(), wout_v[:, sl * KS:(sl + 1) * KS, dc * 512:(dc + 1) * 512], r=[w_out], w=[w_s])
                    S_.op("dve", lambda e, w_s=w_s, w_b=w_b: e.tensor_copy(w_b.ap(), w_s.ap()), r=[w_s], w=[w_b])
                    for k8 in range(KS):
                        kc = sl * KS + k8
                        for j in range(GT_):
                            S_.op("pe", lambda e, j=j, k8=k8, kc=kc, w_b=w_b: e.matmul(
                                banks[2 + j].ap(), mT.ap()[:, kc, j * 128:(j + 1) * 128], w_b.ap()[:, k8, :],
                                start=(kc == 0), stop=(kc == KC - 1)), r=[w_b, mT], w=[banks[2 + j]])
                for j in range(GT_):
                    x_c = xc[ci % 2]; ci += 1
                    rows = slice(t0 + j * 128, t0 + (j + 1) * 128)
                    S_.dma("sync", x_c.ap(), xin.ap()[rows, dc * 512:(dc + 1) * 512], r=[xin], w=[x_c])
                    S_.op("dve", lambda e, j=j, dc=dc: e.tensor_tensor(
                        tmpA.ap(), banks[2 + j].ap(), g2b.ap()[:, dc * 512:(dc + 1) * 512], ALU.mult),
                        r=[banks[2 + j], g2b], w=[tmpA])
                    S_.op("dve", lambda e, x_c=x_c: e.tensor_tensor(x_c.ap(), x_c.ap(), tmpA.ap(), ALU.add),
                          r=[x_c, tmpA], w=[x_c])
                    S_.dma("pool", x1_s.ap()[rows, dc * 512:(dc + 1) * 512], x_c.ap(), r=[x_c], w=[x1_s])
            for tt in range(GT_):
                ti = g * GT_ + tt
                x_ = xt[0]
                rows = slice(t0 + tt * 128, t0 + (tt + 1) * 128)
                S_.dma("sync", x_.ap(), x1_s.ap()[rows, :], r=[x1_s], w=[x_])
                for ch in range(D // 512):
                    S_.op("act", lambda e, x_=x_, ch=ch: e.activation(
                        tmpA.ap(), x_.ap()[:, ch * 512:(ch + 1) * 512], AF.Square,
                        accum_out=ss.ap()[:, ch:ch + 1]), r=[x_], w=[tmpA, ss])
                S_.op("dve", lambda e: e.tensor_reduce(rs.ap()[:, 1:2], ss.ap()[:, 0:D // 512], AX.X, ALU.add), r=[ss], w=[rs])
                S_.op("dve", lambda e: e.tensor_scalar(rs.ap()[:, 0:1], rs.ap()[:, 1:2], 1.0 / D, EPS, ALU.mult, ALU.add),
                      r=[rs], w=[rs])
                S_.op("act", lambda e: e.activation(rs.ap()[:, 0:1], rs.ap()[:, 0:1], AF.Sqrt), r=[rs], w=[rs])
                S_.op("dve", lambda e: e.reciprocal(rs.ap()[:, 0:1], rs.ap()[:, 0:1]), r=[rs], w=[rs])
                S_.op("act", lambda e, x_=x_: e.activation(x_.ap(), x_.ap(), AF.Copy, scale=rs.ap()[:, 0:1]),
                      r=[x_, rs], w=[x_])
                for ch in range(D // 512):
                    cs_ = slice(ch * 512, (ch + 1) * 512)
                    S_.op("dve", lambda e, cs_=cs_: e.tensor_tensor(tmpA.ap(), x_.ap()[:, cs_], s2row.ap()[:, cs_], ALU.mult),
                          r=[x_, s2row], w=[tmpA])
                    S_.op("dve", lambda e, cs_=cs_: e.tensor_tensor(hk.ap()[:, cs_], tmpA.ap(), t2row.ap()[:, cs_], ALU.add),
                          r=[tmpA, t2row], w=[hk])
                S_.dma("pool", h2tok_s.ap()[rows, :], hk.ap(), r=[hk], w=[h2tok_s])
                for kc in range(KC):
                    pb = banks[kc % 2]
                    S_.op("pe", lambda e, x_=x_, kc=kc, pb=pb: e.transpose(
                        pb.ap()[:, 0:128], x_.ap()[:, kc * 128:(kc + 1) * 128], ident.ap()), r=[x_, ident], w=[pb])
                    S_.op("act", lambda e, kc=kc, pb=pb: e.activation(
                        h2f.ap()[:, kc, :], pb.ap()[:, 0:128], AF.Identity,
                        bias=modT.ap()[:, 3 * KC + kc, 0:1], scale=s2.ap()[:, kc:kc + 1]), r=[pb, modT, s2], w=[h2f])
                for kc in range(KC):
                    S_.op("pe", lambda e, kc=kc: e.matmul(banks[6].ap()[:, 0:E], h2f.ap()[:, kc, :], wr.ap()[:, kc, :],
                                                          start=(kc == 0), stop=(kc == KC - 1)), r=[h2f, wr], w=[banks[6]])
                S_.op("dve", lambda e: e.tensor_tensor(lg.ap(), banks[6].ap()[:, 0:E], brb.ap(), ALU.add), r=[banks[6], brb], w=[lg])
                S_.op("dve", lambda e: e.max(m8.ap(), lg.ap()), r=[lg], w=[m8])
                S_.op("dve", lambda e: e.max_index(i8.ap(), m8.ap(), lg.ap()), r=[lg, m8], w=[i8])
                S_.op("dve", lambda e, ti=ti: e.tensor_scalar(msk_all.ap()[:, ti, :], lg.ap(), m8.ap()[:, TOPK - 1:TOPK], None, ALU.is_ge),
                      r=[lg, m8], w=[msk_all])
                S_.op("dve", lambda e, ti=ti: e.tensor_copy(i8f_all.ap()[:, ti, :], i8.ap()[:, 0:4]), r=[i8], w=[i8f_all])
                S_.op("dve", lambda e: e.tensor_scalar(nm.ap(), m8.ap()[:, 0:1], -1.0, None, ALU.mult), r=[m8], w=[nm])
                S_.op("act", lambda e: e.activation(e4.ap(), m8.ap()[:, 0:4], AF.Exp, bias=nm.ap()), r=[m8, nm], w=[e4])
                S_.op("dve", lambda e: e.tensor_reduce(nm.ap(), e4.ap(), AX.X, ALU.add), r=[e4], w=[nm])
                S_.op("dve", lambda e: e.reciprocal(nm.ap(), nm.ap()), r=[nm], w=[nm])
                S_.op("dve", lambda e, ti=ti: e.tensor_scalar(gk_all.ap()[:, ti, :], e4.ap(), nm.ap(), None, ALU.mult),
                      r=[e4, nm], w=[gk_all])
                S_.op("pe", lambda e, ti=ti: e.matmul(banks[7].ap()[:, 0:E], ust.ap(), msk_all.ap()[:, ti, :],
                                                      start=True, stop=(ti == 0)), r=[ust, msk_all], w=[banks[7]])
                for tp in range(ti):
                    S_.op("pe", lambda e, tp=tp, ti=ti: e.matmul(banks[7].ap()[:, 0:E], ones128.ap(), msk_all.ap()[:, tp, :],
                                                                 start=False, stop=(tp == ti - 1)), r=[ones128, msk_all], w=[banks[7]])
                S_.op("dve", lambda e, ti=ti: e.tensor_copy(rank_all.ap()[:, ti, :], banks[7].ap()[:, 0:E]), r=[banks[7]], w=[rank_all])

        cnt = sb("cnt", [128, E], F32); nbt = sb("nbt", [128, E], F32); tq = sb("tq", [128, E], F32)
        cA = sb("cA", [128, E], F32); cB = sb("cB", [128, E], F32); startv = sb("startv", [128, E], F32)
        sfull = sb("sfull", [128, NTL, E], F32); oh = sb("oh", [128, NTL, E], F32); slot_f = sb("slot_f", [128, NTL, 4], F32)
        ie = sb("ie", [128, E], F32); thr = sb("thr", [128, NB], F32); cmpb = sb("cmpb", [128, NB, E], F32)
        ebf = sb("ebf", [128, NB], F32); ebm = sb("ebm", [128, NB], F32)
        ipf = sb("ipf", [128, FC], F32); ipd = sb("ipd", [128, DC * 2], F32); ipp = sb("ipp", [128, 1], F32)
        assert E >= max(FC, DC * 2)
        wtmp = cmpb
        S_.dma("sync", ie.ap(), iota_e.ap(), r=[iota_e], w=[ie])
        S_.dma("sync", thr.ap(), blkthr.ap(), r=[blkthr], w=[thr])
        S_.dma("sync", ipf.ap(), iota_pf.ap(), r=[iota_pf], w=[ipf])
        S_.dma("sync", ipd.ap(), iota_pd.ap(), r=[iota_pd], w=[ipd])
        S_.dma("sync", ipp.ap(), iota_p.ap(), r=[iota_p], w=[ipp])
        for i in range(NTL):
            S_.op("pe", lambda e, i=i: e.matmul(banks[7].ap()[:, 0:E], ones128.ap(), msk_all.ap()[:, i, :],
                                                start=(i == 0), stop=(i == NTL - 1)), r=[ones128, msk_all], w=[banks[7]])
        S_.op("dve", lambda e: e.tensor_copy(cnt.ap(), banks[7].ap()[:, 0:E]), r=[banks[7]], w=[cnt])
        S_.op("dve", lambda e: e.tensor_scalar(nbt.ap(), cnt.ap(), 0.0, None, ALU.is_gt), r=[cnt], w=[nbt])
        for j in range(1, -(-HALF // BS)):
            S_.op("dve", lambda e, j=j: e.tensor_scalar(tq.ap(), cnt.ap(), float(j * BS), None, ALU.is_gt), r=[cnt], w=[tq])
            S_.op("dve", lambda e: e.tensor_tensor(nbt.ap(), nbt.ap(), tq.ap(), ALU.add), r=[nbt, tq], w=[nbt])
        S_.op("dve", lambda e: e.tensor_scalar(nbt.ap(), nbt.ap(), float(BS), None, ALU.mult), r=[nbt], w=[nbt])
        S_.op("dve", lambda e: e.tensor_copy(cA.ap(), nbt.ap()), r=[nbt], w=[cA])
        cur, nxt = cA, cB
        sh_ = 1
        while sh_ < E:
            S_.op("dve", lambda e, cur=cur, nxt=nxt, sh_=sh_: e.tensor_copy(nxt.ap()[:, 0:sh_], cur.ap()[:, 0:sh_]), r=[cur], w=[nxt])
            S_.op("dve", lambda e, cur=cur, nxt=nxt, sh_=sh_: e.tensor_tensor(
                nxt.ap()[:, sh_:E], cur.ap()[:, sh_:E], cur.ap()[:, 0:E - sh_], ALU.add), r=[cur], w=[nxt])
            cur, nxt = nxt, cur
            sh_ *= 2
        endp = cur
        S_.op("dve", lambda e: e.tensor_tensor(startv.ap(), endp.ap(), nbt.ap(), ALU.subtract), r=[endp, nbt], w=[startv])
        S_.op("dve", lambda e: e.tensor_tensor(sfull.ap(), rank_all.ap(), startv.ap().unsqueeze(1).broadcast_to([128, NTL, E]), ALU.add),
              r=[rank_all, startv], w=[sfull])
        for k in range(4):
            S_.op("dve", lambda e, k=k: e.tensor_tensor(
                oh.ap(), ie.ap().unsqueeze(1).broadcast_to([128, NTL, E]),
                i8f_all.ap()[:, :, k:k + 1].broadcast_to([128, NTL, E]), ALU.is_equal), r=[ie, i8f_all], w=[oh])
            S_.op("dve", lambda e: e.tensor_tensor(oh.ap(), oh.ap(), sfull.ap(), ALU.mult), r=[oh, sfull], w=[oh])
            S_.op("dve", lambda e, k=k: e.tensor_reduce(slot_f.ap()[:, :, k], oh.ap(), AX.X, ALU.add), r=[oh], w=[slot_f])
        S_.op("dve", lambda e: e.tensor_copy(slot_u.ap(), slot_f.ap()), r=[slot_f], w=[slot_u])
        S_.op("dve", lambda e: e.tensor_tensor(
            cmpb.ap(), endp.ap().unsqueeze(1).broadcast_to([128, NB, E]),
            thr.ap().unsqueeze(2).broadcast_to([128, NB, E]), ALU.is_le), r=[endp, thr], w=[cmpb])
        S_.op("dve", lambda e: e.tensor_reduce(ebf.ap(), cmpb.ap(), AX.X, ALU.add), r=[cmpb], w=[ebf])
        S_.op("dve", lambda e: e.tensor_scalar(ebf.ap(), ebf.ap(), float(E - 1), None, ALU.min), r=[ebf], w=[ebf])
        S_.op("dve", lambda e: e.tensor_scalar(ebm.ap(), ebf.ap(), float(FC * 128), None, ALU.mult), r=[ebf], w=[ebm])
        S_.op("dve", lambda e: e.tensor_tensor(
            wtmp.ap()[:, :, 0:FC], ebm.ap().unsqueeze(2).broadcast_to([128, NB, FC]),
            ipf.ap().unsqueeze(1).broadcast_to([128, NB, FC]), ALU.add), r=[ebm, ipf], w=[wtmp])
        S_.op("dve", lambda e: e.tensor_copy(widx_g.ap(), wtmp.ap()[:, :, 0:FC]), r=[wtmp], w=[widx_g])
        S_.op("dve", lambda e: e.tensor_scalar(ebm.ap(), ebf.ap(), float(DC * 2 * 128), None, ALU.mult), r=[ebf], w=[ebm])
        S_.op("dve", lambda e: e.tensor_tensor(
            wtmp.ap()[:, :, 0:DC * 2], ebm.ap().unsqueeze(2).broadcast_to([128, NB, DC * 2]),
            ipd.ap().unsqueeze(1).broadcast_to([128, NB, DC * 2]), ALU.add), r=[ebm, ipd], w=[wtmp])
        S_.op("dve", lambda e: e.tensor_copy(widx_d.ap(), wtmp.ap()[:, :, 0:DC * 2]), r=[wtmp], w=[widx_d])
        S_.op("dve", lambda e: e.tensor_scalar(ebm.ap(), ebf.ap(), 128.0, ipp.ap(), ALU.mult, ALU.add), r=[ebf, ipp], w=[ebm])
        S_.op("dve", lambda e: e.tensor_copy(bidx.ap(), ebm.ap()), r=[ebm], w=[bidx])
        S_.op("dve", lambda e: e.tensor_copy(bdidx.ap(), ebf.ap()), r=[ebf], w=[bdidx])
        for i in range(NTL):
            S_.dma("sync", hk.ap(), h2tok_s.ap()[i * 128:(i + 1) * 128, :], r=[h2tok_s], w=[hk])
            for k in range(4):
                S_.idma(xs_s.ap(), hk.ap(), slot_u.ap()[:, i, k:k + 1], scatter=True, r=[hk, slot_u], w=[xs_s])
    barrier()
    if _STOP == 'E':
        S_.finish()
        return nc

    LIM = 7.0
    with ExitStack() as es:
        def sb(name, shape, dt):
            return Buf(name, es.enter_context(nc.sbuf_tensor(name, list(shape), dt)).ap())
        h2T = sb("h2Tf", [128, KC, BS], BF16)
        xr = [sb("xr0", [128, D], BF16), sb("xr1", [128, D], BF16)]
        STW = max(KC * 128, H2 * 512)
        st = [sb("st0", [128, STW], F32), sb("st1", [128, STW], F32), sb("st2", [128, STW], F32)]
        wgb = [sb("wgb0", [128, KC, 128], BF16), sb("wgb1", [128, KC, 128], BF16)]
        wub = [sb("wub0", [128, KC, 128], BF16), sb("wub1", [128, KC, 128], BF16)]
        wdb = [sb("wdb0", [128, H2, 512], BF16), sb("wdb1", [128, H2, 512], BF16)]
        actT = sb("actT", [128, FC, BS], BF16)
        ta = sb("ta", [128, BS], F32); tb_ = sb("tb", [128, BS], F32); tsg = sb("tsg", [128, BS], F32)
        bgb = [sb("bgb0", [128, FC], F32), sb("bgb1", [128, FC], F32)]
        bub = [sb("bub0", [128, FC], F32), sb("bub1", [128, FC], F32)]
        bdb = [sb("bdb0", [128, D], F32), sb("bdb1", [128, D], F32)]
        yc = [sb("yc0", [128, 512], F32), sb("yc1", [128, 512], F32), sb("yc2", [128, 512], F32), sb("yc3", [128, 512], F32)]
        sti = 0; fci = 0; xri = 0; yci = 0
        for b in range(NB):
            bg_ = bgb[b % 2]; bu_ = bub[b % 2]; bd_ = bdb[b % 2]
            S_.idma(bg_.ap(), bgT.ap(), bidx.ap()[:, b:b + 1], scatter=False, r=[bgT, bidx], w=[bg_])
            S_.idma(bu_.ap(), buT.ap(), bidx.ap()[:, b:b + 1], scatter=False, r=[buT, bidx], w=[bu_])
            S_.idma(bd_.ap(), b_down.ap(), bdidx.ap()[:, b:b + 1], scatter=False, r=[b_down, bdidx], w=[bd_])
            for tt in range(BS // 128):
                x_ = xr[xri % 2]; xri += 1
                S_.dma("sync", x_.ap(), xs_s.ap()[b * BS + tt * 128:b * BS + (tt + 1) * 128, :], r=[xs_s], w=[x_])
                for k4 in range(KC // 4):
                    pb = banks[6 + (k4 % 2)]
                    pbv = pb.ap().bitcast(BF16)
                    for q_ in range(4):
                        kc = k4 * 4 + q_
                        S_.op("pe", lambda e, x_=x_, kc=kc, q_=q_, pbv=pbv: e.transpose(
                            pbv[:, q_ * 128:(q_ + 1) * 128], x_.ap()[:, kc * 128:(kc + 1) * 128], identb.ap()),
                            r=[x_, identb], w=[pb])
                    eng = "act" if k4 % 2 == 0 else "dve"
                    if eng == "act":
                        S_.op("act", lambda e, k4=k4, tt=tt, pbv=pbv: e.activation(
                            h2T.ap()[:, k4 * 4:(k4 + 1) * 4, tt * 128:(tt + 1) * 128],
                            pbv[:, 0:512].rearrange("p (c t) -> p c t", t=128), AF.Copy), r=[pb], w=[h2T])
                    else:
                        S_.op("dve", lambda e, k4=k4, tt=tt, pbv=pbv: e.tensor_copy(
                            h2T.ap()[:, k4 * 4:(k4 + 1) * 4, tt * 128:(tt + 1) * 128],
                            pbv[:, 0:512].rearrange("p (c t) -> p c t", t=128)), r=[pb], w=[h2T])
            for fc in range(FC):
                psg = banks[fci % 2]; psu = banks[2 + fci % 2]
                wg_ = wgb[fci % 2]; wu_ = wub[fci % 2]; fci += 1
                sg_ = st[sti % 3]; sti += 1
                S_.idma(sg_.ap()[:, 0:KC * 128], w_gate.ap(), widx_g.ap()[:, b, fc:fc + 1], scatter=False, r=[w_gate, widx_g], w=[sg_])
                S_.op("dve", lambda e, sg_=sg_, wg_=wg_: e.tensor_copy(
                    wg_.ap(), sg_.ap()[:, 0:KC * 128].rearrange("p (kc f) -> p kc f", f=128)), r=[sg_], w=[wg_])
                su_ = st[sti % 3]; sti += 1
                S_.idma(su_.ap()[:, 0:KC * 128], w_up.ap(), widx_g.ap()[:, b, fc:fc + 1], scatter=False, r=[w_up, widx_g], w=[su_])
                S_.op("act", lambda e, su_=su_, wu_=wu_: e.activation(
                    wu_.ap(), su_.ap()[:, 0:KC * 128].rearrange("p (kc f) -> p kc f", f=128), AF.Copy), r=[su_], w=[wu_])
                for kc in range(KC):
                    S_.op("pe", lambda e, kc=kc, psg=psg, wg_=wg_: e.matmul(
                        psg.ap()[:, 0:BS], wg_.ap()[:, kc, :], h2T.ap()[:, kc, :], start=(kc == 0), stop=(kc == KC - 1)),
                        r=[wg_, h2T], w=[psg])
                for kc in range(KC):
                    S_.op("pe", lambda e, kc=kc, psu=psu, wu_=wu_: e.matmul(
                        psu.ap()[:, 0:BS], wu_.ap()[:, kc, :], h2T.ap()[:, kc, :], start=(kc == 0), stop=(kc == KC - 1)),
                        r=[wu_, h2T], w=[psu])
                S_.op("dve", lambda e, psg=psg, fc=fc, bg_=bg_: e.tensor_scalar(
                    ta.ap(), psg.ap()[:, 0:BS], bg_.ap()[:, fc:fc + 1], LIM, ALU.add, ALU.min), r=[psg, bg_], w=[ta])
                S_.op("act", lambda e: e.activation(tsg.ap(), ta.ap(), AF.Sigmoid, scale=1.702), r=[ta], w=[tsg])
                S_.op("dve", lambda e, psu=psu, fc=fc, bu_=bu_: e.tensor_scalar(
                    tb_.ap(), psu.ap()[:, 0:BS], bu_.ap()[:, fc:fc + 1], LIM, ALU.add, ALU.min), r=[psu, bu_], w=[tb_])
                S_.op("dve", lambda e: e.tensor_scalar(tb_.ap(), tb_.ap(), -LIM, 1.0, ALU.max, ALU.add), r=[tb_], w=[tb_])
                S_.op("dve", lambda e: e.tensor_tensor(ta.ap(), ta.ap(), tsg.ap(), ALU.mult), r=[ta, tsg], w=[ta])
                S_.op("dve", lambda e, fc=fc: e.tensor_tensor(actT.ap()[:, fc, :], ta.ap(), tb_.ap(), ALU.mult),
                      r=[ta, tb_], w=[actT])
            for dc in range(DC):
                for hf in range(2):
                    sd_ = st[sti % 3]; sti += 1
                    S_.idma(sd_.ap()[:, 0:H2 * 512], w_down.ap(), widx_d.ap()[:, b, dc * 2 + hf:dc * 2 + hf + 1], scatter=False,
                            r=[w_down, widx_d], w=[sd_])
                    if hf == 0:
                        S_.op("act", lambda e, sd_=sd_: e.activation(
                            wdb[0].ap(), sd_.ap()[:, 0:H2 * 512].rearrange("p (fc d) -> p fc d", d=512), AF.Copy),
                            r=[sd_], w=[wdb[0]])
                    else:
                        S_.op("dve", lambda e, sd_=sd_: e.tensor_copy(
                            wdb[1].ap(), sd_.ap()[:, 0:H2 * 512].rearrange("p (fc d) -> p fc d", d=512)),
                            r=[sd_], w=[wdb[1]])
                for tt in range(BS // 128):
                    for fc in range(FC):
                        S_.op("pe", lambda e, tt=tt, fc=fc: e.matmul(
                            banks[4 + tt].ap(), actT.ap()[:, fc, tt * 128:(tt + 1) * 128], wdb[fc // H2].ap()[:, fc % H2, :],
                            start=(fc == 0), stop=(fc == FC - 1)), r=[actT, wdb[fc // H2]], w=[banks[4 + tt]])
                for tt in range(BS // 128):
                    y_ = yc[yci % 4]; yci += 1
                    S_.op("dve", lambda e, tt=tt, dc=dc, y_=y_, bd_=bd_: e.tensor_tensor(
                        y_.ap(), banks[4 + tt].ap(), bd_.ap()[:, dc * 512:(dc + 1) * 512], ALU.add),
                        r=[banks[4 + tt], bd_], w=[y_])
                    yp = ys_parts[(dc * 512) // YW]; c0 = (dc * 512) % YW
                    S_.dma("sync", yp.ap()[b * BS + tt * 128:b * BS + (tt + 1) * 128, c0:c0 + 512], y_.ap(),
                           r=[y_], w=[yp])
    barrier()

    with ExitStack() as es:
        def sb(name, shape, dt):
            return Buf(name, es.enter_context(nc.sbuf_tensor(name, list(shape), dt)).ap())
        x1t = sb("x1t", [128, D], F32); g5b = sb("g5b", [128, D], F32); fgb = sb("fgb", [128, D], F32)
        yk = [sb("yk0", [128, D], F32), sb("yk1", [128, D], F32)]
        accs = [sb("acc0", [128, D], F32), sb("acc1", [128, D], F32)]
        ss = sb("ssG", [128, 8], F32); rs = sb("rsG", [128, 8], F32); junk = sb("junkG", [128, 512], F32)
        S_.dma("sync", g5b.ap(), bcast_rows(modrow_s.ap().tensor, D, D), r=[modrow_s], w=[g5b])
        S_.dma("sync", fgb.ap(), bcast_rows(fg_row.ap().tensor, 0, D), r=[fg_row], w=[fgb])
        yi = 0
        for i in range(NTL):
            rows = slice(i * 128, (i + 1) * 128)
            acc = accs[i % 2]
            S_.dma("sync", x1t.ap(), x1_s.ap()[rows, :], r=[x1_s], w=[x1t])
            for k in range(4):
                y_ = yk[yi % 2]; yi += 1
                for yp_i, yp in enumerate(ys_parts):
                    S_.idma(y_.ap()[:, yp_i * YW:(yp_i + 1) * YW], yp.ap(), slot_u.ap()[:, i, k:k + 1], scatter=False,
                            r=[yp, slot_u], w=[y_])
                if k == 0:
                    S_.op("dve", lambda e, y_=y_, i=i, acc=acc: e.tensor_scalar(
                        acc.ap(), y_.ap(), gk_all.ap()[:, i, 0:1], None, ALU.mult), r=[y_, gk_all], w=[acc])
                else:
                    S_.op("dve", lambda e, y_=y_, i=i, k=k, acc=acc: e.scalar_tensor_tensor(
                        acc.ap(), y_.ap(), gk_all.ap()[:, i, k:k + 1], acc.ap(), ALU.mult, ALU.add), r=[y_, gk_all, acc], w=[acc])
            S_.op("dve", lambda e, acc=acc: e.tensor_tensor(acc.ap(), acc.ap(), g5b.ap(), ALU.mult), r=[acc, g5b], w=[acc])
            S_.op("dve", lambda e, acc=acc: e.tensor_tensor(acc.ap(), acc.ap(), x1t.ap(), ALU.add), r=[acc, x1t], w=[acc])
            for ch in range(D // 512):
                S_.op("act", lambda e, ch=ch, acc=acc: e.activation(
                    junk.ap(), acc.ap()[:, ch * 512:(ch + 1) * 512], AF.Square,
                    accum_out=ss.ap()[:, ch:ch + 1]), r=[acc], w=[junk, ss])
            S_.op("dve", lambda e: e.tensor_reduce(rs.ap()[:, 1:2], ss.ap()[:, 0:D // 512], AX.X, ALU.add), r=[ss], w=[rs])
            S_.op("dve", lambda e: e.tensor_scalar(rs.ap()[:, 0:1], rs.ap()[:, 1:2], 1.0 / D, EPS, ALU.mult, ALU.add),
                  r=[rs], w=[rs])
            S_.op("act", lambda e: e.activation(rs.ap()[:, 0:1], rs.ap()[:, 0:1], AF.Sqrt), r=[rs], w=[rs])
            S_.op("dve", lambda e: e.reciprocal(rs.ap()[:, 0:1], rs.ap()[:, 0:1]), r=[rs], w=[rs])
            S_.op("act", lambda e, acc=acc: e.activation(acc.ap(), acc.ap(), AF.Copy, scale=rs.ap()[:, 0:1]), r=[acc, rs], w=[acc])
            S_.op("dve", lambda e, acc=acc: e.tensor_tensor(acc.ap(), acc.ap(), fgb.ap(), ALU.mult), r=[acc, fgb], w=[acc])
            S_.dma("sync", out.ap()[rows, :], acc.ap(), r=[acc], w=[out], is_output=True)
    barrier()
    S_.finish()
    return nc


def token_order(cfg, s):
    S, HALF = cfg["S"], cfg["HALF"]
    own = np.arange(s * HALF, (s + 1) * HALF)
    if s == 0:
        before = np.arange(S - 8, S); after = np.arange(HALF, HALF + 8); rest = np.arange(HALF + 8, S - 8)
        hm = np.array([0.0] * 8 + [1.0] * 8, np.float32)
    else:
        before = np.arange(HALF - 8, HALF); after = np.arange(0, 8); rest = np.arange(8, HALF - 8)
        hm = np.array([1.0] * 8 + [0.0] * 8, np.float32)
    return own, np.concatenate([before, after, rest]), hm


def fmaj(v, n=128):
    v = np.asarray(v, np.float32)
    return np.ascontiguousarray(v.reshape(-1, n).T)


def prep_shared(cfg, inp):
    D, E, FF, FC, PG = cfg["D"], cfg["E"], cfg["FF"], cfg["FC"], cfg["PG"]
    f = lambda a: np.ascontiguousarray(np.asarray(a, np.float32))
    sh = {}
    sh["w_ada"] = f(inp["w_ada"][0]); sh["bT_ada"] = fmaj(inp["b_ada"][0])
    sh["g1T"] = fmaj(inp["norm1_g"][0]); sh["g2T"] = fmaj(inp["norm2_g"][0])
    sh["fg_row"] = f(inp["final_g"]).reshape(1, D)
    sh["w_in"] = f(inp["w_in"][0])
    sh["gq"] = np.ascontiguousarray(np.tile(f(inp["q_norm_g"][0])[None, :], (128, 1)))
    sh["gk"] = np.ascontiguousarray(np.tile(f(inp["k_norm_g"][0])[None, :], (128, 1)))
    sh["w_pool"] = f(inp["w_pool"][0]).reshape(4 * PG, PG)
    sh["pscT"] = fmaj(inp["pool_scale"][0])
    sh["w_out"] = f(inp["w_out"][0])
    sh["w_router"] = np.ascontiguousarray(f(inp["w_router"][0]).reshape(cfg["KC"], 128, E).transpose(1, 0, 2).reshape(128, cfg["KC"] * E))
    sh["b_router"] = np.ascontiguousarray(np.tile(f(inp["b_router"][0]).reshape(1, E), (128, 1)))
    KC, DC, H2 = cfg["KC"], D // 512, FC // 2
    G = cfg["BS"]; NB = -(-4 * cfg["HALF"] // G) + E
    def relayout_gu(w):
        w = f(w).reshape(E, KC, 128, FC, 128)
        return np.ascontiguousarray(w.transpose(0, 3, 2, 1, 4)).reshape(E * FC * 128, KC * 128)
    sh["w_gate"] = relayout_gu(inp["w_gate"][0])
    sh["w_up"] = relayout_gu(inp["w_up"][0])
    wd = f(inp["w_down"][0]).reshape(E, 2, H2, 128, DC, 512)
    sh["w_down"] = np.ascontiguousarray(wd.transpose(0, 4, 1, 3, 2, 5)).reshape(E * DC * 2 * 128, H2 * 512)
    sh["bgT"] = np.ascontiguousarray(f(inp["b_gate"][0]).reshape(E, FC, 128).transpose(0, 2, 1)).reshape(E * 128, FC)
    sh["buT"] = np.ascontiguousarray(f(inp["b_up"][0]).reshape(E, FC, 128).transpose(0, 2, 1)).reshape(E * 128, FC)
    sh["b_down"] = f(inp["b_down"][0])
    p = np.arange(128, dtype=np.float32)
    sh["ustrict"] = np.ascontiguousarray((p[:, None] < p[None, :]).astype(np.float32))
    sh["iota_e"] = np.ascontiguousarray(np.tile(np.arange(E, dtype=np.float32)[None, :], (128, 1)))
    sh["iota_pf"] = np.ascontiguousarray(p[:, None] + 128.0 * np.arange(FC, dtype=np.float32)[None, :])
    sh["iota_pd"] = np.ascontiguousarray(p[:, None] + 128.0 * np.arange(DC * 2, dtype=np.float32)[None, :])
    sh["iota_p"] = np.ascontiguousarray(p[:, None])
    sh["blkthr"] = np.ascontiguousarray(np.tile((np.arange(NB, dtype=np.float32) * G)[None, :], (128, 1)))
    sh["ident"] = np.eye(128, dtype=np.float32)
    return sh


def prep_core(cfg, inp, sh, b, s):
    D, S, HALF, CTX, NT, KC = cfg["D"], cfg["S"], cfg["HALF"], cfg["CTX"], cfg["NT"], cfg["KC"]
    own, other, hm = token_order(cfg, s)
    x = np.asarray(inp["x"], np.float32); ctx = np.asarray(inp["ctx"], np.float32)
    m = dict(sh)
    m["xin"] = np.ascontiguousarray(np.concatenate([x[b][own], x[b][other], ctx[b]], axis=0))
    cT = np.stack([fmaj(np.asarray(inp["c"], np.float32)[b]), fmaj(inp["c_ctx"])], axis=-1)
    m["cT"] = np.ascontiguousarray(cT)
    tok = np.concatenate([own, other]).astype(np.float64)
    freqs = ROPE_THETA ** (-np.arange(32, dtype=np.float64) / 32.0)
    freqs32 = freqs.astype(np.float32)
    row = np.floor(tok / GRID_W).astype(np.float32); col = (tok % GRID_W).astype(np.float32)
    ang = np.concatenate([row[:, None] * freqs32[None, :], col[:, None] * freqs32[None, :]], axis=-1).astype(np.float32)
    cs = np.concatenate([np.cos(ang), np.sin(ang)], axis=-1).astype(np.float32)
    cs_ctx = np.concatenate([np.ones((CTX, 64), np.float32), np.zeros((CTX, 64), np.float32)], axis=-1)
    m["cs"] = np.ascontiguousarray(np.concatenate([cs, cs_ctx], axis=0))
    t = own
    ic = []
    for w in (2, 4, 8, 16):
        lo = np.clip(t - w // 2, 0, S); hi = np.clip(t + w // 2, 0, S)
        ic.append(1.0 / (hi - lo).astype(np.float32))
    m["invcnt"] = np.ascontiguousarray(np.stack(ic).astype(np.float32))
    m["hmask"] = hm.reshape(1, 16)
    return m


def kernel(**inputs):
    cfg = FULL
    B, S, HALF, D = cfg["B"], cfg["S"], cfg["HALF"], cfg["D"]
    inp = {k: np.asarray(v) for k, v in inputs.items()}
    sh = prep_shared(cfg, inp)
    in_maps = []
    for b in range(B):
        for s in range(2):
            in_maps.append(prep_core(cfg, inp, sh, b, s))
    nc = build_program(cfg, debug=False)
    res = run_bass_kernel_spmd(nc, in_maps, core_ids=list(range(2 * B)))
    out = np.empty((B, S, D), np.float32)
    for b in range(B):
        for s in range(2):
            out[b, s * HALF:(s + 1) * HALF] = res.results[b * 2 + s]["out"]
    return out
```

```python
import numpy as np
import concourse.bass as bass
import concourse.mybir as mybir
from concourse.bass_utils import run_bass_kernel_spmd
from contextlib import ExitStack

F32 = mybir.dt.float32
BF16 = mybir.dt.bfloat16
U32 = mybir.dt.uint32
AF = mybir.ActivationFunctionType
ALU = mybir.AluOpType
AX = mybir.AxisListType


class Buf:
    def __init__(self, name, ap):
        self.name = name
        self._ap = ap
        self.lw = None
        self.rd = []

    def ap(self):
        return self._ap


class Sched:
    def __init__(self, nc, n_dma_sems=24):
        self.nc = nc
        self.eng = {"pe": nc.tensor, "act": nc.scalar, "dve": nc.vector,
                    "pool": nc.gpsimd, "sync": nc.sync}
        self.sems = {}
        self.tick = {}
        self.waited = {k: {} for k in self.eng}
        for k in ("pe", "act", "dve", "pool"):
            self.sems[k] = nc.alloc_semaphore("s_" + k)
            self.tick[k] = 0
        self.dpool = {}
        for q in ("sync", "pool", "act"):
            lst = []
            for i in range(n_dma_sems if q != "act" else 8):
                key = "d_%s_%d" % (q, i)
                self.sems[key] = nc.alloc_semaphore(key)
                lst.append([key, 0])
            self.dpool[q] = [lst, 0]
        self.out_deps = []
        self.n_ops = 0

    def sbuf(self, name, shape, dt):
        return Buf(name, self.nc.alloc_sbuf_tensor(name, list(shape), dt).ap())

    def psum(self, name, shape, dt):
        return Buf(name, self.nc.alloc_psum_tensor(name, list(shape), dt).ap())

    def dram(self, ap, name):
        return Buf(name, ap)

    def scratch(self, name, shape, dt):
        return Buf(name, self.nc.dram_tensor(name, list(shape), dt, kind="Internal").ap())

    def _wait(self, engine, key, val):
        if key == engine and engine == "pe":
            return
        w = self.waited[engine]
        if w.get(key, 0) >= val:
            return
        self.eng[engine].wait_ge(self.sems[key], val)
        w[key] = val

    def _deps(self, r, w):
        deps = []
        for b in r:
            if b.lw is not None:
                deps.append(b.lw)
        for b in w:
            if b.lw is not None:
                deps.append(b.lw)
            deps.extend(b.rd)
        return deps

    def _mark(self, me, r, w):
        for b in r:
            b.rd.append(me)
            if len(b.rd) > 64:
                d = {}
                for k, v in b.rd:
                    d[k] = max(d.get(k, 0), v)
                b.rd = list(d.items())
        for b in w:
            b.lw = me
            b.rd = []

    def op(self, engine, fn, r=(), w=()):
        for k, v in self._deps(r, w):
            self._wait(engine, k, v)
        ins = fn(self.eng[engine])
        self.tick[engine] += 1
        ins.then_inc(self.sems[engine], 1)
        self._mark((engine, self.tick[engine]), r, w)
        self.n_ops += 1
        return ins

    def dma(self, queue, out_ap, in_ap, r=(), w=(), is_output=False, **kw):
        for k, v in self._deps(r, w):
            self._wait(queue, k, v)
        lst, idx = self.dpool[queue]
        ent = lst[idx % len(lst)]
        self.dpool[queue][1] = idx + 1
        key, cnt = ent
        if cnt > 0:
            self._wait(queue, key, 16 * cnt)
        ins = self.eng[queue].dma_start(out=out_ap, in_=in_ap, **kw)
        ent[1] = cnt + 1
        ins.then_inc(self.sems[key], 16)
        me = (key, 16 * (cnt + 1))
        self._mark(me, r, w)
        if is_output:
            self.out_deps.append(me)
        self.n_ops += 1
        return ins

    def idma(self, out_ap, in_ap, idx_ap, scatter, r=(), w=()):
        queue = "pool"
        for k, v in self._deps(r, w):
            self._wait(queue, k, v)
        lst, idx = self.dpool[queue]
        ent = lst[idx % len(lst)]
        self.dpool[queue][1] = idx + 1
        key, cnt = ent
        if cnt > 0:
            self._wait(queue, key, 16 * cnt)
        off = bass.IndirectOffsetOnAxis(ap=idx_ap, axis=0)
        if scatter:
            ins = self.nc.gpsimd.indirect_dma_start(out=out_ap, out_offset=off, in_=in_ap, in_offset=None)
        else:
            ins = self.nc.gpsimd.indirect_dma_start(out=out_ap, out_offset=None, in_=in_ap, in_offset=off)
        ent[1] = cnt + 1
        ins.then_inc(self.sems[key], 16)
        self._mark((key, 16 * (cnt + 1)), r, w)
        self.n_ops += 1
        return ins

    def finish(self):
        for q in ("sync", "pool", "act"):
            for key, cnt in self.dpool[q][0]:
                if cnt > 0:
                    self._wait("sync", key, 16 * cnt)
        for k in ("pe", "act", "dve", "pool"):
            if self.tick[k] > 0:
                self._wait("sync", k, self.tick[k])


def make_cfg(D=4096, S=4096, CTX=256, E=32, FF=1536, B=4, BS=None):
    c = dict(D=D, S=S, CTX=CTX, E=E, FF=FF, B=B)
    c["KC"] = D // 128
    c["HALF"] = S // 2
    c["NT"] = S + CTX
    c["G"] = min(512, c["HALF"])
    c["ATTN_W"] = 3 * D // 4
    c["NH"] = c["ATTN_W"] // 128
    c["NKV"] = c["NH"] // 3
    c["KVW"] = c["NKV"] * 128
    c["POOL_W"] = D // 4
    c["PG"] = c["POOL_W"] // 4
    c["IN_W"] = c["ATTN_W"] + 2 * c["KVW"] + c["POOL_W"]
    c["FC"] = FF // 128
    c["BS"] = BS if BS else c["G"]
    return c


FULL = make_cfg(BS=384)
_STOP = ''
EPS = 1e-6
ROPE_THETA = 10000.0
GRID_W = 64
TOPK = 4


def bcast_rows(buf_ap_tensor, offset, n, parts=128):
    return bass.AP(tensor=buf_ap_tensor, offset=offset, ap=[[0, parts], [1, n]])


def build_program(cfg, debug=False):
    D, KC, HALF, NT, G, CTX = cfg["D"], cfg["KC"], cfg["HALF"], cfg["NT"], cfg["G"], cfg["CTX"]
    NH, NKV, KVW, ATTN_W, POOL_W, PG, IN_W = (cfg["NH"], cfg["NKV"], cfg["KVW"], cfg["ATTN_W"],
                                             cfg["POOL_W"], cfg["PG"], cfg["IN_W"])
    E, FF, FC, S = cfg["E"], cfg["FF"], cfg["FC"], cfg["S"]
    GT_ = G // 128
    TT = NT // 128
    NG_OWN = HALF // G
    PCH = POOL_W // 128
    PGC = PG // 128
    KS = 8
    assert KC % KS == 0 and FC % 2 == 0 and CTX <= G and CTX % 128 == 0

    nc = bass.Bass("TRN2", target_bir_lowering=False)
    S_ = Sched(nc)

    def din(name, shape, dt=F32):
        return S_.dram(nc.dram_tensor(name, list(shape), dt, kind="ExternalInput").ap(), name)

    xin = din("xin", [NT, D])
    cT = din("cT", [128, KC, 2])
    w_ada = din("w_ada", [D, 6 * D])
    bT_ada = din("bT_ada", [128, 6 * KC])
    g1T = din("g1T", [128, KC])
    g2T = din("g2T", [128, KC])
    fg_row = din("fg_row", [1, D])
    w_in = din("w_in", [D, IN_W])
    gq = din("gq", [128, 128])
    gk = din("gk", [128, 128])
    w_pool = din("w_pool", [4 * PG, PG])
    pscT = din("pscT", [128, PCH])
    w_out = din("w_out", [D, D])
    w_router = din("w_router", [128, KC * E])
    b_router = din("b_router", [128, E])
    DC = D // 512
    H2 = FC // 2
    BS = cfg["BS"]
    NTL = HALF // 128
    NB = -(-4 * HALF // BS) + E
    NSLOT = NB * BS
    w_gate = din("w_gate", [E * FC * 128, KC * 128])
    bgT = din("bgT", [E * 128, FC])
    w_up = din("w_up", [E * FC * 128, KC * 128])
    buT = din("buT", [E * 128, FC])
    w_down = din("w_down", [E * DC * 2 * 128, H2 * 512])
    b_down = din("b_down", [E, D])
    ustrict = din("ustrict", [128, 128])
    iota_e = din("iota_e", [128, E])
    iota_pf = din("iota_pf", [128, FC])
    iota_pd = din("iota_pd", [128, DC * 2])
    iota_p = din("iota_p", [128, 1])
    blkthr = din("blkthr", [128, NB])
    cs = din("cs", [NT, 128])
    ident_in = din("ident", [128, 128])
    invcnt = din("invcnt", [4, HALF])
    hmask = din("hmask", [1, 16])

    out = S_.dram(nc.dram_tensor("out", [HALF, D], F32, kind="ExternalOutput").ap(), "out")

    dbg_kind = "ExternalOutput" if debug else "Internal"

    def scr(name, shape, dt):
        return S_.dram(nc.dram_tensor(name, list(shape), dt, kind=dbg_kind).ap(), name)

    qT_s = scr("qT_s", [NH, 128, HALF], BF16)
    kT_s = scr("kT_s", [NKV, 128, NT], BF16)
    v_s = scr("v_s", [NT, KVW], BF16)
    uT_s = scr("uT_s", [PCH, 128, HALF + 16], F32)
    mixT_s = scr("mixT_s", [KC, 128, HALF], BF16)
    x1_s = scr("x1_s", [HALF, D], F32)
    h2tok_s = scr("h2tok_s", [HALF, D], BF16)
    xs_s = scr("xs_s", [NSLOT, D], BF16)
    NYS = 2
    YW = D // NYS
    ys_parts = [scr("ys_s%d" % i, [NSLOT, YW], F32) for i in range(NYS)]
    modrow_s = scr("modrow_s", [4, D], F32)

    banks = [S_.psum("bank%d" % i, [128, 512], F32) for i in range(8)]

    ident = S_.sbuf("ident_sb", [128, 128], F32)
    identb = S_.sbuf("identb", [128, 128], BF16)
    modT = S_.sbuf("modT", [128, 6 * KC, 2], F32)
    s1 = S_.sbuf("s1", [128, KC, 2], F32)
    s2 = S_.sbuf("s2", [128, KC], F32)
    slot_u = S_.sbuf("slot_u", [128, NTL, 4], U32)
    gk_all = S_.sbuf("gk_all", [128, NTL, 4], F32)
    widx_g = S_.sbuf("widx_g", [128, NB, FC], U32)
    widx_d = S_.sbuf("widx_d", [128, NB, DC * 2], U32)
    bidx = S_.sbuf("bidx", [128, NB], U32)
    bdidx = S_.sbuf("bdidx", [128, NB], U32)
    S_.dma("sync", ident.ap(), ident_in.ap(), r=[ident_in], w=[ident])
    S_.op("dve", lambda e: e.tensor_copy(identb.ap(), ident.ap()), r=[ident], w=[identb])

    def barrier():
        keys = []
        for k in ("pe", "act", "dve", "pool"):
            if S_.tick[k] > 0:
                keys.append((k, S_.tick[k]))
        for q in ("sync", "pool", "act"):
            for key, cnt in S_.dpool[q][0]:
                if cnt > 0:
                    keys.append((key, 16 * cnt))
        for eng in ("pe", "act", "dve", "pool", "sync"):
            for k, v in keys:
                if k != eng:
                    S_._wait(eng, k, v)

    def rstd_from_ss(ss, rstd, n, inv_n):
        S_.op("dve", lambda e: e.tensor_scalar(rstd.ap()[:, 0:n], ss.ap()[:, 0:n], inv_n, EPS, ALU.mult, ALU.add),
              r=[ss], w=[rstd])
        S_.op("act", lambda e: e.activation(rstd.ap()[:, 0:n], rstd.ap()[:, 0:n], AF.Sqrt), r=[rstd], w=[rstd])
        S_.op("dve", lambda e: e.reciprocal(rstd.ap()[:, 0:n], rstd.ap()[:, 0:n]), r=[rstd], w=[rstd])

    with ExitStack() as es:
        def sb(name, shape, dt):
            return Buf(name, es.enter_context(nc.sbuf_tensor(name, list(shape), dt)).ap())
        sc = sb("sc", [128, KC, 2], F32)
        aw = [sb("adaw0", [128, KC, 128], F32), sb("adaw1", [128, KC, 128], F32)]
        sm = sb("smallT", [128, 6 * KC], F32); rowt = sb("rowt", [KC, 128], F32)
        S_.dma("sync", sc.ap(), cT.ap(), r=[cT], w=[sc])
        S_.op("act", lambda e: e.activation(sc.ap(), sc.ap(), AF.Silu), r=[sc], w=[sc])
        S_.dma("sync", sm.ap(), bT_ada.ap(), r=[bT_ada], w=[sm])
        pm = banks[0]
        wv = w_ada.ap().rearrange("(kc p) n -> p kc n", p=128)
        for j in range(6 * KC):
            a = aw[j % 2]
            S_.dma("sync", a.ap(), wv[:, :, j * 128:(j + 1) * 128], r=[w_ada], w=[a])
            for kc in range(KC):
                S_.op("pe", lambda e, a=a, kc=kc, j=j: e.matmul(
                    pm.ap()[:, 2 * j:2 * j + 2], a.ap()[:, kc, :], sc.ap()[:, kc, :],
                    start=(kc == 0), stop=(kc == KC - 1)), r=[a, sc], w=[pm])
        S_.op("dve", lambda e: e.tensor_tensor(
            modT.ap(), pm.ap()[:, 0:12 * KC].rearrange("p (j m) -> p j m", m=2),
            sm.ap().unsqueeze(2).broadcast_to([128, 6 * KC, 2]), ALU.add), r=[pm, sm], w=[modT])
        S_.dma("sync", sm.ap()[:, 0:KC], g1T.ap(), r=[g1T], w=[sm])
        S_.dma("sync", sm.ap()[:, KC:2 * KC], g2T.ap(), r=[g2T], w=[sm])
        S_.op("dve", lambda e: e.tensor_scalar(s1.ap(), modT.ap()[:, KC:2 * KC, :], 1.0, None, ALU.add),
              r=[modT], w=[s1])
        S_.op("dve", lambda e: e.tensor_tensor(
            s1.ap(), s1.ap(), sm.ap()[:, 0:KC].unsqueeze(2).broadcast_to([128, KC, 2]), ALU.mult),
            r=[s1, sm], w=[s1])
        S_.op("dve", lambda e: e.tensor_scalar(s2.ap(), modT.ap()[:, 4 * KC:5 * KC, 0], 1.0, None, ALU.add),
              r=[modT], w=[s2])
        S_.op("dve", lambda e: e.tensor_tensor(s2.ap(), s2.ap(), sm.ap()[:, KC:2 * KC], ALU.mult),
              r=[s2, sm], w=[s2])
        row_srcs = [(modT, modT.ap()[:, 2 * KC:3 * KC, 0]), (modT, modT.ap()[:, 5 * KC:6 * KC, 0]),
                    (s2, s2.ap()), (modT, modT.ap()[:, 3 * KC:4 * KC, 0])]
        for i, (sb_src, src_ap) in enumerate(row_srcs):
            S_.op("dve", lambda e, src_ap=src_ap: e.tensor_copy(sm.ap()[:, 2 * KC:3 * KC], src_ap),
                  r=[sb_src], w=[sm])
            S_.op("pe", lambda e: e.transpose(banks[1].ap()[0:KC, 0:128], sm.ap()[:, 2 * KC:3 * KC], ident.ap()),
                  r=[sm, ident], w=[banks[1]])
            S_.op("dve", lambda e: e.tensor_copy(rowt.ap(), banks[1].ap()[0:KC, 0:128]), r=[banks[1]], w=[rowt])
            S_.dma("pool", modrow_s.ap()[i:i + 1, :].rearrange("o (kc p) -> (o kc) p", p=128), rowt.ap(),
                   r=[rowt], w=[modrow_s])
    barrier()

    groups = []
    for g in range(NG_OWN):
        groups.append((g * G, G, "own"))
    for g in range(NG_OWN):
        groups.append((HALF + g * G, G, "other0" if g == 0 else "other"))
    groups.append((S, CTX, "ctx"))
    QC = ATTN_W // 512
    KCH = KVW // 512
    UCH = POOL_W // 512
    assert ATTN_W % 512 == 0 and KVW % 512 == 0 and POOL_W % 512 == 0

    with ExitStack() as es:
        def sb(name, shape, dt):
            return Buf(name, es.enter_context(nc.sbuf_tensor(name, list(shape), dt)).ap())
        hT = sb("hT", [128, KC, G], BF16)
        xt = [sb("xt0", [128, D], F32), sb("xt1", [128, D], F32)]
        ws = [sb("ws0", [128, KS, 512], F32), sb("ws1", [128, KS, 512], F32)]
        wb = [sb("wb0", [128, KS, 512], BF16), sb("wb1", [128, KS, 512], BF16)]
        ss = sb("ssA", [128, 8], F32); rs = sb("rsA", [128, 8], F32)
        tmpA = sb("tmpA", [128, 512], F32); tmpB = sb("tmpB", [128, 512], F32)
        tmpC = sb("tmpC", [128, 256], F32); tmpD = sb("tmpD", [128, 256], F32)
        qr = sb("qr", [128, 512], BF16); qTg = sb("qTg", [128, 4, G], BF16)
        vg = sb("vg", [128, GT_, 512], BF16); ug = sb("ug", [128, 4, G], F32)
        gqs = sb("gqs", [128, 128], F32); gks = sb("gks", [128, 128], F32)
        cst = sb("cst", [128, GT_, 128], F32)
        S_.dma("sync", gqs.ap(), gq.ap(), r=[gq], w=[gqs])
        S_.dma("sync", gks.ap(), gk.ap(), r=[gk], w=[gks])
        win_v = w_in.ap().rearrange("(kc p) n -> p kc n", p=128)
        slab_i = [0]
        xi = [0]

        for (t0, ng, kind) in groups:
            ntt = ng // 128
            mcol = 1 if kind == "ctx" else 0
            S_.dma("sync", cst.ap()[:, 0:ntt, :], cs.ap()[t0:t0 + ng, :].rearrange("(t p) c -> p t c", p=128),
                   r=[cs], w=[cst])
            for tt in range(ntt):
                x_ = xt[xi[0] % 2]; xi[0] += 1
                S_.dma("sync", x_.ap(), xin.ap()[t0 + tt * 128:t0 + (tt + 1) * 128, :], r=[xin], w=[x_])
                S_.op("act", lambda e, x_=x_: e.activation(tmpA.ap()[:, 0:512], x_.ap()[:, 0:512], AF.Square),
                      r=[x_], w=[tmpA]) if False else None
                for ch in range(D // 512):
                    S_.op("act", lambda e, x_=x_, ch=ch: e.activation(
                        tmpA.ap(), x_.ap()[:, ch * 512:(ch + 1) * 512], AF.Square,
                        accum_out=ss.ap()[:, ch:ch + 1]), r=[x_], w=[tmpA, ss])
                S_.op("dve", lambda e: e.tensor_reduce(ss.ap()[:, 7:8] if D // 512 < 8 else rs.ap()[:, 1:2],
                                                       ss.ap()[:, 0:D // 512], AX.X, ALU.add), r=[ss], w=[rs, ss])
                src = ss if D // 512 < 8 else rs
                sc_col = 7 if D // 512 < 8 else 1
                S_.op("dve", lambda e, src=src, sc_col=sc_col: e.tensor_scalar(
                    rs.ap()[:, 0:1], src.ap()[:, sc_col:sc_col + 1], 1.0 / D, EPS, ALU.mult, ALU.add), r=[src], w=[rs])
                S_.op("act", lambda e: e.activation(rs.ap()[:, 0:1], rs.ap()[:, 0:1], AF.Sqrt), r=[rs], w=[rs])
                S_.op("dve", lambda e: e.reciprocal(rs.ap()[:, 0:1], rs.ap()[:, 0:1]), r=[rs], w=[rs])
                S_.op("act", lambda e, x_=x_: e.activation(x_.ap(), x_.ap(), AF.Copy, scale=rs.ap()[:, 0:1]),
                      r=[x_, rs], w=[x_])
                for kc in range(KC):
                    pb = banks[kc % 2]
                    S_.op("pe", lambda e, x_=x_, kc=kc, pb=pb: e.transpose(
                        pb.ap()[:, 0:128], x_.ap()[:, kc * 128:(kc + 1) * 128], ident.ap()), r=[x_, ident], w=[pb])
                    S_.op("act", lambda e, kc=kc, pb=pb, tt=tt, mcol=mcol: e.activation(
                        hT.ap()[:, kc, tt * 128:(tt + 1) * 128], pb.ap()[:, 0:128], AF.Identity,
                        bias=modT.ap()[:, kc, mcol:mcol + 1], scale=s1.ap()[:, kc, mcol:mcol + 1]),
                        r=[pb, modT, s1], w=[hT])

            def proj_chunk(col0, mode, hidx):
                for sl in range(KC // KS):
                    w_s = ws[slab_i[0] % 2]; w_b = wb[slab_i[0] % 2]; slab_i[0] += 1
                    S_.dma("sync", w_s.ap(), win_v[:, sl * KS:(sl + 1) * KS, col0:col0 + 512], r=[w_in], w=[w_s])
                    S_.op("dve", lambda e, w_s=w_s, w_b=w_b: e.tensor_copy(w_b.ap(), w_s.ap()), r=[w_s], w=[w_b])
                    for k8 in range(KS):
                        kc = sl * KS + k8
                        for j in range(4 if mode == "u" else ntt):
                            pb = banks[2 + j]
                            if mode == "u":
                                S_.op("pe", lambda e, pb=pb, j=j, k8=k8, kc=kc, w_b=w_b: e.matmul(
                                    pb.ap()[:, 0:ng], w_b.ap()[:, k8, j * 128:(j + 1) * 128], hT.ap()[:, kc, 0:ng],
                                    start=(kc == 0), stop=(kc == KC - 1)), r=[w_b, hT], w=[pb])
                            else:
                                S_.op("pe", lambda e, pb=pb, j=j, k8=k8, kc=kc, w_b=w_b: e.matmul(
                                    pb.ap(), hT.ap()[:, kc, j * 128:(j + 1) * 128], w_b.ap()[:, k8, :],
                                    start=(kc == 0), stop=(kc == KC - 1)), r=[w_b, hT], w=[pb])
                if mode == "u":
                    for j in range(4):
                        S_.op("act", lambda e, j=j: e.activation(ug.ap()[:, j, 0:ng], banks[2 + j].ap()[:, 0:ng], AF.Copy),
                              r=[banks[2 + j]], w=[ug])
                    n_st = ng if kind == "own" else 16
                    S_.dma("pool", uT_s.ap()[hidx * 4:(hidx + 1) * 4, :, t0 if kind == "own" else HALF:(t0 if kind == "own" else HALF) + n_st]
                           .rearrange("c p t -> p c t"), ug.ap()[:, :, 0:n_st], r=[ug], w=[uT_s])
                    return
                if mode == "v":
                    for j in range(ntt):
                        S_.op("act", lambda e, j=j: e.activation(vg.ap()[:, j, :], banks[2 + j].ap(), AF.Copy),
                              r=[banks[2 + j]], w=[vg])
                    S_.dma("pool", v_s.ap()[t0:t0 + ng, hidx * 512:(hidx + 1) * 512].rearrange("(t p) c -> p t c", p=128),
                           vg.ap()[:, 0:ntt, :], r=[vg], w=[v_s])
                    return
                gg = gqs if mode == "q" else gks
                for j in range(ntt):
                    pb = banks[2 + j]
                    S_.op("act", lambda e, pb=pb: e.activation(tmpA.ap(), pb.ap(), AF.Square), r=[pb], w=[tmpA])
                    S_.op("dve", lambda e: e.tensor_reduce(ss.ap()[:, 0:4], tmpA.ap().rearrange("p (h d) -> p h d", d=128),
                                                           AX.X, ALU.add), r=[tmpA], w=[ss])
                    rstd_from_ss(ss, rs, 4, 1.0 / 128)
                    S_.op("dve", lambda e, pb=pb: e.tensor_tensor(
                        tmpB.ap().rearrange("p (h d) -> p h d", d=128), pb.ap().rearrange("p (h d) -> p h d", d=128),
                        rs.ap()[:, 0:4].unsqueeze(2).broadcast_to([128, 4, 128]), ALU.mult), r=[pb, rs], w=[tmpB])
                    S_.op("dve", lambda e, gg=gg: e.tensor_tensor(
                        tmpB.ap().rearrange("p (h d) -> p h d", d=128), tmpB.ap().rearrange("p (h d) -> p h d", d=128),
                        gg.ap().unsqueeze(1).broadcast_to([128, 4, 128]), ALU.mult), r=[tmpB, gg], w=[tmpB])
                    tb = tmpB.ap().rearrange("p (h d) -> p h d", d=128)
                    x1v, x2v = tb[:, :, 0:64], tb[:, :, 64:128]
                    cosv = cst.ap()[:, j, 0:64].unsqueeze(1).broadcast_to([128, 4, 64])
                    sinv = cst.ap()[:, j, 64:128].unsqueeze(1).broadcast_to([128, 4, 64])
                    cv = tmpC.ap().rearrange("p (h d) -> p h d", d=64)
                    dv = tmpD.ap().rearrange("p (h d) -> p h d", d=64)
                    qv = qr.ap().rearrange("p (h d) -> p h d", d=128)
                    S_.op("dve", lambda e: e.tensor_tensor(cv, x1v, cosv, ALU.mult), r=[tmpB, cst], w=[tmpC])
                    S_.op("dve", lambda e: e.tensor_tensor(dv, x2v, sinv, ALU.mult), r=[tmpB, cst], w=[tmpD])
                    S_.op("dve", lambda e: e.tensor_tensor(qv[:, :, 0:64], cv, dv, ALU.subtract), r=[tmpC, tmpD], w=[qr])
                    S_.op("dve", lambda e: e.tensor_tensor(cv, x1v, sinv, ALU.mult), r=[tmpB, cst], w=[tmpC])
                    S_.op("dve", lambda e: e.tensor_tensor(dv, x2v, cosv, ALU.mult), r=[tmpB, cst], w=[tmpD])
                    S_.op("dve", lambda e: e.tensor_tensor(qv[:, :, 64:128], cv, dv, ALU.add), r=[tmpC, tmpD], w=[qr])
                    tp = banks[6 + (j % 2)]
                    tpv = tp.ap().bitcast(BF16)
                    for hd in range(4):
                        S_.op("pe", lambda e, hd=hd, tpv=tpv, tp=tp: e.transpose(
                            tpv[:, hd * 128:(hd + 1) * 128], qr.ap()[:, hd * 128:(hd + 1) * 128], identb.ap()),
                            r=[qr, identb], w=[tp])
                    S_.op("act", lambda e, tpv=tpv, tp=tp, j=j: e.activation(
                        qTg.ap()[:, :, j * 128:(j + 1) * 128], tpv[:, 0:512].rearrange("p (h t) -> p h t", t=128), AF.Copy),
                        r=[tp], w=[qTg])
                if mode == "q":
                    S_.dma("pool", qT_s.ap()[hidx * 4:(hidx + 1) * 4, :, t0:t0 + ng].rearrange("h p t -> p h t"),
                           qTg.ap()[:, :, 0:ng], r=[qTg], w=[qT_s])
                else:
                    S_.dma("pool", kT_s.ap()[hidx * 4:(hidx + 1) * 4, :, t0:t0 + ng].rearrange("h p t -> p h t"),
                           qTg.ap()[:, :, 0:ng], r=[qTg], w=[kT_s])

            if kind == "own":
                for qc in range(QC):
                    proj_chunk(qc * 512, "q", qc)
            for kc_ in range(KCH):
                proj_chunk(ATTN_W + kc_ * 512, "k", kc_)
            for vc in range(KCH):
                proj_chunk(ATTN_W + KVW + vc * 512, "v", vc)
            if kind in ("own", "other0"):
                for uc in range(UCH):
                    proj_chunk(ATTN_W + 2 * KVW + uc * 512, "u", uc)
    barrier()

    ATT_SCALE = 128.0 ** -0.5
    with ExitStack() as es:
        def sb(name, shape, dt):
            return Buf(name, es.enter_context(nc.sbuf_tensor(name, list(shape), dt)).ap())
        KT = sb("KT", [128, NT], BF16); Vh = sb("Vh", [128, TT, 130], BF16)
        QT = [sb("QT0", [128, G], BF16), sb("QT1", [128, G], BF16)]
        PT = [sb("PT0", [128, G], BF16), sb("PT1", [128, G], BF16)]
        ob = [sb("ob0", [128, 128], BF16), sb("ob1", [128, 128], BF16)]
        rinv = [sb("rinv0", [128, 1], F32), sb("rinv1", [128, 1], F32)]
        oT = [sb("oT0", [128, G], BF16), sb("oT1", [128, G], BF16)]
        S_.op("dve", lambda e: e.memset(Vh.ap()[:, :, 128:130], 1.0), w=[Vh])
        qi = 0
        for h in range(NKV):
            S_.dma("sync", KT.ap(), kT_s.ap()[h], r=[kT_s], w=[KT])
            S_.dma("sync", Vh.ap()[:, :, 0:128], v_s.ap()[:, h * 128:(h + 1) * 128].rearrange("(t p) c -> p t c", p=128),
                   r=[v_s], w=[Vh])
            for gi in range(3):
                n = h * 3 + gi
                for qg in range(NG_OWN):
                    Q = QT[qi % 2]; o_T = oT[qi % 2]; qi += 1
                    S_.dma("sync", Q.ap(), qT_s.ap()[n, :, qg * G:(qg + 1) * G], r=[qT_s], w=[Q])
                    def qk(kt, Q=Q):
                        sbk = banks[kt % 2]
                        S_.op("pe", lambda e, sbk=sbk, kt=kt, Q=Q: e.matmul(
                            sbk.ap()[:, 0:G], KT.ap()[:, kt * 128:(kt + 1) * 128], Q.ap(), start=True, stop=True),
                            r=[KT, Q], w=[sbk])
                    qk(0)
                    for kt in range(TT):
                        sbk = banks[kt % 2]; P = PT[kt % 2]
                        S_.op("act", lambda e, sbk=sbk, P=P: e.activation(P.ap(), sbk.ap()[:, 0:G], AF.Exp, scale=ATT_SCALE),
                              r=[sbk], w=[P])
                        if kt + 1 < TT:
                            qk(kt + 1)
                        for qs in range(GT_):
                            S_.op("pe", lambda e, qs=qs, kt=kt, P=P: e.matmul(
                                banks[2 + qs].ap()[:, 0:129], P.ap()[:, qs * 128:(qs + 1) * 128], Vh.ap()[:, kt, 0:129],
                                start=(kt == 0), stop=(kt == TT - 1)), r=[P, Vh], w=[banks[2 + qs]])
                    tpv = banks[6].ap().bitcast(BF16)
                    for qs in range(GT_):
                        ri = rinv[qs % 2]; o_ = ob[qs % 2]
                        S_.op("dve", lambda e, ri=ri, qs=qs: e.reciprocal(ri.ap(), banks[2 + qs].ap()[:, 128:129]),
                              r=[banks[2 + qs]], w=[ri])
                        S_.op("act", lambda e, ri=ri, o_=o_, qs=qs: e.activation(
                            o_.ap(), banks[2 + qs].ap()[:, 0:128], AF.Copy, scale=ri.ap()), r=[banks[2 + qs], ri], w=[o_])
                        S_.op("pe", lambda e, o_=o_, qs=qs: e.transpose(tpv[:, qs * 128:(qs + 1) * 128], o_.ap(), identb.ap()),
                              r=[o_, identb], w=[banks[6]])
                    S_.op("dve", lambda e, o_T=o_T: e.tensor_copy(o_T.ap(), tpv[:, 0:G]), r=[banks[6]], w=[o_T])
                    S_.dma("pool", mixT_s.ap()[n, :, qg * G:(qg + 1) * G], o_T.ap(), r=[o_T], w=[mixT_s])
    barrier()

    with ExitStack() as es:
        def sb(name, shape, dt):
            return Buf(name, es.enter_context(nc.sbuf_tensor(name, list(shape), dt)).ap())
        L = HALF + 16
        Up = sb("Up", [128, L], F32); PA = sb("PA", [128, L], F32); PB = sb("PB", [128, L], F32)
        icn = sb("icn", [128, HALF], F32); dT = sb("dT", [128, PGC, HALF], BF16)
        wpf = sb("wpf", [128, PGC, PG], F32); wpb = sb("wpb", [128, PGC, PG], BF16)
        hmt = sb("hmt", [128, 16], F32); psc = sb("psc", [128, PCH], F32)
        yb = [sb("yb0", [128, G], BF16), sb("yb1", [128, G], BF16)]
        S_.dma("sync", hmt.ap(), bcast_rows(hmask.ap().tensor, 0, 16), r=[hmask], w=[hmt])
        S_.dma("sync", psc.ap(), pscT.ap(), r=[pscT], w=[psc])
        yi = 0
        for gi, w in enumerate((2, 4, 8, 16)):
            S_.dma("sync", icn.ap(), bcast_rows(invcnt.ap().tensor, gi * HALF, HALF), r=[invcnt], w=[icn])
            S_.dma("sync", wpf.ap(), w_pool.ap()[gi * PG:(gi + 1) * PG, :].rearrange("(cc p) e -> p cc e", p=128),
                   r=[w_pool], w=[wpf])
            S_.op("dve", lambda e: e.tensor_copy(wpb.ap(), wpf.ap()), r=[wpf], w=[wpb])
            for cc in range(PGC):
                ch = gi * PGC + cc
                S_.dma("sync", Up.ap()[:, 0:8], uT_s.ap()[ch, :, HALF:HALF + 8], r=[uT_s], w=[Up])
                S_.dma("sync", Up.ap()[:, 8:8 + HALF], uT_s.ap()[ch, :, 0:HALF], r=[uT_s], w=[Up])
                S_.dma("sync", Up.ap()[:, 8 + HALF:L], uT_s.ap()[ch, :, HALF + 8:HALF + 16], r=[uT_s], w=[Up])
                S_.op("dve", lambda e: e.tensor_tensor(Up.ap()[:, 0:8], Up.ap()[:, 0:8], hmt.ap()[:, 0:8], ALU.mult),
                      r=[Up, hmt], w=[Up])
                S_.op("dve", lambda e: e.tensor_tensor(Up.ap()[:, 8 + HALF:L], Up.ap()[:, 8 + HALF:L], hmt.ap()[:, 8:16], ALU.mult),
                      r=[Up, hmt], w=[Up])
                S_.op("dve", lambda e: e.tensor_tensor(PA.ap()[:, 1:L], Up.ap()[:, 0:L - 1], Up.ap()[:, 1:L], ALU.add),
                      r=[Up], w=[PA])
                fin = PA
                if w >= 4:
                    S_.op("dve", lambda e: e.tensor_tensor(PB.ap()[:, 2:L - 1], PA.ap()[:, 1:L - 2], PA.ap()[:, 3:L], ALU.add),
                          r=[PA], w=[PB])
                    fin = PB
                if w >= 8:
                    S_.op("dve", lambda e: e.tensor_tensor(PA.ap()[:, 4:L - 3], PB.ap()[:, 2:L - 5], PB.ap()[:, 6:L - 1], ALU.add),
                          r=[PB], w=[PA])
                    fin = PA
                if w >= 16:
                    S_.op("dve", lambda e: e.tensor_tensor(PB.ap()[:, 8:L - 7], PA.ap()[:, 4:L - 11], PA.ap()[:, 12:L - 3], ALU.add),
                          r=[PA], w=[PB])
                    fin = PB
                oth = PA if fin is PB else PB
                S_.op("dve", lambda e, fin=fin, oth=oth: e.tensor_tensor(
                    oth.ap()[:, 8:8 + HALF], fin.ap()[:, 8:8 + HALF], icn.ap(), ALU.mult), r=[fin, icn], w=[oth])
                S_.op("dve", lambda e, oth=oth, cc=cc: e.tensor_tensor(
                    dT.ap()[:, cc, :], oth.ap()[:, 8:8 + HALF], Up.ap()[:, 8:8 + HALF], ALU.subtract), r=[oth, Up], w=[dT])
            for ec in range(PGC):
                for tg in range(NG_OWN):
                    pb = banks[yi % 2]; y_ = yb[yi % 2]; yi += 1
                    for cc in range(PGC):
                        S_.op("pe", lambda e, pb=pb, cc=cc, ec=ec, tg=tg: e.matmul(
                            pb.ap()[:, 0:G], wpb.ap()[:, cc, ec * 128:(ec + 1) * 128], dT.ap()[:, cc, tg * G:(tg + 1) * G],
                            start=(cc == 0), stop=(cc == PGC - 1)), r=[wpb, dT], w=[pb])
                    co = gi * PGC + ec
                    S_.op("act", lambda e, pb=pb, y_=y_, co=co: e.activation(
                        y_.ap(), pb.ap()[:, 0:G], AF.Copy, scale=psc.ap()[:, co:co + 1]), r=[pb, psc], w=[y_])
                    S_.dma("pool", mixT_s.ap()[NH + co, :, tg * G:(tg + 1) * G], y_.ap(), r=[y_], w=[mixT_s])
    barrier()

    with ExitStack() as es:
        def sb(name, shape, dt):
            return Buf(name, es.enter_context(nc.sbuf_tensor(name, list(shape), dt)).ap())
        mT = sb("mT", [128, KC, G], BF16)
        h2f = sb("h2f", [128, KC, 128], F32)
        xt = [sb("xe0", [128, D], F32)]
        s2row = sb("s2row", [128, D], F32); t2row = sb("t2row", [128, D], F32)
        hk = sb("hk", [128, D], BF16)
        i8 = sb("i8", [128, 8], U32); e4 = sb("e4", [128, 4], F32)
        msk_all = sb("msk_all", [128, NTL, E], F32); rank_all = sb("rank_all", [128, NTL, E], F32)
        i8f_all = sb("i8f_all", [128, NTL, 4], F32)
        ones128 = sb("ones128", [128, 128], F32); ust = sb("ust", [128, 128], F32)
        S_.op("dve", lambda e: e.memset(ones128.ap(), 1.0), w=[ones128])
        S_.dma("sync", ust.ap(), ustrict.ap(), r=[ustrict], w=[ust])
        S_.dma("sync", s2row.ap(), bcast_rows(modrow_s.ap().tensor, 2 * D, D), r=[modrow_s], w=[s2row])
        S_.dma("sync", t2row.ap(), bcast_rows(modrow_s.ap().tensor, 3 * D, D), r=[modrow_s], w=[t2row])
        ws = [sb("wse0", [128, KS, 512], F32), sb("wse1", [128, KS, 512], F32)]
        wb = [sb("wbe0", [128, KS, 512], BF16), sb("wbe1", [128, KS, 512], BF16)]
        g2b = sb("g2b", [128, D], F32)
        xc = [sb("xc0", [128, 512], F32), sb("xc1", [128, 512], F32)]
        wr = sb("wr", [128, KC, E], F32); brb = sb("brb", [128, E], F32)
        ss = sb("ssE", [128, 8], F32); rs = sb("rsE", [128, 8], F32); tmpA = sb("tmpE", [128, 512], F32)
        lg = sb("lg", [128, E], F32); m8 = sb("m8", [128, 8], F32); msk = sb("msk", [128, E], F32)
        nm = sb("nm", [128, 1], F32)
        S_.dma("sync", g2b.ap(), bcast_rows(modrow_s.ap().tensor, 0, D), r=[modrow_s], w=[g2b])
        S_.dma("sync", wr.ap(), w_router.ap().rearrange("p (kc e) -> p kc e", e=E), r=[w_router], w=[wr])
        S_.dma("sync", brb.ap(), b_router.ap(), r=[b_router], w=[brb])
        wout_v = w_out.ap().rearrange("(kc p) n -> p kc n", p=128)
        si = 0; xi_ = 0; ci = 0
        for g in range(NG_OWN):
            t0 = g * G
            S_.dma("sync", mT.ap(), mixT_s.ap()[:, :, t0:t0 + G].rearrange("c p t -> p c t"), r=[mixT_s], w=[mT])
            for dc in range(D // 512):
                for sl in range(KC // KS):
                    w_s = ws[si % 2]; w_b = wb[si % 2]; si += 1
                    S_.dma("sync", w_s.ap(), wout_v[:, sl * KS:(sl + 1) * KS, dc * 512:(dc + 1) * 512], r=[w_out], w=[w_s])
                    S_.op("dve", lambda e, w_s=w_s, w_b=w_b: e.tensor_copy(w_b.ap(), w_s.ap()), r=[w_s], w=[w_b])
                    for k8 in range(KS):
                        kc = sl * KS + k8
                        for j in range(GT_):
                            S_.op("pe", lambda e, j=j, k8=k8, kc=kc, w_b=w_b: e.matmul(
                                banks[2 + j].ap(), mT.ap()[:, kc, j * 128:(j + 1) * 128], w_b.ap()[:, k8, :],
                                start=(kc == 0), stop=(kc == KC - 1)), r=[w_b, mT], w=[banks[2 + j]])
                for j in range(GT_):
                    x_c = xc[ci % 2]; ci += 1
                    rows = slice(t0 + j * 128, t0 + (j + 1) * 128)
                    S_.dma("sync", x_c.ap(), xin.ap()[rows, dc * 512:(dc + 1) * 512], r=[xin], w=[x_c])
                    S_.op("dve", lambda e, j=j, dc=dc: e.tensor_tensor(
                        tmpA.ap(), banks[2 + j].ap(), g2b.ap()[:, dc * 512:(dc + 1) * 512], ALU.mult),
                        r=[banks[2 + j], g2b], w=[tmpA])
                    S_.op("dve", lambda e, x_c=x_c: e.tensor_tensor(x_c.ap(), x_c.ap(), tmpA.ap(), ALU.add),
                          r=[x_c, tmpA], w=[x_c])
                    S_.dma("pool", x1_s.ap()[rows, dc * 512:(dc + 1) * 512], x_c.ap(), r=[x_c], w=[x1_s])
            for tt in range(GT_):
                ti = g * GT_ + tt
                x_ = xt[0]
                rows = slice(t0 + tt * 128, t0 + (tt + 1) * 128)
                S_.dma("sync", x_.ap(), x1_s.ap()[rows, :], r=[x1_s], w=[x_])
                for ch in range(D // 512):
                    S_.op("act", lambda e, x_=x_, ch=ch: e.activation(
                        tmpA.ap(), x_.ap()[:, ch * 512:(ch + 1) * 512], AF.Square,
                        accum_out=ss.ap()[:, ch:ch + 1]), r=[x_], w=[tmpA, ss])
                S_.op("dve", lambda e: e.tensor_reduce(rs.ap()[:, 1:2], ss.ap()[:, 0:D // 512], AX.X, ALU.add), r=[ss], w=[rs])
                S_.op("dve", lambda e: e.tensor_scalar(rs.ap()[:, 0:1], rs.ap()[:, 1:2], 1.0 / D, EPS, ALU.mult, ALU.add),
                      r=[rs], w=[rs])
                S_.op("act", lambda e: e.activation(rs.ap()[:, 0:1], rs.ap()[:, 0:1], AF.Sqrt), r=[rs], w=[rs])
                S_.op("dve", lambda e: e.reciprocal(rs.ap()[:, 0:1], rs.ap()[:, 0:1]), r=[rs], w=[rs])
                S_.op("act", lambda e, x_=x_: e.activation(x_.ap(), x_.ap(), AF.Copy, scale=rs.ap()[:, 0:1]),
                      r=[x_, rs], w=[x_])
                for ch in range(D // 512):
                    cs_ = slice(ch * 512, (ch + 1) * 512)
                    S_.op("dve", lambda e, cs_=cs_: e.tensor_tensor(tmpA.ap(), x_.ap()[:, cs_], s2row.ap()[:, cs_], ALU.mult),
                          r=[x_, s2row], w=[tmpA])
                    S_.op("dve", lambda e, cs_=cs_: e.tensor_tensor(hk.ap()[:, cs_], tmpA.ap(), t2row.ap()[:, cs_], ALU.add),
                          r=[tmpA, t2row], w=[hk])
                S_.dma("pool", h2tok_s.ap()[rows, :], hk.ap(), r=[hk], w=[h2tok_s])
                for kc in range(KC):
                    pb = banks[kc % 2]
                    S_.op("pe", lambda e, x_=x_, kc=kc, pb=pb: e.transpose(
                        pb.ap()[:, 0:128], x_.ap()[:, kc * 128:(kc + 1) * 128], ident.ap()), r=[x_, ident], w=[pb])
                    S_.op("act", lambda e, kc=kc, pb=pb: e.activation(
                        h2f.ap()[:, kc, :], pb.ap()[:, 0:128], AF.Identity,
                        bias=modT.ap()[:, 3 * KC + kc, 0:1], scale=s2.ap()[:, kc:kc + 1]), r=[pb, modT, s2], w=[h2f])
                for kc in range(KC):
                    S_.op("pe", lambda e, kc=kc: e.matmul(banks[6].ap()[:, 0:E], h2f.ap()[:, kc, :], wr.ap()[:, kc, :],
                                                          start=(kc == 0), stop=(kc == KC - 1)), r=[h2f, wr], w=[banks[6]])
                S_.op("dve", lambda e: e.tensor_tensor(lg.ap(), banks[6].ap()[:, 0:E], brb.ap(), ALU.add), r=[banks[6], brb], w=[lg])
                S_.op("dve", lambda e: e.max(m8.ap(), lg.ap()), r=[lg], w=[m8])
                S_.op("dve", lambda e: e.max_index(i8.ap(), m8.ap(), lg.ap()), r=[lg, m8], w=[i8])
                S_.op("dve", lambda e, ti=ti: e.tensor_scalar(msk_all.ap()[:, ti, :], lg.ap(), m8.ap()[:, TOPK - 1:TOPK], None, ALU.is_ge),
                      r=[lg, m8], w=[msk_all])
                S_.op("dve", lambda e, ti=ti: e.tensor_copy(i8f_all.ap()[:, ti, :], i8.ap()[:, 0:4]), r=[i8], w=[i8f_all])
                S_.op("dve", lambda e: e.tensor_scalar(nm.ap(), m8.ap()[:, 0:1], -1.0, None, ALU.mult), r=[m8], w=[nm])
                S_.op("act", lambda e: e.activation(e4.ap(), m8.ap()[:, 0:4], AF.Exp, bias=nm.ap()), r=[m8, nm], w=[e4])
                S_.op("dve", lambda e: e.tensor_reduce(nm.ap(), e4.ap(), AX.X, ALU.add), r=[e4], w=[nm])
                S_.op("dve", lambda e: e.reciprocal(nm.ap(), nm.ap()), r=[nm], w=[nm])
                S_.op("dve", lambda e, ti=ti: e.tensor_scalar(gk_all.ap()[:, ti, :], e4.ap(), nm.ap(), None, ALU.mult),
                      r=[e4, nm], w=[gk_all])
                S_.op("pe", lambda e, ti=ti: e.matmul(banks[7].ap()[:, 0:E], ust.ap(), msk_all.ap()[:, ti, :],
                                                      start=True, stop=(ti == 0)), r=[ust, msk_all], w=[banks[7]])
                for tp in range(ti):
                    S_.op("pe", lambda e, tp=tp, ti=ti: e.matmul(banks[7].ap()[:, 0:E], ones128.ap(), msk_all.ap()[:, tp, :],
                                                                 start=False, stop=(tp == ti - 1)), r=[ones128, msk_all], w=[banks[7]])
                S_.op("dve", lambda e, ti=ti: e.tensor_copy(rank_all.ap()[:, ti, :], banks[7].ap()[:, 0:E]), r=[banks[7]], w=[rank_all])

        cnt = sb("cnt", [128, E], F32); nbt = sb("nbt", [128, E], F32); tq = sb("tq", [128, E], F32)
        cA = sb("cA", [128, E], F32); cB = sb("cB", [128, E], F32); startv = sb("startv", [128, E], F32)
        sfull = sb("sfull", [128, NTL, E], F32); oh = sb("oh", [128, NTL, E], F32); slot_f = sb("slot_f", [128, NTL, 4], F32)
        ie = sb("ie", [128, E], F32); thr = sb("thr", [128, NB], F32); cmpb = sb("cmpb", [128, NB, E], F32)
        ebf = sb("ebf", [128, NB], F32); ebm = sb("ebm", [128, NB], F32)
        ipf = sb("ipf", [128, FC], F32); ipd = sb("ipd", [128, DC * 2], F32); ipp = sb("ipp", [128, 1], F32)
        assert E >= max(FC, DC * 2)
        wtmp = cmpb
        S_.dma("sync", ie.ap(), iota_e.ap(), r=[iota_e], w=[ie])
        S_.dma("sync", thr.ap(), blkthr.ap(), r=[blkthr], w=[thr])
        S_.dma("sync", ipf.ap(), iota_pf.ap(), r=[iota_pf], w=[ipf])
        S_.dma("sync", ipd.ap(), iota_pd.ap(), r=[iota_pd], w=[ipd])
        S_.dma("sync", ipp.ap(), iota_p.ap(), r=[iota_p], w=[ipp])
        for i in range(NTL):
            S_.op("pe", lambda e, i=i: e.matmul(banks[7].ap()[:, 0:E], ones128.ap(), msk_all.ap()[:, i, :],
                                                start=(i == 0), stop=(i == NTL - 1)), r=[ones128, msk_all], w=[banks[7]])
        S_.op("dve", lambda e: e.tensor_copy(cnt.ap(), banks[7].ap()[:, 0:E]), r=[banks[7]], w=[cnt])
        S_.op("dve", lambda e: e.tensor_scalar(nbt.ap(), cnt.ap(), 0.0, None, ALU.is_gt), r=[cnt], w=[nbt])
        for j in range(1, -(-HALF // BS)):
            S_.op("dve", lambda e, j=j: e.tensor_scalar(tq.ap(), cnt.ap(), float(j * BS), None, ALU.is_gt), r=[cnt], w=[tq])
            S_.op("dve", lambda e: e.tensor_tensor(nbt.ap(), nbt.ap(), tq.ap(), ALU.add), r=[nbt, tq], w=[nbt])
        S_.op("dve", lambda e: e.tensor_scalar(nbt.ap(), nbt.ap(), float(BS), None, ALU.mult), r=[nbt], w=[nbt])
        S_.op("dve", lambda e: e.tensor_copy(cA.ap(), nbt.ap()), r=[nbt], w=[cA])
        cur, nxt = cA, cB
        sh_ = 1
        while sh_ < E:
            S_.op("dve", lambda e, cur=cur, nxt=nxt, sh_=sh_: e.tensor_copy(nxt.ap()[:, 0:sh_], cur.ap()[:, 0:sh_]), r=[cur], w=[nxt])
            S_.op("dve", lambda e, cur=cur, nxt=nxt, sh_=sh_: e.tensor_tensor(
                nxt.ap()[:, sh_:E], cur.ap()[:, sh_:E], cur.ap()[:, 0:E - sh_], ALU.add), r=[cur], w=[nxt])
            cur, nxt = nxt, cur
            sh_ *= 2
        endp = cur
        S_.op("dve", lambda e: e.tensor_tensor(startv.ap(), endp.ap(), nbt.ap(), ALU.subtract), r=[endp, nbt], w=[startv])
        S_.op("dve", lambda e: e.tensor_tensor(sfull.ap(), rank_all.ap(), startv.ap().unsqueeze(1).broadcast_to([128, NTL, E]), ALU.add),
              r=[rank_all, startv], w=[sfull])
        for k in range(4):
            S_.op("dve", lambda e, k=k: e.tensor_tensor(
                oh.ap(), ie.ap().unsqueeze(1).broadcast_to([128, NTL, E]),
                i8f_all.ap()[:, :, k:k + 1].broadcast_to([128, NTL, E]), ALU.is_equal), r=[ie, i8f_all], w=[oh])
            S_.op("dve", lambda e: e.tensor_tensor(oh.ap(), oh.ap(), sfull.ap(), ALU.mult), r=[oh, sfull], w=[oh])
            S_.op("dve", lambda e, k=k: e.tensor_reduce(slot_f.ap()[:, :, k], oh.ap(), AX.X, ALU.add), r=[oh], w=[slot_f])
        S_.op("dve", lambda e: e.tensor_copy(slot_u.ap(), slot_f.ap()), r=[slot_f], w=[slot_u])
        S_.op("dve", lambda e: e.tensor_tensor(
            cmpb.ap(), endp.ap().unsqueeze(1).broadcast_to([128, NB, E]),
            thr.ap().unsqueeze(2).broadcast_to([128, NB, E]), ALU.is_le), r=[endp, thr], w=[cmpb])
        S_.op("dve", lambda e: e.tensor_reduce(ebf.ap(), cmpb.ap(), AX.X, ALU.add), r=[cmpb], w=[ebf])
        S_.op("dve", lambda e: e.tensor_scalar(ebf.ap(), ebf.ap(), float(E - 1), None, ALU.min), r=[ebf], w=[ebf])
        S_.op("dve", lambda e: e.tensor_scalar(ebm.ap(), ebf.ap(), float(FC * 128), None, ALU.mult), r=[ebf], w=[ebm])
        S_.op("dve", lambda e: e.tensor_tensor(
            wtmp.ap()[:, :, 0:FC], ebm.ap().unsqueeze(2).broadcast_to([128, NB, FC]),
            ipf.ap().unsqueeze(1).broadcast_to([128, NB, FC]), ALU.add), r=[ebm, ipf], w=[wtmp])
        S_.op("dve", lambda e: e.tensor_copy(widx_g.ap(), wtmp.ap()[:, :, 0:FC]), r=[wtmp], w=[widx_g])
        S_.op("dve", lambda e: e.tensor_scalar(ebm.ap(), ebf.ap(), float(DC * 2 * 128), None, ALU.mult), r=[ebf], w=[ebm])
        S_.op("dve", lambda e: e.tensor_tensor(
            wtmp.ap()[:, :, 0:DC * 2], ebm.ap().unsqueeze(2).broadcast_to([128, NB, DC * 2]),
            ipd.ap().unsqueeze(1).broadcast_to([128, NB, DC * 2]), ALU.add), r=[ebm, ipd], w=[wtmp])
        S_.op("dve", lambda e: e.tensor_copy(widx_d.ap(), wtmp.ap()[:, :, 0:DC * 2]), r=[wtmp], w=[widx_d])
        S_.op("dve", lambda e: e.tensor_scalar(ebm.ap(), ebf.ap(), 128.0, ipp.ap(), ALU.mult, ALU.add), r=[ebf, ipp], w=[ebm])
        S_.op("dve", lambda e: e.tensor_copy(bidx.ap(), ebm.ap()), r=[ebm], w=[bidx])
        S_.op("dve", lambda e: e.tensor_copy(bdidx.ap(), ebf.ap()), r=[ebf], w=[bdidx])
        for i in range(NTL):
            S_.dma("sync", hk.ap(), h2tok_s.ap()[i * 128:(i + 1) * 128, :], r=[h2tok_s], w=[hk])
            for k in range(4):
                S_.idma(xs_s.ap(), hk.ap(), slot_u.ap()[:, i, k:k + 1], scatter=True, r=[hk, slot_u], w=[xs_s])
    barrier()
    if _STOP == 'E':
        S_.finish()
        return nc

    LIM = 7.0
    with ExitStack() as es:
        def sb(name, shape, dt):
            return Buf(name, es.enter_context(nc.sbuf_tensor(name, list(shape), dt)).ap())
        h2T = sb("h2Tf", [128, KC, BS], BF16)
        xr = [sb("xr0", [128, D], BF16), sb("xr1", [128, D], BF16)]
        STW = max(KC * 128, H2 * 512)
        st = [sb("st0", [128, STW], F32), sb("st1", [128, STW], F32), sb("st2", [128, STW], F32)]
        wgb = [sb("wgb0", [128, KC, 128], BF16), sb("wgb1", [128, KC, 128], BF16)]
        wub = [sb("wub0", [128, KC, 128], BF16), sb("wub1", [128, KC, 128], BF16)]
        wdb = [sb("wdb0", [128, H2, 512], BF16), sb("wdb1", [128, H2, 512], BF16)]
        actT = sb("actT", [128, FC, BS], BF16)
        ta = sb("ta", [128, BS], F32); tb_ = sb("tb", [128, BS], F32); tsg = sb("tsg", [128, BS], F32)
        bgb = [sb("bgb0", [128, FC], F32), sb("bgb1", [128, FC], F32)]
        bub = [sb("bub0", [128, FC], F32), sb("bub1", [128, FC], F32)]
        bdb = [sb("bdb0", [128, D], F32), sb("bdb1", [128, D], F32)]
        yc = [sb("yc0", [128, 512], F32), sb("yc1", [128, 512], F32), sb("yc2", [128, 512], F32), sb("yc3", [128, 512], F32)]
        wdb2 = [wdb, [sb("wdb2", [128, H2, 512], BF16), sb("wdb3", [128, H2, 512], BF16)]]
        cnt_ = dict(sti=0, xri=0, yci=0)

        def preamble(b):
            bg_ = bgb[b % 2]; bu_ = bub[b % 2]; bd_ = bdb[b % 2]
            S_.idma(bg_.ap(), bgT.ap(), bidx.ap()[:, b:b + 1], scatter=False, r=[bgT, bidx], w=[bg_])
            S_.idma(bu_.ap(), buT.ap(), bidx.ap()[:, b:b + 1], scatter=False, r=[buT, bidx], w=[bu_])
            S_.idma(bd_.ap(), b_down.ap(), bdidx.ap()[:, b:b + 1], scatter=False, r=[b_down, bdidx], w=[bd_])
            for tt in range(BS // 128):
                x_ = xr[cnt_["xri"] % 2]; cnt_["xri"] += 1
                S_.dma("sync", x_.ap(), xs_s.ap()[b * BS + tt * 128:b * BS + (tt + 1) * 128, :], r=[xs_s], w=[x_])
                for k4 in range(KC // 4):
                    pb = banks[k4 % 2]
                    pbv = pb.ap().bitcast(BF16)
                    for q_ in range(4):
                        kc = k4 * 4 + q_
                        S_.op("pe", lambda e, x_=x_, kc=kc, q_=q_, pbv=pbv: e.transpose(
                            pbv[:, q_ * 128:(q_ + 1) * 128], x_.ap()[:, kc * 128:(kc + 1) * 128], identb.ap()),
                            r=[x_, identb], w=[pb])
                    if k4 % 2 == 0:
                        S_.op("act", lambda e, k4=k4, tt=tt, pbv=pbv: e.activation(
                            h2T.ap()[:, k4 * 4:(k4 + 1) * 4, tt * 128:(tt + 1) * 128],
                            pbv[:, 0:512].rearrange("p (c t) -> p c t", t=128), AF.Copy), r=[pb], w=[h2T])
                    else:
                        S_.op("dve", lambda e, k4=k4, tt=tt, pbv=pbv: e.tensor_copy(
                            h2T.ap()[:, k4 * 4:(k4 + 1) * 4, tt * 128:(tt + 1) * 128],
                            pbv[:, 0:512].rearrange("p (c t) -> p c t", t=128)), r=[pb], w=[h2T])

        def load_step(step):
            kind, b, j, gi = step
            if kind == "fc":
                wg_ = wgb[gi % 2]; wu_ = wub[gi % 2]
                sg_ = st[cnt_["sti"] % 3]; cnt_["sti"] += 1
                S_.idma(sg_.ap()[:, 0:KC * 128], w_gate.ap(), widx_g.ap()[:, b, j:j + 1], scatter=False, r=[w_gate, widx_g], w=[sg_])
                S_.op("dve", lambda e, sg_=sg_, wg_=wg_: e.tensor_copy(
                    wg_.ap(), sg_.ap()[:, 0:KC * 128].rearrange("p (kc f) -> p kc f", f=128)), r=[sg_], w=[wg_])
                su_ = st[cnt_["sti"] % 3]; cnt_["sti"] += 1
                S_.idma(su_.ap()[:, 0:KC * 128], w_up.ap(), widx_g.ap()[:, b, j:j + 1], scatter=False, r=[w_up, widx_g], w=[su_])
                S_.op("act", lambda e, su_=su_, wu_=wu_: e.activation(
                    wu_.ap(), su_.ap()[:, 0:KC * 128].rearrange("p (kc f) -> p kc f", f=128), AF.Copy), r=[su_], w=[wu_])
            else:
                wd_ = wdb2[gi % 2]
                for hf in range(2):
                    sd_ = st[cnt_["sti"] % 3]; cnt_["sti"] += 1
                    S_.idma(sd_.ap()[:, 0:H2 * 512], w_down.ap(), widx_d.ap()[:, b, j * 2 + hf:j * 2 + hf + 1], scatter=False,
                            r=[w_down, widx_d], w=[sd_])
                    if hf == 0:
                        S_.op("act", lambda e, sd_=sd_, wd_=wd_: e.activation(
                            wd_[0].ap(), sd_.ap()[:, 0:H2 * 512].rearrange("p (fc d) -> p fc d", d=512), AF.Copy),
                            r=[sd_], w=[wd_[0]])
                    else:
                        S_.op("dve", lambda e, sd_=sd_, wd_=wd_: e.tensor_copy(
                            wd_[1].ap(), sd_.ap()[:, 0:H2 * 512].rearrange("p (fc d) -> p fc d", d=512)),
                            r=[sd_], w=[wd_[1]])

        def compute_step(step):
            kind, b, j, gi = step
            bg_ = bgb[b % 2]; bu_ = bub[b % 2]; bd_ = bdb[b % 2]
            if kind == "fc":
                fc = j
                psg = banks[gi % 2]; psu = banks[2 + gi % 2]
                wg_ = wgb[gi % 2]; wu_ = wub[gi % 2]
                for kc in range(KC):
                    S_.op("pe", lambda e, kc=kc, psg=psg, wg_=wg_: e.matmul(
                        psg.ap()[:, 0:BS], wg_.ap()[:, kc, :], h2T.ap()[:, kc, :], start=(kc == 0), stop=(kc == KC - 1)),
                        r=[wg_, h2T], w=[psg])
                for kc in range(KC):
                    S_.op("pe", lambda e, kc=kc, psu=psu, wu_=wu_: e.matmul(
                        psu.ap()[:, 0:BS], wu_.ap()[:, kc, :], h2T.ap()[:, kc, :], start=(kc == 0), stop=(kc == KC - 1)),
                        r=[wu_, h2T], w=[psu])
                S_.op("dve", lambda e, psg=psg, fc=fc, bg_=bg_: e.tensor_scalar(
                    ta.ap(), psg.ap()[:, 0:BS], bg_.ap()[:, fc:fc + 1], LIM, ALU.add, ALU.min), r=[psg, bg_], w=[ta])
                S_.op("act", lambda e: e.activation(tsg.ap(), ta.ap(), AF.Sigmoid, scale=1.702), r=[ta], w=[tsg])
                S_.op("dve", lambda e, psu=psu, fc=fc, bu_=bu_: e.tensor_scalar(
                    tb_.ap(), psu.ap()[:, 0:BS], bu_.ap()[:, fc:fc + 1], LIM, ALU.add, ALU.min), r=[psu, bu_], w=[tb_])
                S_.op("dve", lambda e: e.tensor_scalar(tb_.ap(), tb_.ap(), -LIM, 1.0, ALU.max, ALU.add), r=[tb_], w=[tb_])
                S_.op("dve", lambda e: e.tensor_tensor(ta.ap(), ta.ap(), tsg.ap(), ALU.mult), r=[ta, tsg], w=[ta])
                S_.op("dve", lambda e, fc=fc: e.tensor_tensor(actT.ap()[:, fc, :], ta.ap(), tb_.ap(), ALU.mult),
                      r=[ta, tb_], w=[actT])
            else:
                dc = j
                wd_ = wdb2[gi % 2]
                for tt in range(BS // 128):
                    for fc in range(FC):
                        S_.op("pe", lambda e, tt=tt, fc=fc, wd_=wd_: e.matmul(
                            banks[4 + tt].ap(), actT.ap()[:, fc, tt * 128:(tt + 1) * 128], wd_[fc // H2].ap()[:, fc % H2, :],
                            start=(fc == 0), stop=(fc == FC - 1)), r=[actT, wd_[fc // H2]], w=[banks[4 + tt]])
                for tt in range(BS // 128):
                    y_ = yc[cnt_["yci"] % 4]; cnt_["yci"] += 1
                    S_.op("dve", lambda e, tt=tt, dc=dc, y_=y_, bd_=bd_: e.tensor_tensor(
                        y_.ap(), banks[4 + tt].ap(), bd_.ap()[:, dc * 512:(dc + 1) * 512], ALU.add),
                        r=[banks[4 + tt], bd_], w=[y_])
                    yp = ys_parts[(dc * 512) // YW]; c0 = (dc * 512) % YW
                    S_.dma("sync", yp.ap()[b * BS + tt * 128:b * BS + (tt + 1) * 128, c0:c0 + 512], y_.ap(),
                           r=[y_], w=[yp])

        steps = []
        gfc = 0; gdc = 0
        for b in range(NB):
            for fc in range(FC):
                steps.append(("fc", b, fc, gfc)); gfc += 1
            for dc in range(DC):
                steps.append(("dc", b, dc, gdc)); gdc += 1
        preamble(0)
        load_step(steps[0])
        for i, stp in enumerate(steps):
            if i + 1 < len(steps):
                load_step(steps[i + 1])
            compute_step(stp)
            if stp[0] == "dc" and stp[2] == 0 and stp[1] + 1 < NB:
                preamble(stp[1] + 1)
    barrier()

    with ExitStack() as es:
        def sb(name, shape, dt):
            return Buf(name, es.enter_context(nc.sbuf_tensor(name, list(shape), dt)).ap())
        x1t = sb("x1t", [128, D], F32); g5b = sb("g5b", [128, D], F32); fgb = sb("fgb", [128, D], F32)
        yk = [sb("yk0", [128, D], F32), sb("yk1", [128, D], F32)]
        accs = [sb("acc0", [128, D], F32), sb("acc1", [128, D], F32)]
        ss = sb("ssG", [128, 8], F32); rs = sb("rsG", [128, 8], F32); junk = sb("junkG", [128, 512], F32)
        S_.dma("sync", g5b.ap(), bcast_rows(modrow_s.ap().tensor, D, D), r=[modrow_s], w=[g5b])
        S_.dma("sync", fgb.ap(), bcast_rows(fg_row.ap().tensor, 0, D), r=[fg_row], w=[fgb])
        yi = 0
        for i in range(NTL):
            rows = slice(i * 128, (i + 1) * 128)
            acc = accs[i % 2]
            S_.dma("sync", x1t.ap(), x1_s.ap()[rows, :], r=[x1_s], w=[x1t])
            for k in range(4):
                y_ = yk[yi % 2]; yi += 1
                for yp_i, yp in enumerate(ys_parts):
                    S_.idma(y_.ap()[:, yp_i * YW:(yp_i + 1) * YW], yp.ap(), slot_u.ap()[:, i, k:k + 1], scatter=False,
                            r=[yp, slot_u], w=[y_])
                if k == 0:
                    S_.op("dve", lambda e, y_=y_, i=i, acc=acc: e.tensor_scalar(
                        acc.ap(), y_.ap(), gk_all.ap()[:, i, 0:1], None, ALU.mult), r=[y_, gk_all], w=[acc])
                else:
                    S_.op("dve", lambda e, y_=y_, i=i, k=k, acc=acc: e.scalar_tensor_tensor(
                        acc.ap(), y_.ap(), gk_all.ap()[:, i, k:k + 1], acc.ap(), ALU.mult, ALU.add), r=[y_, gk_all, acc], w=[acc])
            S_.op("dve", lambda e, acc=acc: e.tensor_tensor(acc.ap(), acc.ap(), g5b.ap(), ALU.mult), r=[acc, g5b], w=[acc])
            S_.op("dve", lambda e, acc=acc: e.tensor_tensor(acc.ap(), acc.ap(), x1t.ap(), ALU.add), r=[acc, x1t], w=[acc])
            for ch in range(D // 512):
                S_.op("act", lambda e, ch=ch, acc=acc: e.activation(
                    junk.ap(), acc.ap()[:, ch * 512:(ch + 1) * 512], AF.Square,
                    accum_out=ss.ap()[:, ch:ch + 1]), r=[acc], w=[junk, ss])
            S_.op("dve", lambda e: e.tensor_reduce(rs.ap()[:, 1:2], ss.ap()[:, 0:D // 512], AX.X, ALU.add), r=[ss], w=[rs])
            S_.op("dve", lambda e: e.tensor_scalar(rs.ap()[:, 0:1], rs.ap()[:, 1:2], 1.0 / D, EPS, ALU.mult, ALU.add),
                  r=[rs], w=[rs])
            S_.op("act", lambda e: e.activation(rs.ap()[:, 0:1], rs.ap()[:, 0:1], AF.Sqrt), r=[rs], w=[rs])
            S_.op("dve", lambda e: e.reciprocal(rs.ap()[:, 0:1], rs.ap()[:, 0:1]), r=[rs], w=[rs])
            S_.op("act", lambda e, acc=acc: e.activation(acc.ap(), acc.ap(), AF.Copy, scale=rs.ap()[:, 0:1]), r=[acc, rs], w=[acc])
            S_.op("dve", lambda e, acc=acc: e.tensor_tensor(acc.ap(), acc.ap(), fgb.ap(), ALU.mult), r=[acc, fgb], w=[acc])
            S_.dma("sync", out.ap()[rows, :], acc.ap(), r=[acc], w=[out], is_output=True)
    barrier()
    S_.finish()
    return nc


def token_order(cfg, s):
    S, HALF = cfg["S"], cfg["HALF"]
    own = np.arange(s * HALF, (s + 1) * HALF)
    if s == 0:
        before = np.arange(S - 8, S); after = np.arange(HALF, HALF + 8); rest = np.arange(HALF + 8, S - 8)
        hm = np.array([0.0] * 8 + [1.0] * 8, np.float32)
    else:
        before = np.arange(HALF - 8, HALF); after = np.arange(0, 8); rest = np.arange(8, HALF - 8)
        hm = np.array([1.0] * 8 + [0.0] * 8, np.float32)
    return own, np.concatenate([before, after, rest]), hm


def fmaj(v, n=128):
    v = np.asarray(v, np.float32)
    return np.ascontiguousarray(v.reshape(-1, n).T)


def prep_shared(cfg, inp):
    D, E, FF, FC, PG = cfg["D"], cfg["E"], cfg["FF"], cfg["FC"], cfg["PG"]
    f = lambda a: np.ascontiguousarray(np.asarray(a, np.float32))
    sh = {}
    sh["w_ada"] = f(inp["w_ada"][0]); sh["bT_ada"] = fmaj(inp["b_ada"][0])
    sh["g1T"] = fmaj(inp["norm1_g"][0]); sh["g2T"] = fmaj(inp["norm2_g"][0])
    sh["fg_row"] = f(inp["final_g"]).reshape(1, D)
    sh["w_in"] = f(inp["w_in"][0])
    sh["gq"] = np.ascontiguousarray(np.tile(f(inp["q_norm_g"][0])[None, :], (128, 1)))
    sh["gk"] = np.ascontiguousarray(np.tile(f(inp["k_norm_g"][0])[None, :], (128, 1)))
    sh["w_pool"] = f(inp["w_pool"][0]).reshape(4 * PG, PG)
    sh["pscT"] = fmaj(inp["pool_scale"][0])
    sh["w_out"] = f(inp["w_out"][0])
    sh["w_router"] = np.ascontiguousarray(f(inp["w_router"][0]).reshape(cfg["KC"], 128, E).transpose(1, 0, 2).reshape(128, cfg["KC"] * E))
    sh["b_router"] = np.ascontiguousarray(np.tile(f(inp["b_router"][0]).reshape(1, E), (128, 1)))
    KC, DC, H2 = cfg["KC"], D // 512, FC // 2
    G = cfg["BS"]; NB = -(-4 * cfg["HALF"] // G) + E
    def relayout_gu(w):
        w = f(w).reshape(E, KC, 128, FC, 128)
        return np.ascontiguousarray(w.transpose(0, 3, 2, 1, 4)).reshape(E * FC * 128, KC * 128)
    sh["w_gate"] = relayout_gu(inp["w_gate"][0])
    sh["w_up"] = relayout_gu(inp["w_up"][0])
    wd = f(inp["w_down"][0]).reshape(E, 2, H2, 128, DC, 512)
    sh["w_down"] = np.ascontiguousarray(wd.transpose(0, 4, 1, 3, 2, 5)).reshape(E * DC * 2 * 128, H2 * 512)
    sh["bgT"] = np.ascontiguousarray(f(inp["b_gate"][0]).reshape(E, FC, 128).transpose(0, 2, 1)).reshape(E * 128, FC)
    sh["buT"] = np.ascontiguousarray(f(inp["b_up"][0]).reshape(E, FC, 128).transpose(0, 2, 1)).reshape(E * 128, FC)
    sh["b_down"] = f(inp["b_down"][0])
    p = np.arange(128, dtype=np.float32)
    sh["ustrict"] = np.ascontiguousarray((p[:, None] < p[None, :]).astype(np.float32))
    sh["iota_e"] = np.ascontiguousarray(np.tile(np.arange(E, dtype=np.float32)[None, :], (128, 1)))
    sh["iota_pf"] = np.ascontiguousarray(p[:, None] + 128.0 * np.arange(FC, dtype=np.float32)[None, :])
    sh["iota_pd"] = np.ascontiguousarray(p[:, None] + 128.0 * np.arange(DC * 2, dtype=np.float32)[None, :])
    sh["iota_p"] = np.ascontiguousarray(p[:, None])
    sh["blkthr"] = np.ascontiguousarray(np.tile((np.arange(NB, dtype=np.float32) * G)[None, :], (128, 1)))
    sh["ident"] = np.eye(128, dtype=np.float32)
    return sh


def prep_core(cfg, inp, sh, b, s):
    D, S, HALF, CTX, NT, KC = cfg["D"], cfg["S"], cfg["HALF"], cfg["CTX"], cfg["NT"], cfg["KC"]
    own, other, hm = token_order(cfg, s)
    x = np.asarray(inp["x"], np.float32); ctx = np.asarray(inp["ctx"], np.float32)
    m = dict(sh)
    m["xin"] = np.ascontiguousarray(np.concatenate([x[b][own], x[b][other], ctx[b]], axis=0))
    cT = np.stack([fmaj(np.asarray(inp["c"], np.float32)[b]), fmaj(inp["c_ctx"])], axis=-1)
    m["cT"] = np.ascontiguousarray(cT)
    tok = np.concatenate([own, other]).astype(np.float64)
    freqs = ROPE_THETA ** (-np.arange(32, dtype=np.float64) / 32.0)
    freqs32 = freqs.astype(np.float32)
    row = np.floor(tok / GRID_W).astype(np.float32); col = (tok % GRID_W).astype(np.float32)
    ang = np.concatenate([row[:, None] * freqs32[None, :], col[:, None] * freqs32[None, :]], axis=-1).astype(np.float32)
    cs = np.concatenate([np.cos(ang), np.sin(ang)], axis=-1).astype(np.float32)
    cs_ctx = np.concatenate([np.ones((CTX, 64), np.float32), np.zeros((CTX, 64), np.float32)], axis=-1)
    m["cs"] = np.ascontiguousarray(np.concatenate([cs, cs_ctx], axis=0))
    t = own
    ic = []
    for w in (2, 4, 8, 16):
        lo = np.clip(t - w // 2, 0, S); hi = np.clip(t + w // 2, 0, S)
        ic.append(1.0 / (hi - lo).astype(np.float32))
    m["invcnt"] = np.ascontiguousarray(np.stack(ic).astype(np.float32))
    m["hmask"] = hm.reshape(1, 16)
    return m


def kernel(**inputs):
    cfg = FULL
    B, S, HALF, D = cfg["B"], cfg["S"], cfg["HALF"], cfg["D"]
    inp = {k: np.asarray(v) for k, v in inputs.items()}
    sh = prep_shared(cfg, inp)
    in_maps = []
    for b in range(B):
        for s in range(2):
            in_maps.append(prep_core(cfg, inp, sh, b, s))
    nc = build_program(cfg, debug=False)
    res = run_bass_kernel_spmd(nc, in_maps, core_ids=list(range(2 * B)))
    out = np.empty((B, S, D), np.float32)
    for b in range(B):
        for s in range(2):
            out[b, s * HALF:(s + 1) * HALF] = res.results[b * 2 + s]["out"]
    return out
```
